# Optimizing a Trainium2 kernel written in Bass

```python
import math
import jax, jax.numpy as jnp
from jax import lax
import numpy as np

D_MODEL = 1024
BATCH = 32
SEQ = 2048
DEPTH = 1

MIX_WIDTH = D_MODEL
DIFF_WIDTH = MIX_WIDTH // 2
GLA_WIDTH = MIX_WIDTH - DIFF_WIDTH
DIFF_HEADS = 4
DIFF_HEAD_DIM = DIFF_WIDTH // (2 * DIFF_HEADS)
Q_BLOCK = 128
ROPE_THETA = 10000.0
GLA_HEADS = 4
GLA_DV = GLA_WIDTH // GLA_HEADS
GLA_DK = GLA_DV // 2
GLA_GATE_RANK = 16
GLA_GATE_TAU = 16.0
GLA_CHUNK = 64
PEER_HEADS = 8
PEER_TOPK = 16
PEER_NKEYS = 128
PEER_N_EXPERTS = PEER_NKEYS * PEER_NKEYS
PEER_QDIM = 256
PEER_TOKEN_BLOCK = 128
EPS = 1e-6

DIFF_Q_COLS = DIFF_HEADS * 2 * DIFF_HEAD_DIM
DIFF_K_COLS = DIFF_HEADS * 2 * DIFF_HEAD_DIM
DIFF_V_COLS = DIFF_HEADS * 2 * DIFF_HEAD_DIM
GLA_Q_COLS = GLA_HEADS * GLA_DK
GLA_K_COLS = GLA_HEADS * GLA_DK
GLA_V_COLS = GLA_HEADS * GLA_DV
GLA_R_COLS = GLA_HEADS * GLA_DV
GLA_G_COLS = GLA_GATE_RANK
IN_COLS = (DIFF_Q_COLS + DIFF_K_COLS + DIFF_V_COLS + GLA_Q_COLS + GLA_K_COLS
           + GLA_V_COLS + GLA_R_COLS + GLA_G_COLS)

kernel_name = "hybrid_diffattn_gla_peer_adaln"


def rmsnorm(x, g):
    xf = x.astype(jnp.float32)
    y = xf * lax.rsqrt(jnp.mean(xf * xf, axis=-1, keepdims=True) + EPS)
    return (y * g.astype(jnp.float32)).astype(x.dtype)


def rope_cos_sin(positions, dim):
    inv_freq = ROPE_THETA ** (-jnp.arange(0, dim, 2, dtype=jnp.float32) / dim)
    ang = positions.astype(jnp.float32)[..., None] * inv_freq
    return jnp.cos(ang), jnp.sin(ang)


def apply_rope(x, cos, sin):
    x1, x2 = jnp.split(x, 2, axis=-1)
    cos = cos.astype(x.dtype)
    sin = sin.astype(x.dtype)
    return jnp.concatenate([x1 * cos - x2 * sin, x2 * cos + x1 * sin], axis=-1)


def diff_attention(q, k, v, qn_g, kn_g, lam_q1, lam_k1, lam_q2, lam_k2, subln_g, positions, lambda_init):
    B, S, H, _, d = q.shape
    nb = S // Q_BLOCK
    q = rmsnorm(q, qn_g)
    k = rmsnorm(k, kn_g)
    cos, sin = rope_cos_sin(positions, d)
    cos = cos[:, :, None, None, :]
    sin = sin[:, :, None, None, :]
    q = apply_rope(q, cos, sin)
    k = apply_rope(k, cos, sin)
    lam = (jnp.exp(jnp.sum(lam_q1.astype(jnp.float32) * lam_k1.astype(jnp.float32)))
           - jnp.exp(jnp.sum(lam_q2.astype(jnp.float32) * lam_k2.astype(jnp.float32)))
           + lambda_init)
    scale = d ** -0.5
    qb = q.reshape(B, nb, Q_BLOCK, H, 2, d).transpose(1, 0, 3, 4, 2, 5)
    kt = k.transpose(0, 2, 3, 1, 4)
    vt = v.transpose(0, 2, 1, 3)
    key_pos = jnp.arange(S)

    def block(args):
        qblk, i = args
        s = jnp.einsum('bhmqd,bhmkd->bhmqk', qblk, kt,
                       preferred_element_type=jnp.float32) * scale
        q_pos = i * Q_BLOCK + jnp.arange(Q_BLOCK)
        mask = key_pos[None, :] <= q_pos[:, None]
        p = jax.nn.softmax(jnp.where(mask, s, -jnp.inf), axis=-1)
        w = p[:, :, 0] - lam * p[:, :, 1]
        return jnp.einsum('bhqk,bhke->bhqe', w.astype(vt.dtype), vt)

    o = lax.map(block, (qb, jnp.arange(nb)))
    o = o.transpose(1, 0, 3, 2, 4).reshape(B, S, H, 2 * d)
    o = rmsnorm(o, subln_g) * (1.0 - lambda_init)
    return o.reshape(B, S, H * 2 * d)


def gla(q, k, v, r, g_low, w_gate2, b_gate, norm_g):
    B, S, H, dk = q.shape
    dv = v.shape[-1]
    C = GLA_CHUNK
    nc = S // C
    log_a = jax.nn.log_sigmoid((g_low @ w_gate2 + b_gate).astype(jnp.float32)) / GLA_GATE_TAU
    log_a = log_a.reshape(B, S, H, dk)

    def chunks(t):
        return t.reshape(B, nc, C, H, t.shape[-1]).transpose(0, 3, 1, 2, 4).astype(jnp.float32)

    qc = chunks(q) * (dk ** -0.5)
    kc = chunks(k)
    vc = chunks(v)
    bc = jnp.cumsum(chunks(log_a), axis=3)
    b_last = bc[:, :, :, -1:]
    b_mid = bc[:, :, :, C // 2:C // 2 + 1]
    a = jnp.einsum('bhncd,bhnjd->bhncj', qc * jnp.exp(bc - b_mid), kc * jnp.exp(b_mid - bc))
    causal = jnp.tril(jnp.ones((C, C), dtype=bool))
    a = jnp.where(causal, a, 0.0)
    o = jnp.einsum('bhncj,bhnje->bhnce', a, vc)
    ds = jnp.einsum('bhncd,bhnce->bhnde', kc * jnp.exp(b_last - bc), vc)
    decay = jnp.exp(b_last[:, :, :, 0])

    def step(state, inp):
        dec, dsn = inp
        return dec[..., None] * state + dsn, state

    s0 = jnp.zeros((B, H, dk, dv), jnp.float32)
    _, s_prev = lax.scan(step, s0, (jnp.moveaxis(decay, 2, 0), jnp.moveaxis(ds, 2, 0)))
    s_prev = jnp.moveaxis(s_prev, 0, 2)
    o = o + jnp.einsum('bhncd,bhnde->bhnce', qc * jnp.exp(bc), s_prev)
    o = o.transpose(0, 2, 3, 1, 4).reshape(B, S, H, dv)
    o = rmsnorm(o, norm_g).reshape(B, S, H * dv)
    return (o * jax.nn.silu(r.astype(jnp.float32))).astype(q.dtype)


def peer(h, w_query, b_query, keys1, keys2, expert_u, expert_v):
    B, S, D = h.shape
    tokens = h.reshape(-1, PEER_TOKEN_BLOCK, D)
    half = PEER_QDIM // 2

    def block(xb):
        T = xb.shape[0]
        q = (xb @ w_query + b_query).reshape(T, PEER_HEADS, PEER_QDIM)
        s1 = jnp.einsum('thd,kd->thk', q[..., :half], keys1)
        s2 = jnp.einsum('thd,kd->thk', q[..., half:], keys2)
        v1, i1 = lax.top_k(s1, PEER_TOPK)
        v2, i2 = lax.top_k(s2, PEER_TOPK)
        cand = (v1[..., :, None] + v2[..., None, :]).reshape(T, PEER_HEADS, PEER_TOPK * PEER_TOPK)
        cidx = (i1[..., :, None] * PEER_NKEYS + i2[..., None, :]).reshape(T, PEER_HEADS, PEER_TOPK * PEER_TOPK)
        sc, pos = lax.top_k(cand, PEER_TOPK)
        eidx = jnp.take_along_axis(cidx, pos, axis=-1)
        g = jax.nn.softmax(sc.astype(jnp.float32), axis=-1)
        u = expert_u[eidx]
        act = jax.nn.gelu(jnp.einsum('thkd,td->thk', u, xb).astype(jnp.float32), approximate=False) * g
        return jnp.einsum('thk,thkd->td', act.astype(xb.dtype), expert_v[eidx])

    return lax.map(block, tokens).reshape(B, S, D)


def in_proj_offsets():
    sizes = [DIFF_Q_COLS, DIFF_K_COLS, DIFF_V_COLS, GLA_Q_COLS, GLA_K_COLS, GLA_V_COLS, GLA_R_COLS]
    offs = []
    acc = 0
    for s in sizes:
        acc += s
        offs.append(acc)
    return offs


def setup_inputs(seed: int = 0) -> dict:
    key = jax.random.key(seed)
    ks = jax.random.split(key, 26)
    f32 = jnp.float32
    L, D = DEPTH, D_MODEL

    def nrm(k, shape, scale):
        return jax.random.normal(k, shape, f32) * scale

    x = jax.random.normal(ks[0], (BATCH, SEQ, D), f32)
    c = jax.random.normal(ks[1], (BATCH, D), f32)
    positions = (jnp.arange(SEQ, dtype=jnp.int32)[None, :]
                 + jax.random.randint(ks[2], (BATCH, 1), 0, 1024, dtype=jnp.int32))
    return {
        "x": x,
        "c": c,
        "positions": positions,
        "w_ada": nrm(ks[3], (L, D, 6 * D), D ** -0.5),
        "b_ada": nrm(ks[4], (L, 6 * D), 0.01),
        "norm1_g": 1.0 + nrm(ks[5], (L, D), 0.02),
        "w_in": nrm(ks[6], (L, D, IN_COLS), D ** -0.5),
        "qn_g": 1.0 + nrm(ks[7], (L, DIFF_HEAD_DIM), 0.02),
        "kn_g": 1.0 + nrm(ks[8], (L, DIFF_HEAD_DIM), 0.02),
        "lam_q1": nrm(ks[9], (L, DIFF_HEAD_DIM), 0.1),
        "lam_k1": nrm(ks[10], (L, DIFF_HEAD_DIM), 0.1),
        "lam_q2": nrm(ks[11], (L, DIFF_HEAD_DIM), 0.1),
        "lam_k2": nrm(ks[12], (L, DIFF_HEAD_DIM), 0.1),
        "diff_norm_g": 1.0 + nrm(ks[13], (L, 2 * DIFF_HEAD_DIM), 0.02),
        "w_gate2": nrm(ks[14], (L, GLA_GATE_RANK, GLA_HEADS * GLA_DK), GLA_GATE_RANK ** -0.5),
        "b_gate": nrm(ks[15], (L, GLA_HEADS * GLA_DK), 0.01),
        "gla_norm_g": 1.0 + nrm(ks[16], (L, GLA_DV), 0.02),
        "w_out": nrm(ks[17], (L, MIX_WIDTH, D), MIX_WIDTH ** -0.5),
        "norm2_g": 1.0 + nrm(ks[18], (L, D), 0.02),
        "w_query": nrm(ks[19], (L, D, PEER_HEADS * PEER_QDIM), D ** -0.5),
        "b_query": nrm(ks[20], (L, PEER_HEADS * PEER_QDIM), 0.01),
        "peer_keys1": nrm(ks[21], (L, PEER_NKEYS, PEER_QDIM // 2), (PEER_QDIM // 2) ** -0.5),
        "peer_keys2": nrm(ks[22], (L, PEER_NKEYS, PEER_QDIM // 2), (PEER_QDIM // 2) ** -0.5),
        "expert_u": nrm(ks[23], (L, PEER_N_EXPERTS, D), D ** -0.5),
        "expert_v": nrm(ks[24], (L, PEER_N_EXPERTS, D), PEER_HEADS ** -0.5),
    }


def reference(x, c, positions, w_ada, b_ada, norm1_g, w_in, qn_g, kn_g, lam_q1, lam_k1, lam_q2, lam_k2,
              diff_norm_g, w_gate2, b_gate, gla_norm_g, w_out, norm2_g, w_query, b_query,
              peer_keys1, peer_keys2, expert_u, expert_v):
    B, S, D = x.shape
    offs = in_proj_offsets()
    for l in range(DEPTH):
        mod = jax.nn.silu(c) @ w_ada[l] + b_ada[l]
        sh1, sc1, gt1, sh2, sc2, gt2 = [m[:, None, :] for m in jnp.split(mod, 6, axis=-1)]

        h = rmsnorm(x, norm1_g[l]) * (1.0 + sc1) + sh1
        proj = h @ w_in[l]
        dq, dk, dv, gq, gk, gv, gr, gg = jnp.split(proj, offs, axis=-1)
        lambda_init = 0.8 - 0.6 * math.exp(-0.3 * l)
        y_diff = diff_attention(
            dq.reshape(B, S, DIFF_HEADS, 2, DIFF_HEAD_DIM),
            dk.reshape(B, S, DIFF_HEADS, 2, DIFF_HEAD_DIM),
            dv.reshape(B, S, DIFF_HEADS, 2 * DIFF_HEAD_DIM),
            qn_g[l], kn_g[l], lam_q1[l], lam_k1[l], lam_q2[l], lam_k2[l], diff_norm_g[l],
            positions, lambda_init)
        y_gla = gla(
            gq.reshape(B, S, GLA_HEADS, GLA_DK),
            gk.reshape(B, S, GLA_HEADS, GLA_DK),
            gv.reshape(B, S, GLA_HEADS, GLA_DV),
            gr, gg, w_gate2[l], b_gate[l], gla_norm_g[l])
        mixed = jnp.concatenate([y_diff, y_gla], axis=-1) @ w_out[l]
        x = x + gt1 * mixed

        h2 = rmsnorm(x, norm2_g[l]) * (1.0 + sc2) + sh2
        x = x + gt2 * peer(h2, w_query[l], b_query[l], peer_keys1[l], peer_keys2[l], expert_u[l], expert_v[l])
    return x
```

```python
import math
import os
import numpy as np
import concourse.bass as bass
import concourse.mybir as mybir
from concourse.bass_utils import run_bass_kernel_spmd

F32 = mybir.dt.float32
BF16 = mybir.dt.bfloat16
I32 = mybir.dt.int32
AF = mybir.ActivationFunctionType
ALU = mybir.AluOpType
AX = mybir.AxisListType

D = 1024
NCORES = 8
EPS = 1e-6
NEXP = 16384
BIG = 1.0e4
GSUB = int(os.environ.get("GSUB", "9"))


class Key:
    __slots__ = ("name", "excl", "const", "lw", "rd", "sem", "semcnt")

    def __init__(self, name, excl=False, const=False):
        self.name = name
        self.excl = excl
        self.const = const
        self.lw = None
        self.rd = []
        self.sem = None
        self.semcnt = 0


class Sched:
    def __init__(self, nc):
        self.nc = nc
        self.engs = ("pe", "act", "dve", "pool", "sp")
        self.prog = {e: [] for e in self.engs}
        self.cnt = {e: 0 for e in self.engs}
        self.seen = {e: {} for e in self.engs}
        self.esem = {e: nc.alloc_semaphore(name="tl_" + e) for e in self.engs}
        self.n_ins = 0

    def _need(self, eng, tok, waits, raw):
        if tok is None:
            return
        if tok[0] == "e":
            _, f, n = tok
            if f == eng and eng == "pe":
                return
            k = ("e", f)
        else:
            _, key, n = tok
            k = ("d", key)
        if self.seen[eng].get(k, 0) >= n:
            return
        if n > waits.get(k, 0):
            waits[k] = n

    def _deps(self, eng, reads, writes, ww_ok=False):
        waits = {}
        for k in reads:
            self._need(eng, k.lw, waits, True)
        for k in writes:
            if not (ww_ok and k.lw is not None and k.lw[0] == "e" and k.lw[1] == eng):
                self._need(eng, k.lw, waits, False)
            for t in k.rd:
                self._need(eng, t, waits, False)
        for k, n in waits.items():
            self.seen[eng][k] = n
            sem = self.esem[k[1]] if k[0] == "e" else k[1].sem
            self.prog[eng].append(("w", sem, n))

    def _commit(self, tok, reads, writes):
        for k in reads:
            if k.excl:
                k.lw = tok
                k.rd = []
            elif not k.const:
                k.rd.append(tok)
        for k in writes:
            k.lw = tok
            k.rd = []

    def op(self, eng, fn, reads=(), writes=(), inc=True, ww_ok=False):
        self._deps(eng, reads, writes, ww_ok)
        self.n_ins += 1
        if inc:
            self.cnt[eng] += 1
            tok = ("e", eng, self.cnt[eng])
            self.prog[eng].append(("i", fn, self.esem[eng], 1))
        else:
            tok = ("e", eng, self.cnt[eng] + 1)
            self.prog[eng].append(("i", fn, None, 0))
        self._commit(tok, reads, writes)
        return tok

    def dma(self, eng, out, in_, owner, reads=(), writes=(), **kw):
        self._deps(eng, reads, writes)
        if owner.sem is None:
            owner.sem = self.nc.alloc_semaphore(name="d_" + owner.name)
        owner.semcnt += 16
        tok = ("d", owner, owner.semcnt)
        self.n_ins += 1
        self.prog[eng].append(
            ("i", lambda e, o=out, i=in_, kw=kw: e.dma_start(out=o, in_=i, **kw), owner.sem, 16))
        self._commit(tok, reads, writes)
        return tok

    def wait_all(self, eng, toks):
        waits = {}
        for t in toks:
            self._need(eng, t, waits, True)
        for k, n in waits.items():
            self.seen[eng][k] = n
            sem = self.esem[k[1]] if k[0] == "e" else k[1].sem
            self.prog[eng].append(("w", sem, n))

    def emit(self):
        progs = self.prog

        def run(e, lst):
            for it in lst:
                if it[0] == "w":
                    e.wait_ge(it[1], it[2])
                else:
                    ins = it[1](e)
                    if it[2] is not None:
                        ins.then_inc(it[2], it[3])

        with self.nc.Block() as block:
            @block.tensor
            def _(e):
                run(e, progs["pe"])

            @block.scalar
            def _(e):
                run(e, progs["act"])

            @block.vector
            def _(e):
                run(e, progs["dve"])

            @block.gpsimd
            def _(e):
                run(e, progs["pool"])

            @block.sync
            def _(e):
                run(e, progs["sp"])


class Tl:
    def __init__(self, nc, name, shape, dt, psum=False, const=False):
        if psum:
            self.t = nc.alloc_psum_tensor("p_" + name, shape, dt)
        else:
            self.t = nc.alloc_sbuf_tensor("s_" + name, shape, dt)
        self.k = Key(name, excl=psum, const=const)

    def __getitem__(self, idx):
        return self.t[idx]


def _consts():
    p = np.arange(128)
    same = (p[:, None] // 64) == (p[None, :] // 64)
    c = {}
    c["ident"] = np.eye(128, dtype=np.float32)
    c["causal"] = (p[:, None] <= p[None, :]).astype(np.float32)
    c["mcum"] = (same & (p[:, None] <= p[None, :])).astype(np.float32)
    c["mblk"] = same.astype(np.float32)
    c["mmid"] = (same & ((p[:, None] % 64) <= 32)).astype(np.float32)
    c["chunkind"] = np.stack([(p < 64), (p >= 64)], axis=1).astype(np.float32)
    invf = (10000.0 ** (-np.arange(0, 64, 2, dtype=np.float32) / 64)).astype(np.float32)
    c["invf"] = np.tile(invf[None, :], (128, 1)).astype(np.float32)
    order = ["ident", "causal", "mcum", "mblk", "mmid", "chunkind", "invf"]
    offs = {}
    cols = 0
    for n in order:
        offs[n] = (cols, c[n].shape[1])
        cols += c[n].shape[1]
    blob = np.concatenate([c[n] for n in order], axis=1).astype(np.float32)
    return blob, offs


def build_program(NB, SEQ, do_peer=True, dbg=(), stage="full", chain_casts=True):
    STAGES = ["pro", "ada", "norm", "qkv", "proj", "attn", "gla", "full"]
    slevel = STAGES.index(stage)
    NT = SEQ // 128
    ST = 2
    NST = NT // ST
    NTOK = NB * SEQ
    nc = bass.Bass("TRN2", target_bir_lowering=False)
    S = Sched(nc)
    cblob, coffs = _consts()
    CW = cblob.shape[1]

    def din(name, shape, dt=F32):
        return nc.dram_tensor(name, list(shape), dt, kind="ExternalInput").ap()

    x_d = din("x", [NTOK, D])
    cT_d = din("cT", [D, NB])
    pos_d = din("posT", [128, NB * NT], I32)
    wada_d = din("w_ada", [D, 6 * D])
    badaT_d = din("b_adaT", [128, 48])
    bada_d = din("b_ada", [1, 6 * D])
    g1T_d = din("g1T", [128, 8])
    g2T_d = din("g2T", [128, 8])
    win_d = din("w_in", [D, 3088])
    wout_d = din("w_out", [D, D])
    wq_d = din("w_query", [D, 2048])
    bqT_d = din("b_queryT", [128, 16])
    k1T_d = din("keys1T", [128, 128])
    k2T_d = din("keys2T", [128, 128])
    uT_d = din("uT", [D, NEXP])
    v_d = din("ev", [NEXP, D])
    vec64_d = din("vec64", [1, 6 * 64])
    vec128_d = din("vec128", [1, 2 * 128])
    wg2_d = din("w_gate2", [16, 256])
    bg_d = din("b_gate", [1, 256])
    cst_d = din("consts", [128, CW])
    y_d = nc.dram_tensor("y", [NTOK, D], F32, kind="ExternalOutput").ap()
    dbg_d = {n: nc.dram_tensor("dbg_" + n, list(shp), F32, kind="ExternalOutput").ap() for n, shp in dbg}

    win_s = nc.dram_tensor("win_s", [D, 3088], BF16, kind="Internal").ap()
    wout_s = nc.dram_tensor("wout_s", [D, D], BF16, kind="Internal").ap()
    wq_s = nc.dram_tensor("wq_s", [D, 2048], BF16, kind="Internal").ap()
    uT_s = nc.dram_tensor("uT_s", [D, NEXP], BF16, kind="Internal").ap()
    v_s = nc.dram_tensor("v_s", [NEXP, D], BF16, kind="Internal").ap()
    k_win_s, k_wout_s, k_wq_s, k_uT_s, k_v_s = (Key(n, const=True) for n in ("kwin", "kwout", "kwq", "kuT", "kv"))

    def sb(name, shape, dt=F32, const=False):
        return Tl(nc, name, shape, dt, const=const)

    cst = sb("cst", [128, CW], F32, const=True)
    identb = sb("identb", [128, 128], BF16, const=True)
    causb = sb("causb", [128, 128], BF16, const=True)
    onesr = sb("onesr", [1, 128], F32, const=True)
    cT = sb("cT", [128, 8, NB], F32, const=True)
    posi = sb("posi", [128, NB * NT], I32, const=True)
    posf = sb("posf", [128, NB * NT], F32, const=True)
    badaT = sb("badaT", [128, 48], F32, const=True)
    g1T = sb("g1T", [128, 8], F32, const=True)
    g2T = sb("g2T", [128, 8], F32, const=True)
    bqT = sb("bqT", [128, 16], F32, const=True)
    k1T = sb("k1T", [128, 128], BF16, const=True)
    k2T = sb("k2T", [128, 128], BF16, const=True)
    kst = sb("kst", [128, 128], F32)
    v64 = sb("v64", [128, 6 * 64], F32, const=True)
    v128 = sb("v128", [128, 256], F32, const=True)
    qgB = sb("qgB", [128, 64], F32, const=True)
    subgB = sb("subgB", [128, 128], F32, const=True)
    neglam = sb("neglam", [128, 1], F32, const=True)
    lamt = sb("lamt", [128, 64], F32)
    lams = sb("lams", [128, 4], F32)
    wg2 = sb("wg2", [16, 256], F32, const=True)
    bgr = sb("bgr", [1, 256], F32, const=True)
    cvals = sb("cvals", [128, 4], F32, const=True)

    def C(name):
        o, w = coffs[name]
        return cst[:, o:o + w]

    sT = sb("sT", [128, 8], F32)
    modT = sb("modT", [128, 4, 8], F32)
    gm1 = sb("gm1", [128, 8], F32)
    gm2 = sb("gm2", [128, 8], F32)
    gt1B = sb("gt1B", [128, D], F32)
    gt2B = sb("gt2B", [128, D], F32)
    wst = [sb("wst%d" % i, [128, 8, 128], F32) for i in range(2)]

    NRING = 4
    ring = [sb("ring%d" % i, [128, 8, 512], BF16) for i in range(NRING)]
    rc = [0]

    def ring_next():
        r = ring[rc[0] % NRING]
        rc[0] += 1
        return r

    KT = [sb("KT%d" % i, [128, 4, 128], BF16) for i in range(NT)]
    VC = [sb("VC%d" % i, [128, 4, 130], BF16) for i in range(NT)]
    Sst = [sb("Sst%d" % i, [128, 128], F32) for i in range(2)]

    x1 = sb("x1", [128, ST, D], F32)
    xk = [Key("x1_%d" % i) for i in range(ST)]
    junk = sb("junk", [128, D], BF16)
    xn = sb("xn", [128, D], BF16)
    st1 = sb("st1", [128, 4], F32)
    hT = sb("hT", [128, 8, 128], BF16)
    h2T = sb("h2T", [128, 8, ST * 128], BF16)
    sq = sb("sq", [128, 512], F32)
    ss8 = sb("ss8", [128, 8], F32)
    qn = sb("qn", [128, 512], F32)
    rt = [sb("rt%d" % i, [128, 256], F32) for i in range(2)]
    qr = sb("qr", [128, 512], BF16)
    qT = sb("qT", [128, 4, 128], BF16)
    ang = sb("ang", [128, 32], F32)
    ang2 = sb("ang2", [128, 32], F32)
    ang3 = sb("ang3", [128, 32], F32)
    angi = sb("angi", [128, 32], I32)
    sinT = sb("sinT", [128, 32], F32)
    cosT = sb("cosT", [128, 32], F32)
    PT = [sb("PT%d" % i, [128, 512], BF16) for i in range(2)]
    osb = sb("osb", [128, 128], F32)
    rz = sb("rz", [128, 4], F32)
    ybf = sb("ybf", [128, D], BF16)
    yT = sb("yT", [128, 8, 128], BF16)
    gqk = sb("gqk", [128, 512], F32)
    gv = sb("gv", [128, 512], F32)
    sr = sb("sr", [128, 512], F32)
    ggT = sb("ggT", [16, 128], F32)
    la = sb("la", [128, 256], F32)
    gl = [sb("gl%d" % i, [128, 256], F32) for i in range(8)]
    dec = sb("dec", [128, 2, 2], F32)
    T3 = sb("T3", [128, 3, 128], F32)
    ATs = sb("ATs", [128, 128], F32)

    psT = Tl(nc, "psT", [128, 1024], BF16, psum=True)
    psP = [Tl(nc, "psP%d" % i, [128, 512], F32, psum=True) for i in range(2)]
    psS = [Tl(nc, "psS%d" % i, [128, 512], F32, psum=True) for i in range(2)]
    psO = Tl(nc, "psO", [128, 512], F32, psum=True)
    psG = Tl(nc, "psG", [128, 512], F32, psum=True)
    psG2 = Tl(nc, "psG2", [128, 512], F32, psum=True)

    def mm(out, lhsT, rhs, start, stop, reads, writes, inc=True, skip=False):
        if skip:
            S.op("pe", lambda e: e.matmul(out, lhsT=lhsT, rhs=rhs, start=start, stop=stop, skip_group_check=True),
                 reads, writes, inc)
        else:
            S.op("pe", lambda e: e.matmul(out, lhsT=lhsT, rhs=rhs, start=start, stop=stop), reads, writes, inc)

    def tr(out, in_, ident, reads, writes, inc=True):
        S.op("pe", lambda e: e.transpose(out, in_, ident), reads, writes, inc)

    def act(out, in_, func, reads, writes, bias=None, scale=None, accum_out=None, ww_ok=False):
        kw = {}
        if bias is not None:
            kw["bias"] = bias
        if scale is not None:
            kw["scale"] = scale
        if accum_out is not None:
            kw["accum_out"] = accum_out
        S.op("act", lambda e: e.activation(out=out, in_=in_, func=func, **kw), reads, writes, ww_ok=ww_ok)

    def tt(eng, out, in0, in1, op, reads, writes):
        S.op(eng, lambda e: e.tensor_tensor(out=out, in0=in0, in1=in1, op=op), reads, writes)

    def ts(eng, out, in0, s1, s2, op0, op1, reads, writes):
        if s2 is None:
            S.op(eng, lambda e: e.tensor_scalar(out=out, in0=in0, scalar1=s1, scalar2=None, op0=op0), reads, writes)
        else:
            S.op(eng, lambda e: e.tensor_scalar(out=out, in0=in0, scalar1=s1, scalar2=s2, op0=op0, op1=op1),
                 reads, writes)

    def stt(eng, out, in0, scalar, in1, op0, op1, reads, writes):
        S.op(eng, lambda e: e.scalar_tensor_tensor(out=out, in0=in0, scalar=scalar, in1=in1, op0=op0, op1=op1),
             reads, writes)

    def cp(eng, out, in_, reads, writes):
        if eng == "act":
            S.op(eng, lambda e: e.activation(out=out, in_=in_, func=AF.Identity), reads, writes)
        else:
            S.op(eng, lambda e: e.tensor_copy(out=out, in_=in_), reads, writes)

    def red(eng, out, in_, op, reads, writes):
        S.op(eng, lambda e: e.tensor_reduce(out=out, in_=in_, axis=AX.X, op=op), reads, writes)

    def rsqrt_mean(dst, src, n, keys):
        act(dst, src, AF.Ln, list(keys) + [cvals.k], keys, bias=cvals[:, 1:2], scale=1.0 / n)
        act(dst, dst, AF.Exp, keys, keys, scale=-0.5)

    def ld(out, in_, tile, reads=(), **kw):
        return S.dma("sp", out, in_, owner=tile.k, reads=reads, writes=[tile.k], **kw)

    def cast_copy(dst, src, key, rows, cols, cchunk):
        for r0 in range(0, rows, 128):
            for c0 in range(0, cols, cchunk):
                c1 = min(cols, c0 + cchunk)
                if chain_casts and key.lw is not None:
                    S.wait_all("pool", [key.lw])
                S.dma("pool", dst[r0:r0 + 128, c0:c1], src[r0:r0 + 128, c0:c1], owner=key, writes=[key])

    cast_copy(win_s, win_d, k_win_s, D, 3088, 3088)
    cast_copy(wout_s, wout_d, k_wout_s, D, D, D)
    cast_copy(wq_s, wq_d, k_wq_s, D, 2048, 2048)
    if do_peer:
        cast_copy(uT_s, uT_d, k_uT_s, D, NEXP, 4096)
        cast_copy(v_s, v_d, k_v_s, NEXP, D, D)

    ld(cst[:, :], cst_d, cst)
    ld(cT[:, :, :], cT_d.rearrange("(k p) b -> p k b", p=128), cT, allow_slow_non_contiguous=True)
    ld(posi[:, :], pos_d, posi)
    ld(badaT[:, :], badaT_d, badaT)
    ld(g1T[:, :], g1T_d, g1T)
    ld(g2T[:, :], g2T_d, g2T)
    ld(bqT[:, :], bqT_d, bqT)
    ld(v64[:, :], vec64_d[0:1, :].to_broadcast([128, 384]), v64)
    ld(v128[:, :], vec128_d[0:1, :].to_broadcast([128, 256]), v128)
    ld(wg2[:, :], wg2_d, wg2)
    ld(bgr[:, :], bg_d, bgr)
    S.op("dve", lambda e: e.memset(onesr[:, :], 1.0), (), [onesr.k])
    S.op("dve", lambda e: e.memset(cvals[:, 0:1], -math.pi), (), [cvals.k])
    S.op("dve", lambda e: e.memset(cvals[:, 1:2], EPS), (), [cvals.k])
    S.op("dve", lambda e: e.memset(cvals[:, 2:3], 1.0), (), [cvals.k])
    S.op("dve", lambda e: e.memset(cvals[:, 3:4], 0.0), (), [cvals.k])
    cp("dve", identb[:, :], C("ident"), [cst.k], [identb.k])
    cp("dve", causb[:, :], C("causal"), [cst.k], [causb.k])
    cp("dve", posf[:, :], posi[:, :], [posi.k], [posf.k])
    ld(kst[:, :], k1T_d, kst)
    cp("dve", k1T[:, :], kst[:, :], [kst.k], [k1T.k])
    ld(kst[:, :], k2T_d, kst)
    cp("dve", k2T[:, :], kst[:, :], [kst.k], [k2T.k])
    ts("dve", qgB[:, :], v64[:, 0:64], 0.125, None, ALU.mult, None, [v64.k], [qgB.k])
    ts("dve", subgB[:, :], v128[:, 0:128], 0.8, None, ALU.mult, None, [v128.k], [subgB.k])
    tt("dve", lamt[:, :], v64[:, 128:192], v64[:, 192:256], ALU.mult, [v64.k], [lamt.k])
    red("dve", lams[:, 0:1], lamt[:, :], ALU.add, [lamt.k], [lams.k])
    tt("dve", lamt[:, :], v64[:, 256:320], v64[:, 320:384], ALU.mult, [v64.k, lams.k], [lamt.k])
    red("dve", lams[:, 1:2], lamt[:, :], ALU.add, [lamt.k], [lams.k])
    act(lams[:, 2:4], lams[:, 0:2], AF.Exp, [lams.k], [lams.k])
    tt("dve", lams[:, 0:1], lams[:, 3:4], lams[:, 2:3], ALU.subtract, [lams.k], [lams.k])
    ts("dve", neglam[:, :], lams[:, 0:1], -0.2, None, ALU.add, None, [lams.k], [neglam.k])
    for i in range(NT):
        S.op("pool", lambda e, i=i: e.memset(VC[i][:, :, :], 1.0), (), [VC[i].k])

    def adaln(b):
        act(sT[:, :], cT[:, :, b], AF.Silu, [cT.k], [sT.k])
        ld(gt1B[:, :], bada_d[0:1, 2 * D:3 * D].to_broadcast([128, D]), gt1B)
        ld(gt2B[:, :], bada_d[0:1, 5 * D:6 * D].to_broadcast([128, D]), gt2B)
        wi = 0
        for sec in range(6):
            for q8 in range(8):
                w = wst[wi % 2]
                wi += 1
                c0 = sec * D + q8 * 128
                ld(w[:, :, :], wada_d[:, c0:c0 + 128].rearrange("(k p) n -> p k n", p=128), w)
                ps = psP[wi % 2]
                if sec in (2, 5):
                    for kc in range(8):
                        mm(ps[:, 0:128], sT[:, kc:kc + 1].to_broadcast([128, 128]), w[:, kc, :],
                           kc == 0, kc == 7, [sT.k, w.k], [ps.k], inc=(kc == 7))
                    g = gt1B if sec == 2 else gt2B
                    tt("dve", g[:, q8 * 128:(q8 + 1) * 128], g[:, q8 * 128:(q8 + 1) * 128], ps[:, 0:128], ALU.add,
                       [ps.k, g.k], [g.k])
                else:
                    mi = {0: 0, 1: 1, 3: 2, 4: 3}[sec]
                    for kc in range(8):
                        mm(ps[:, 0:1], w[:, kc, :], sT[:, kc:kc + 1],
                           kc == 0, kc == 7, [sT.k, w.k], [ps.k], inc=(kc == 7))
                    tt("dve", modT[:, mi, q8:q8 + 1], ps[:, 0:1],
                       badaT[:, sec * 8 + q8:sec * 8 + q8 + 1], ALU.add, [ps.k, badaT.k], [modT.k])
        stt("dve", gm1[:, :], modT[:, 1, :], 1.0, g1T[:, :], ALU.add, ALU.mult, [modT.k, g1T.k], [gm1.k])
        stt("dve", gm2[:, :], modT[:, 3, :], 1.0, g2T[:, :], ALU.add, ALU.mult, [modT.k, g2T.k], [gm2.k])

    def norm_T(xap, xkey, gm, shi, dst, dcol):
        act(junk[:, :], xap, AF.Square, [xkey], [junk.k, st1.k], accum_out=st1[:, 0:1])
        rsqrt_mean(st1[:, 2:3], st1[:, 0:1], D, [st1.k])
        ts("dve", xn[:, :], xap, st1[:, 2:3], None, ALU.mult, None, [xkey, st1.k], [xn.k])
        for j in range(8):
            tr(psT[:, j * 128:(j + 1) * 128], xn[:, j * 128:(j + 1) * 128], identb[:, :],
               [xn.k, identb.k], [psT.k], inc=(j == 7))
        for j in range(8):
            act(dst[:, j, dcol:dcol + 128], psT[:, j * 128:(j + 1) * 128], AF.Identity,
                [psT.k, gm.k, modT.k], [dst.k], bias=modT[:, shi, j:j + 1], scale=gm[:, j:j + 1])

    def rope_tables(col):
        C1 = 6.28125
        C2 = 2.0 * math.pi - C1
        ts("dve", ang[:, :], C("invf"), posf[:, col:col + 1], None, ALU.mult, None, [cst.k, posf.k], [ang.k])
        for (shift, dst) in ((0.0, sinT), (0.5 * math.pi, cosT)):
            ts("dve", ang2[:, :], ang[:, :], shift, 1.0 / (2.0 * math.pi), ALU.add, ALU.mult, [ang.k], [ang2.k])
            cp("dve", angi[:, :], ang2[:, :], [ang2.k], [angi.k])
            cp("dve", ang2[:, :], angi[:, :], [angi.k], [ang2.k])
            stt("dve", ang3[:, :], ang2[:, :], -C1, ang[:, :], ALU.mult, ALU.add, [ang2.k, ang.k], [ang3.k])
            stt("dve", ang3[:, :], ang2[:, :], -C2, ang3[:, :], ALU.mult, ALU.add, [ang2.k, ang3.k], [ang3.k])
            ts("dve", ang3[:, :], ang3[:, :], shift, math.pi, ALU.add, ALU.min, [ang3.k], [ang3.k])
            ts("dve", ang3[:, :], ang3[:, :], -math.pi, None, ALU.max, None, [ang3.k], [ang3.k])
            act(dst[:, :], ang3[:, :], AF.Sin, [ang3.k], [dst.k])

    def proj_block(c0, ncols, ps, lhs=None):
        r = ring_next()
        ld(r[:, :, 0:ncols], win_s[:, c0:c0 + ncols].rearrange("(k p) n -> p k n", p=128), r, reads=[k_win_s])
        for kc in range(8):
            mm(ps[:, 0:ncols], hT[:, kc, :], r[:, kc, 0:ncols], kc == 0, kc == 7,
               [hT.k, r.k], [ps.k], inc=(kc == 7))
        return r

    def qk_post(ps, gB, dstT, dkey):
        act(sq[:, :], ps[:, :], AF.Square, [ps.k], [sq.k])
        red("dve", ss8[:, :], sq[:, :].rearrange("p (g d) -> p g d", d=64), ALU.add, [sq.k], [ss8.k])
        rsqrt_mean(ss8[:, :], ss8[:, :], 64, [ss8.k])
        q3 = qn[:, :].rearrange("p (g d) -> p g d", d=64)
        tt("dve", q3, ps[:, :].rearrange("p (g d) -> p g d", d=64),
           ss8[:, :].unsqueeze(2).to_broadcast([128, 8, 64]), ALU.mult, [ps.k, ss8.k], [qn.k])
        tt("dve", q3, q3, gB.unsqueeze(1).to_broadcast([128, 8, 64]), ALU.mult, [qn.k, v64.k, qgB.k], [qn.k])
        x1v = q3[:, :, 0:32]
        x2v = q3[:, :, 32:64]
        cb = cosT[:, :].unsqueeze(1).to_broadcast([128, 8, 32])
        sbb = sinT[:, :].unsqueeze(1).to_broadcast([128, 8, 32])
        r0, r1 = (t[:, :].rearrange("p (g d) -> p g d", d=32) for t in rt)
        qr3 = qr[:, :].rearrange("p (g d) -> p g d", d=64)
        tt("dve", r0, x1v, cb, ALU.mult, [qn.k, cosT.k], [rt[0].k])
        tt("dve", r1, x2v, sbb, ALU.mult, [qn.k, sinT.k], [rt[1].k])
        tt("dve", qr3[:, :, 0:32], r0, r1, ALU.subtract, [rt[0].k, rt[1].k], [qr.k])
        tt("dve", r0, x2v, cb, ALU.mult, [qn.k, cosT.k], [rt[0].k])
        tt("dve", r1, x1v, sbb, ALU.mult, [qn.k, sinT.k], [rt[1].k])
        tt("dve", qr3[:, :, 32:64], r0, r1, ALU.add, [rt[0].k, rt[1].k], [qr.k])
        for h in range(4):
            tr(psT[:, h * 128:(h + 1) * 128], qr[:, h * 128:(h + 1) * 128], identb[:, :],
               [qr.k, identb.k], [psT.k], inc=(h == 3))
        cp("dve", dstT[:, :, :], psT[:, 0:512].rearrange("p (h t) -> p h t", t=128),
           [psT.k], [dkey])

    dbg_toks = []

    def dump(name, ap, key):
        if name in dbg_d:
            dbg_toks.append(S.dma("sp", dbg_d[name], ap, owner=key, reads=[key]))

    def mixer_tile(b, i, slot):
        col = b * NT + i
        tok0 = b * SEQ + i * 128
        xs = x1[:, slot, :]
        xkey = xk[slot]
        S.dma("sp", xs, x_d[tok0:tok0 + 128, :], owner=xkey, writes=[xkey])
        if slevel < 2:
            return
        norm_T(xs, xkey, gm1, 0, hT, 0)
        rope_tables(col)
        if slevel < 3:
            return
        proj_block(0, 512, psP[0])
        qk_post(psP[0], qgB[:, :], qT, qT.k)
        proj_block(512, 512, psP[1])
        qk_post(psP[1], v64[:, 64:128], KT[i], KT[i].k)
        proj_block(1024, 512, psP[0])
        cp("act", VC[i][:, :, 0:128], psP[0][:, :].rearrange("p (h e) -> p h e", e=128), [psP[0].k], [VC[i].k])
        if slevel < 4:
            return
        proj_block(1536, 512, psP[1])
        cp("act", gqk[:, :], psP[1][:, :], [psP[1].k], [gqk.k])
        proj_block(2048, 512, psP[0])
        cp("act", gv[:, :], psP[0][:, :], [psP[0].k], [gv.k])
        proj_block(2560, 512, psP[1])
        act(sr[:, :], psP[1][:, :], AF.Silu, [psP[1].k], [sr.k])
        r = ring_next()
        ld(r[:, :, 0:16], win_s[:, 3072:3088].rearrange("(k p) n -> p k n", p=128), r, reads=[k_win_s])
        for kc in range(8):
            mm(psP[0][0:16, 0:128], r[:, kc, 0:16], hT[:, kc, :], kc == 0, kc == 7,
               [hT.k, r.k], [psP[0].k], inc=(kc == 7))
        cp("act", ggT[:, :], psP[0][0:16, 0:128], [psP[0].k], [ggT.k])

        if slevel < 5:
            return
        nkb = i + 1
        ngrp = (nkb + 3) // 4
        sidx = 0
        for h in range(4):
            for m in range(2):
                pr = slice(m * 64, (m + 1) * 64)
                for g in range(ngrp):
                    j0 = g * 4
                    nj = min(4, nkb - j0)
                    pss = psS[sidx % 2]
                    pt = PT[sidx % 2]
                    sidx += 1
                    for jj in range(nj):
                        j = j0 + jj
                        mm(pss[:, jj * 128:(jj + 1) * 128], KT[j][pr, h, :], qT[pr, h, :], True, True,
                           [KT[j].k, qT.k], [pss.k], inc=(jj == nj - 1))
                    act(pt[:, 0:nj * 128], pss[:, 0:nj * 128], AF.Exp, [pss.k], [pt.k])
                    if j0 + nj - 1 == i:
                        dsl = slice((nj - 1) * 128, nj * 128)
                        tt("pool", pt[:, dsl], pt[:, dsl], causb[:, :], ALU.mult, [pt.k, causb.k], [pt.k])
                    for jj in range(nj):
                        j = j0 + jj
                        mm(psO[:, m * 129:(m + 1) * 129], pt[:, jj * 128:(jj + 1) * 128], VC[j][:, h, 0:129],
                           j == 0, j == i, [pt.k, VC[j].k], [psO.k], inc=(jj == nj - 1))
            S.op("dve", lambda e: e.reciprocal(out=rz[:, 0:1], in_=psO[:, 128:129]), [psO.k], [rz.k])
            S.op("dve", lambda e: e.reciprocal(out=rz[:, 1:2], in_=psO[:, 257:258]), [psO.k], [rz.k])
            tt("dve", rz[:, 2:3], rz[:, 1:2], neglam[:, :], ALU.mult, [rz.k, neglam.k], [rz.k])
            ts("dve", osb[:, :], psO[:, 0:128], rz[:, 0:1], None, ALU.mult, None, [psO.k, rz.k], [osb.k])
            stt("dve", osb[:, :], psO[:, 129:257], rz[:, 2:3], osb[:, :], ALU.mult, ALU.add,
                [psO.k, rz.k, osb.k], [osb.k])
            act(junk[:, 0:128], osb[:, :], AF.Square, [osb.k], [junk.k, rz.k], accum_out=rz[:, 3:4])
            rsqrt_mean(rz[:, 3:4], rz[:, 3:4], 128, [rz.k])
            stt("dve", ybf[:, h * 128:(h + 1) * 128], osb[:, :], rz[:, 3:4], subgB[:, :], ALU.mult, ALU.mult,
                [osb.k, rz.k, subgB.k], [ybf.k])

        if slevel < 6:
            return
        mm(psG[:, 0:256], ggT[:, :], wg2[:, :], True, False, [ggT.k, wg2.k], [psG.k], inc=False)
        mm(psG[:, 0:256], onesr[:, :], bgr[:, :], False, True, [onesr.k, bgr.k], [psG.k])
        act(la[:, :], psG[:, 0:256], AF.Exp, [psG.k], [la.k], scale=-1.0)
        act(la[:, :], la[:, :], AF.Ln, [la.k, cvals.k], [la.k], bias=cvals[:, 2:3])
        ts("dve", la[:, :], la[:, :], -1.0 / 16, None, ALU.mult, None, [la.k], [la.k])
        if GSUB < 1:
            return
        mm(psG[:, 0:256], C("mcum"), la[:, :], True, True, [cst.k, la.k], [psG.k], inc=False)
        mm(psG[:, 256:512], C("mmid"), la[:, :], True, True, [cst.k, la.k], [psG.k])
        mm(psG2[:, 0:256], C("mblk"), la[:, :], True, True, [cst.k, la.k], [psG2.k], inc=False)
        for hp in range(2):
            for hh in range(2):
                h = hp * 2 + hh
                mm(psG2[hh * 64:(hh + 1) * 64, 256 + hp * 2:256 + hp * 2 + 2], la[:, h * 64:(h + 1) * 64],
                   C("chunkind"), True, True, [la.k, cst.k], [psG2.k], inc=(hp == 1 and hh == 1))
        if GSUB < 2:
            return
        bc, d1, eq, ek, eb, d2, ed, qg = gl
        cp("dve", bc[:, :], psG[:, 0:256], [psG.k], [bc.k])
        tt("dve", d1[:, :], bc[:, :], psG[:, 256:512], ALU.subtract, [bc.k, psG.k], [d1.k])
        tt("dve", d2[:, :], psG2[:, 0:256], bc[:, :], ALU.subtract, [bc.k, psG2.k], [d2.k])
        act(dec[:, :, :], psG2[:, 256:260].rearrange("p (a c) -> p a c", c=2), AF.Exp, [psG2.k], [dec.k])
        act(eq[:, :], d1[:, :], AF.Exp, [d1.k], [eq.k])
        act(ek[:, :], d1[:, :], AF.Exp, [d1.k], [ek.k], scale=-1.0)
        act(eb[:, :], bc[:, :], AF.Exp, [bc.k], [eb.k])
        act(ed[:, :], d2[:, :], AF.Exp, [d2.k], [ed.k])
        stt("dve", eq[:, :], gqk[:, 0:256], 0.125, eq[:, :], ALU.mult, ALU.mult, [gqk.k, eq.k], [eq.k])
        tt("dve", ek[:, :], gqk[:, 256:512], ek[:, :], ALU.mult, [gqk.k, ek.k], [ek.k])
        stt("dve", eb[:, :], gqk[:, 0:256], 0.125, eb[:, :], ALU.mult, ALU.mult, [gqk.k, eb.k], [eb.k])
        tt("dve", ed[:, :], gqk[:, 256:512], ed[:, :], ALU.mult, [gqk.k, ed.k], [ed.k])
        ci = C("chunkind")
        ts("dve", d1[:, :], ed[:, :], ci[:, 0:1], None, ALU.mult, None, [ed.k, cst.k, d1.k], [d1.k])
        ts("dve", d2[:, :], ed[:, :], ci[:, 1:2], None, ALU.mult, None, [ed.k, cst.k, d2.k], [d2.k])
        if GSUB < 3:
            return
        for hp in range(2):
            cs = slice(hp * 128, (hp + 1) * 128)
            mm(psG[:, 0:128], eq[:, cs], C("ident"), True, True, [eq.k, cst.k], [psG.k], inc=False)
            mm(psG[:, 128:256], ek[:, cs], C("ident"), True, True, [ek.k, cst.k], [psG.k], inc=False)
            mm(psG[:, 256:384], eb[:, cs], C("ident"), True, True, [eb.k, cst.k], [psG.k])
            cp("act", T3[:, :, :], psG[:, 0:384].rearrange("p (a t) -> p a t", t=128), [psG.k], [T3.k])
            if GSUB < 4:
                continue
            st = Sst[hp]
            if i == 0:
                S.op("dve", lambda e, st=st: e.memset(st[:, :], 0.0), (), [st.k])
            for hh in range(2):
                h = hp * 2 + hh
                pr = slice(hh * 64, (hh + 1) * 64)
                vcols = slice(h * 128, (h + 1) * 128)
                for c, kdm in enumerate((d1, d2)):
                    mm(psG2[pr, c * 128:(c + 1) * 128], kdm[:, h * 64:(h + 1) * 64], gv[:, vcols], True, True,
                       [kdm.k, gv.k], [psG2.k], inc=False)
                mm(psG2[:, 256:384], T3[pr, 1, :], T3[pr, 0, :], True, True, [T3.k], [psG2.k])
                tt("dve", ATs[:, :], psG2[:, 256:384], C("mcum"), ALU.mult, [psG2.k, cst.k], [ATs.k])
                if GSUB < 5:
                    continue
                mm(psO[:, 0:128], ATs[:, :], gv[:, vcols], True, False, [ATs.k, gv.k], [psO.k], inc=False, skip=True)
                mm(psO[0:64, 0:128], T3[pr, 2, 0:64], st[pr, :], False, False, [T3.k, st.k], [psO.k], inc=True,
                   skip=True)
                if GSUB < 6:
                    continue
                stt("dve", st[pr, :], st[pr, :], dec[pr, hp, 0:1], psG2[pr, 0:128], ALU.mult, ALU.add,
                    [st.k, dec.k, psG2.k], [st.k])
                mm(psO[64:128, 0:128], T3[pr, 2, 64:128], st[pr, :], False, True, [T3.k, st.k], [psO.k], skip=True)
                if GSUB < 7:
                    continue
                stt("dve", st[pr, :], st[pr, :], dec[pr, hp, 1:2], psG2[pr, 128:256], ALU.mult, ALU.add,
                    [st.k, dec.k, psG2.k], [st.k])
                if GSUB < 8:
                    continue
                act(junk[:, 0:128], psO[:, 0:128], AF.Square, [psO.k], [junk.k, rz.k], accum_out=rz[:, 3:4])
                rsqrt_mean(rz[:, 3:4], rz[:, 3:4], 128, [rz.k])
                stt("dve", osb[:, :], psO[:, 0:128], rz[:, 3:4], v128[:, 128:256], ALU.mult, ALU.mult,
                    [psO.k, rz.k, v128.k], [osb.k])
                tt("dve", ybf[:, 512 + h * 128:512 + (h + 1) * 128], osb[:, :], sr[:, vcols], ALU.mult,
                   [osb.k, sr.k], [ybf.k])

        if slevel < 7:
            return
        for j in range(8):
            tr(psT[:, j * 128:(j + 1) * 128], ybf[:, j * 128:(j + 1) * 128], identb[:, :],
               [ybf.k, identb.k], [psT.k], inc=(j == 7))
        cp("act", yT[:, :, :], psT[:, :].rearrange("p (j t) -> p j t", t=128), [psT.k], [yT.k])
        for half in range(2):
            r = ring_next()
            ld(r[:, :, :], wout_s[:, half * 512:(half + 1) * 512].rearrange("(k p) n -> p k n", p=128), r, reads=[k_wout_s])
            ps = psP[half]
            for kc in range(8):
                mm(ps[:, :], yT[:, kc, :], r[:, kc, :], kc == 0, kc == 7, [yT.k, r.k], [ps.k],
                   inc=(kc == 7))
            cs = slice(half * 512, (half + 1) * 512)
            tt("dve", sq[:, :], ps[:, :], gt1B[:, cs], ALU.mult, [ps.k, gt1B.k], [sq.k])
            tt("dve", x1[:, slot, cs], sq[:, :], x1[:, slot, cs], ALU.add, [sq.k, xkey], [xkey])
        if do_peer:
            norm_T(xs, xkey, gm2, 2, h2T, slot * 128)

    if do_peer:
        qpc = [sb("qpc%d" % i, [128, ST * 128], BF16) for i in range(2)]
        s1m = sb("s1m", [128, ST, 8, 128], F32)
        s2m = sb("s2m", [128, ST, 8, 128], F32)
        wk = sb("wk", [128, 256], F32)
        v1 = sb("v1", [128, 8, 16], F32)
        v2 = sb("v2", [128, 8, 16], F32)
        c24 = sb("c24", [128, 8, 24], F32)
        thr = sb("thr", [128, 8], F32)
        nrm = sb("nrm", [128, 8], F32)
        ex16 = sb("ex16", [128, 8, 16], F32)
        DG = [sb("DG%d" % t, [128, 8, 128], BF16) for t in range(ST)]
        zbs = [sb("zb%d" % i, [128, 8, 2, 128], F32) for i in range(2)]
        zb = zbs[0]
        zi = [0]
        cand = zb[:, :, :, :].rearrange("p h a j -> p h (a j)")
        wbs = [sb("wb%d" % i, [128, 8, 2, 128], BF16) for i in range(2)]
        gas = [sb("ga%d" % i, [128, 2, ST * 128], F32) for i in range(2)]
        GTs = [sb("GT%d" % i, [128, 2, 128], BF16) for i in range(2)]
        oacc = sb("oacc", [128, ST, D], F32)
        ok = [Key("oacc_%d" % i) for i in range(ST)]

    out_toks = []

    def top16(src_ap, src_keys, dst, h):
        n = src_ap.shape[-1]
        S.op("dve", lambda e: e.max(out=dst[:, h, 0:8], in_=src_ap), src_keys, [dst.k])
        S.op("dve", lambda e: e.match_replace(out=wk[:, 0:n], in_to_replace=dst[:, h, 0:8], in_values=src_ap,
                                              imm_value=-BIG), list(src_keys) + [dst.k], [wk.k])
        S.op("dve", lambda e: e.max(out=dst[:, h, 8:16], in_=wk[:, 0:n]), [wk.k], [dst.k])

    def peer_supertile(b, st_i):
        T2 = ST * 128
        for blk in range(4):
            r = ring_next()
            ld(r[:, :, :], wq_s[:, blk * 512:(blk + 1) * 512].rearrange("(k p) n -> p k n", p=128), r, reads=[k_wq_s])
            for c4 in range(4):
                cc = blk * 4 + c4
                h, half = cc // 2, cc % 2
                ps = psP[cc % 2]
                qc = qpc[cc % 2]
                for kc in range(8):
                    mm(ps[:, 0:T2], r[:, kc, c4 * 128:(c4 + 1) * 128], h2T[:, kc, :], kc == 0, kc == 7,
                       [r.k, h2T.k], [ps.k], inc=(kc == 7))
                act(qc[:, :], ps[:, 0:T2], AF.Identity, [ps.k, bqT.k], [qc.k], bias=bqT[:, cc:cc + 1])
                pss = psS[cc % 2]
                kT = k1T if half == 0 else k2T
                dstm = s1m if half == 0 else s2m
                for t in range(ST):
                    mm(pss[:, t * 128:(t + 1) * 128], qc[:, t * 128:(t + 1) * 128], kT[:, :], True, True,
                       [qc.k, kT.k], [pss.k], inc=(t == ST - 1))
                cp("dve", dstm[:, :, h, :], pss[:, 0:T2].rearrange("p (t k) -> p t k", k=128), [pss.k], [dstm.k])
        for t in range(ST):
            for h in range(8):
                top16(s1m[:, t, h, :], [s1m.k], v1, h)
                top16(s2m[:, t, h, :], [s2m.k], v2, h)
            tt("dve", cand.rearrange("p h (a b) -> p h a b", b=16),
               v1[:, :, :].unsqueeze(3).to_broadcast([128, 8, 16, 16]),
               v2[:, :, :].unsqueeze(2).to_broadcast([128, 8, 16, 16]), ALU.add, [v1.k, v2.k], [zb.k])
            for h in range(8):
                top16(cand[:, h, :], [zb.k], c24, h)
                S.op("dve", lambda e, h=h: e.match_replace(out=wk[:, :], in_to_replace=c24[:, h, 8:16],
                                                           in_values=wk[:, :], imm_value=-BIG),
                     [wk.k, c24.k], [wk.k])
                S.op("dve", lambda e, h=h: e.max(out=c24[:, h, 16:24], in_=wk[:, :]), [wk.k], [c24.k])
            tt("dve", thr[:, :], c24[:, :, 15], c24[:, :, 16], ALU.add, [c24.k], [thr.k])
            ts("dve", thr[:, :], thr[:, :], 0.5, None, ALU.mult, None, [thr.k], [thr.k])
            tt("dve", ex16[:, :, :], c24[:, :, 0:16], thr[:, :].unsqueeze(2).to_broadcast([128, 8, 16]),
               ALU.subtract, [c24.k, thr.k], [ex16.k])
            act(ex16[:, :, :], ex16[:, :, :], AF.Exp, [ex16.k], [ex16.k])
            red("dve", nrm[:, :], ex16[:, :, :], ALU.add, [ex16.k], [nrm.k])
            S.op("dve", lambda e: e.reciprocal(out=nrm[:, :], in_=nrm[:, :]), [nrm.k], [nrm.k])
            for h in range(8):
                ts("dve", DG[t][:, h, :], C("ident"), nrm[:, h:h + 1], None, ALU.mult, None, [cst.k, nrm.k],
                   [DG[t].k])
            for (sm, vv, sub_thr) in ((s1m, v1, True), (s2m, v2, False)):
                mskt = zbs[1][:, :, 0, :]
                tt("dve", mskt, sm[:, t, :, :], vv[:, :, 15:16].to_broadcast([128, 8, 128]), ALU.is_lt,
                   [sm.k, vv.k], [zbs[1].k])
                stt("dve", sm[:, t, :, :], mskt, -BIG, sm[:, t, :, :], ALU.mult, ALU.add,
                    [zbs[1].k, sm.k], [sm.k])
                if sub_thr:
                    tt("dve", sm[:, t, :, :], sm[:, t, :, :], thr[:, :].unsqueeze(2).to_broadcast([128, 8, 128]),
                       ALU.subtract, [sm.k, thr.k], [sm.k])
        psWs = (psS[0], psP[0])
        psA = psS[1]
        psV = ((psO, psG), (psG2, psP[1]))
        NG = NEXP // 512
        slots = {}

        def load_group(g):
            ru = ring_next()
            ld(ru[:, :, :], uT_s[:, g * 512:(g + 1) * 512].rearrange("(k p) n -> p k n", p=128), ru, reads=[k_uT_s])
            rv = ring_next()
            rv4 = rv[:, :, :].rearrange("p (c a) n -> p c (a n)", a=2)
            ld(rv4, v_s[g * 512:(g + 1) * 512, :].rearrange("(c p) d -> p c d", p=128), rv, reads=[k_v_s])
            slots[g] = (ru, rv, rv4)

        def stage_A(g, sub):
            ru = slots[g][0]
            gt_ = gas[(g * 2 + sub) % 2]
            for ii in range(2):
                ec = sub * 2 + ii
                for kc in range(8):
                    mm(psA[:, ii * T2:(ii + 1) * T2], ru[:, kc, ec * 128:(ec + 1) * 128], h2T[:, kc, :],
                       kc == 0, kc == 7, [ru.k, h2T.k], [psA.k], inc=(kc == 7))
            act(gt_[:, :, :], psA[:, 0:2 * T2].rearrange("p (a t) -> p a t", t=T2), AF.Gelu, [psA.k], [gt_.k])

        its = [(g, sub, t) for g in range(NG) for sub in range(2) for t in range(ST)]

        def stage_PM(k):
            g, sub, t = its[k]
            i0 = g * 4 + sub * 2
            zt = zbs[k % 2]
            wt = wbs[k % 2]
            for h in range(8):
                for ii in range(2):
                    act(zt[:, h, ii, :], s2m[:, t, h, :], AF.Exp, [s1m.k, s2m.k], [zt.k],
                        bias=s1m[:, t, h, i0 + ii:i0 + ii + 1], ww_ok=True)
            stt("dve", wt[:, :, :, :], zt[:, :, :, :], 1.0, zt[:, :, :, :], ALU.is_ge, ALU.mult,
                [zt.k], [wt.k])

        def stage_W(k):
            g, sub, t = its[k]
            wt = wbs[k % 2]
            pw = psWs[k % 2]
            first = True
            for h in range(8):
                for ii in range(2):
                    mm(pw[:, ii * 128:(ii + 1) * 128], wt[:, h, ii, :], DG[t][:, h, :], first, (h == 7),
                       [wt.k, DG[t].k], [pw.k], inc=(h == 7 and ii == 1), skip=True)
                    first = False

        def stage_G(k):
            g, sub, t = its[k]
            pw = psWs[k % 2]
            gt_ = gas[(g * 2 + sub) % 2]
            tt("dve", GTs[k % 2][:, :, :], gt_[:, :, t * 128:(t + 1) * 128],
               pw[:, 0:256].rearrange("p (a t) -> p a t", t=128), ALU.mult, [gt_.k, pw.k], [GTs[k % 2].k])

        def stage_V(k):
            g, sub, t = its[k]
            rv, rv4 = slots[g][1], slots[g][2]
            for ii in range(2):
                ec = sub * 2 + ii
                for half in range(2):
                    pv = psV[t][half]
                    mm(pv[:, :], GTs[k % 2][:, ii, :], rv4[:, ec, half * 512:(half + 1) * 512],
                       (g == 0 and sub == 0 and ii == 0), (g == NG - 1 and sub == 1 and ii == 1),
                       [GTs[k % 2].k, rv.k], [pv.k], inc=(ii == 1 and half == 1))

        load_group(0)
        stage_A(0, 0)
        for k in range(len(its) + 1):
            if k < len(its):
                stage_PM(k)
                stage_W(k)
            if k >= 1:
                stage_G(k - 1)
                stage_V(k - 1)
            if k < len(its):
                g, sub, t = its[k]
                if t == 0:
                    ng, nsub = (g, 1) if sub == 0 else (g + 1, 0)
                    if ng < NG:
                        if nsub == 0:
                            load_group(ng)
                        stage_A(ng, nsub)
        for t in range(ST):
            for half in range(2):
                cs = slice(half * 512, (half + 1) * 512)
                tt("dve", oacc[:, t, cs], psV[t][half][:, :], gt2B[:, cs], ALU.mult, [psV[t][half].k, gt2B.k], [ok[t]])
            tt("dve", oacc[:, t, :], oacc[:, t, :], x1[:, t, :], ALU.add, [ok[t], xk[t]], [ok[t]])
            tok0 = b * SEQ + (st_i * ST + t) * 128
            out_toks.append(S.dma("sp", y_d[tok0:tok0 + 128, :], oacc[:, t, :], owner=ok[t], reads=[ok[t]]))

    for b in range(NB):
        if slevel >= 1:
            adaln(b)
        for st_i in range(NST):
            for t in range(ST):
                mixer_tile(b, st_i * ST + t, t)
            if do_peer:
                peer_supertile(b, st_i)
            else:
                for t in range(ST):
                    tok0 = b * SEQ + (st_i * ST + t) * 128
                    out_toks.append(S.dma("sp", y_d[tok0:tok0 + 128, :], x1[:, t, :], owner=xk[t], reads=[xk[t]]))
    print("sbuf bytes remaining", nc.sbuf_bytes_remaining)
    S.wait_all("sp", out_toks + dbg_toks)
    S.emit()
    return nc, S


def make_in_maps(inputs, n_cores, NB, SEQ):
    f = lambda a: np.ascontiguousarray(np.asarray(a, dtype=np.float32))
    x = f(inputs["x"])
    c = f(inputs["c"])
    pos = np.asarray(inputs["positions"]).astype(np.int32)
    NT = SEQ // 128
    cblob, _ = _consts()
    shared = {
        "w_ada": f(inputs["w_ada"][0]),
        "b_adaT": f(np.asarray(inputs["b_ada"][0]).reshape(48, 128).T),
        "b_ada": f(np.asarray(inputs["b_ada"][0]).reshape(1, -1)),
        "g1T": f(np.asarray(inputs["norm1_g"][0]).reshape(8, 128).T),
        "g2T": f(np.asarray(inputs["norm2_g"][0]).reshape(8, 128).T),
        "w_in": f(inputs["w_in"][0]),
        "w_out": f(inputs["w_out"][0]),
        "w_query": f(inputs["w_query"][0]),
        "b_queryT": f(np.asarray(inputs["b_query"][0]).reshape(16, 128).T),
        "keys1T": f(np.asarray(inputs["peer_keys1"][0]).T),
        "keys2T": f(np.asarray(inputs["peer_keys2"][0]).T),
        "uT": f(np.asarray(inputs["expert_u"][0]).T),
        "ev": f(inputs["expert_v"][0]),
        "vec64": f(np.concatenate([np.asarray(inputs[k][0]).reshape(-1) for k in
                                   ("qn_g", "kn_g", "lam_q1", "lam_k1", "lam_q2", "lam_k2")]).reshape(1, -1)),
        "vec128": f(np.concatenate([np.asarray(inputs[k][0]).reshape(-1) for k in
                                    ("diff_norm_g", "gla_norm_g")]).reshape(1, -1)),
        "w_gate2": f(inputs["w_gate2"][0]),
        "b_gate": f(np.asarray(inputs["b_gate"][0]).reshape(1, -1)),
        "consts": cblob,
    }
    maps = []
    for i in range(n_cores):
        bs = slice(i * NB, (i + 1) * NB)
        m = dict(shared)
        m["x"] = np.ascontiguousarray(x[bs].reshape(NB * SEQ, D))
        m["cT"] = np.ascontiguousarray(c[bs].T)
        m["posT"] = np.ascontiguousarray(pos[bs].reshape(NB * NT, 128).T)
        maps.append(m)
    return maps


def kernel(**inputs):
    x = np.asarray(inputs["x"])
    B, SEQ, _ = x.shape
    NB = B // NCORES
    nc, _ = build_program(NB, SEQ, do_peer=True)
    maps = make_in_maps(inputs, NCORES, NB, SEQ)
    res = run_bass_kernel_spmd(nc, maps, core_ids=list(range(NCORES)))
    out = np.concatenate([np.asarray(r["y"]).reshape(NB, SEQ, D) for r in res.results], axis=0)
    return out.astype(np.float32)
```

```python
import math
import os
import threading
import numpy as np
import concourse.bass as bass
import concourse.mybir as mybir
from concourse.bass_utils import run_bass_kernel_spmd

F32 = mybir.dt.float32
BF16 = mybir.dt.bfloat16
I32 = mybir.dt.int32
AF = mybir.ActivationFunctionType
ALU = mybir.AluOpType
AX = mybir.AxisListType

D = 1024
NCORES = 8
EPS = 1e-6
NEXP = 16384
BIG = 1.0e4
GSUB = int(os.environ.get("GSUB", "9"))
INTERLEAVE = int(os.environ.get("INTERLEAVE", "1"))


class Key:
    __slots__ = ("name", "excl", "const", "lw", "rd", "sem", "semcnt")

    def __init__(self, name, excl=False, const=False):
        self.name = name
        self.excl = excl
        self.const = const
        self.lw = None
        self.rd = []
        self.sem = None
        self.semcnt = 0


class Co:
    def __init__(self, fn):
        self.go = threading.Semaphore(0)
        self.back = threading.Semaphore(0)
        self.done = False
        self.budget = 0
        self.err = None
        self.th = threading.Thread(target=self._run, args=(fn,), daemon=True)
        self.th.start()

    def _run(self, fn):
        self.go.acquire()
        try:
            fn()
        except BaseException as e:
            self.err = e
        self.done = True
        self.back.release()

    def hook(self):
        if self.budget <= 0:
            self.back.release()
            self.go.acquire()
        self.budget -= 1

    def step(self, n):
        if self.done:
            return
        self.budget = n
        self.go.release()
        self.back.acquire()
        if self.err is not None:
            raise self.err

    def finish(self):
        while not self.done:
            self.step(1000)
        if self.err is not None:
            raise self.err


class Sched:
    def __init__(self, nc):
        self.co = None
        self.nc = nc
        self.engs = ("pe", "act", "dve", "pool", "sp")
        self.prog = {e: [] for e in self.engs}
        self.cnt = {e: 0 for e in self.engs}
        self.seen = {e: {} for e in self.engs}
        self.esem = {e: nc.alloc_semaphore(name="tl_" + e) for e in self.engs}
        self.n_ins = 0

    def _need(self, eng, tok, waits, raw):
        if tok is None:
            return
        if tok[0] == "e":
            _, f, n = tok
            if f == eng and eng == "pe":
                return
            k = ("e", f)
        else:
            _, key, n = tok
            k = ("d", key)
        if self.seen[eng].get(k, 0) >= n:
            return
        if n > waits.get(k, 0):
            waits[k] = n

    def _deps(self, eng, reads, writes, ww_ok=False):
        waits = {}
        for k in reads:
            self._need(eng, k.lw, waits, True)
        for k in writes:
            if not (ww_ok and k.lw is not None and k.lw[0] == "e" and k.lw[1] == eng):
                self._need(eng, k.lw, waits, False)
            for t in k.rd:
                self._need(eng, t, waits, False)
        for k, n in waits.items():
            self.seen[eng][k] = n
            sem = self.esem[k[1]] if k[0] == "e" else k[1].sem
            self.prog[eng].append(("w", sem, n))

    def _commit(self, tok, reads, writes):
        for k in reads:
            if k.excl:
                k.lw = tok
                k.rd = []
            elif not k.const:
                k.rd.append(tok)
        for k in writes:
            k.lw = tok
            k.rd = []

    def _co_hook(self):
        co = self.co
        if co is None:
            return False
        if threading.current_thread() is co.th:
            co.hook()
            return False
        return True

    def op(self, eng, fn, reads=(), writes=(), inc=True, ww_ok=False):
        main_with_co = self._co_hook()
        tok = self._op(eng, fn, reads, writes, inc, ww_ok)
        if main_with_co:
            self.co.step(1)
        return tok

    def _op(self, eng, fn, reads=(), writes=(), inc=True, ww_ok=False):
        self._deps(eng, reads, writes, ww_ok)
        self.n_ins += 1
        if inc:
            self.cnt[eng] += 1
            tok = ("e", eng, self.cnt[eng])
            self.prog[eng].append(("i", fn, self.esem[eng], 1))
        else:
            tok = ("e", eng, self.cnt[eng] + 1)
            self.prog[eng].append(("i", fn, None, 0))
        self._commit(tok, reads, writes)
        return tok

    def dma(self, eng, out, in_, owner, reads=(), writes=(), **kw):
        self._co_hook()
        self._deps(eng, reads, writes)
        if owner.sem is None:
            owner.sem = self.nc.alloc_semaphore(name="d_" + owner.name)
        owner.semcnt += 16
        tok = ("d", owner, owner.semcnt)
        self.n_ins += 1
        self.prog[eng].append(
            ("i", lambda e, o=out, i=in_, kw=kw: e.dma_start(out=o, in_=i, **kw), owner.sem, 16))
        self._commit(tok, reads, writes)
        return tok

    def wait_all(self, eng, toks):
        waits = {}
        for t in toks:
            self._need(eng, t, waits, True)
        for k, n in waits.items():
            self.seen[eng][k] = n
            sem = self.esem[k[1]] if k[0] == "e" else k[1].sem
            self.prog[eng].append(("w", sem, n))

    def emit(self):
        progs = self.prog

        def run(e, lst):
            for it in lst:
                if it[0] == "w":
                    e.wait_ge(it[1], it[2])
                else:
                    ins = it[1](e)
                    if it[2] is not None:
                        ins.then_inc(it[2], it[3])

        with self.nc.Block() as block:
            @block.tensor
            def _(e):
                run(e, progs["pe"])

            @block.scalar
            def _(e):
                run(e, progs["act"])

            @block.vector
            def _(e):
                run(e, progs["dve"])

            @block.gpsimd
            def _(e):
                run(e, progs["pool"])

            @block.sync
            def _(e):
                run(e, progs["sp"])


class Tl:
    def __init__(self, nc, name, shape, dt, psum=False, const=False):
        if psum:
            self.t = nc.alloc_psum_tensor("p_" + name, shape, dt)
        else:
            self.t = nc.alloc_sbuf_tensor("s_" + name, shape, dt)
        self.k = Key(name, excl=psum, const=const)

    def __getitem__(self, idx):
        return self.t[idx]


def _consts():
    p = np.arange(128)
    same = (p[:, None] // 64) == (p[None, :] // 64)
    c = {}
    c["ident"] = np.eye(128, dtype=np.float32)
    c["causal"] = (p[:, None] <= p[None, :]).astype(np.float32)
    c["mcum"] = (same & (p[:, None] <= p[None, :])).astype(np.float32)
    c["mblk"] = same.astype(np.float32)
    c["mmid"] = (same & ((p[:, None] % 64) <= 32)).astype(np.float32)
    c["chunkind"] = np.stack([(p < 64), (p >= 64)], axis=1).astype(np.float32)
    invf = (10000.0 ** (-np.arange(0, 64, 2, dtype=np.float32) / 64)).astype(np.float32)
    c["invf"] = np.tile(invf[None, :], (128, 1)).astype(np.float32)
    order = ["ident", "causal", "mcum", "mblk", "mmid", "chunkind", "invf"]
    offs = {}
    cols = 0
    for n in order:
        offs[n] = (cols, c[n].shape[1])
        cols += c[n].shape[1]
    blob = np.concatenate([c[n] for n in order], axis=1).astype(np.float32)
    return blob, offs


def build_program(NB, SEQ, do_peer=True, dbg=(), stage="full", chain_casts=True):
    STAGES = ["pro", "ada", "norm", "qkv", "proj", "attn", "gla", "full"]
    slevel = STAGES.index(stage)
    NT = SEQ // 128
    ST = 2
    NST = NT // ST
    NTOK = NB * SEQ
    nc = bass.Bass("TRN2", target_bir_lowering=False)
    S = Sched(nc)
    cblob, coffs = _consts()
    CW = cblob.shape[1]

    def din(name, shape, dt=F32):
        return nc.dram_tensor(name, list(shape), dt, kind="ExternalInput").ap()

    x_d = din("x", [NTOK, D])
    cT_d = din("cT", [D, NB])
    pos_d = din("posT", [128, NB * NT], I32)
    wada_d = din("w_ada", [D, 6 * D])
    badaT_d = din("b_adaT", [128, 48])
    bada_d = din("b_ada", [1, 6 * D])
    g1T_d = din("g1T", [128, 8])
    g2T_d = din("g2T", [128, 8])
    win_d = din("w_in", [D, 3088])
    wout_d = din("w_out", [D, D])
    wq_d = din("w_query", [D, 2048])
    bqT_d = din("b_queryT", [128, 16])
    k1T_d = din("keys1T", [128, 128])
    k2T_d = din("keys2T", [128, 128])
    uT_d = din("uT", [D, NEXP])
    v_d = din("ev", [NEXP, D])
    vec64_d = din("vec64", [1, 6 * 64])
    vec128_d = din("vec128", [1, 2 * 128])
    wg2_d = din("w_gate2", [16, 256])
    bg_d = din("b_gate", [1, 256])
    cst_d = din("consts", [128, CW])
    y_d = nc.dram_tensor("y", [NTOK, D], F32, kind="ExternalOutput").ap()
    dbg_d = {n: nc.dram_tensor("dbg_" + n, list(shp), F32, kind="ExternalOutput").ap() for n, shp in dbg}

    win_s = nc.dram_tensor("win_s", [D, 3088], BF16, kind="Internal").ap()
    wout_s = nc.dram_tensor("wout_s", [D, D], BF16, kind="Internal").ap()
    wq_s = nc.dram_tensor("wq_s", [D, 2048], BF16, kind="Internal").ap()
    uT_s = nc.dram_tensor("uT_s", [D, NEXP], BF16, kind="Internal").ap()
    v_s = nc.dram_tensor("v_s", [NEXP, D], BF16, kind="Internal").ap()
    k_win_s, k_wout_s, k_wq_s, k_uT_s, k_v_s = (Key(n, const=True) for n in ("kwin", "kwout", "kwq", "kuT", "kv"))

    def sb(name, shape, dt=F32, const=False):
        return Tl(nc, name, shape, dt, const=const)

    cst = sb("cst", [128, CW], F32, const=True)
    identb = sb("identb", [128, 128], BF16, const=True)
    causb = sb("causb", [128, 128], BF16, const=True)
    onesr = sb("onesr", [1, 128], F32, const=True)
    cT = sb("cT", [128, 8, NB], F32, const=True)
    posi = sb("posi", [128, NB * NT], I32, const=True)
    posf = sb("posf", [128, NB * NT], F32, const=True)
    badaT = sb("badaT", [128, 48], F32, const=True)
    g1T = sb("g1T", [128, 8], F32, const=True)
    g2T = sb("g2T", [128, 8], F32, const=True)
    bqT = sb("bqT", [128, 16], F32, const=True)
    k1T = sb("k1T", [128, 128], BF16, const=True)
    k2T = sb("k2T", [128, 128], BF16, const=True)
    kst = sb("kst", [128, 128], F32)
    v64 = sb("v64", [128, 6 * 64], F32, const=True)
    v128 = sb("v128", [128, 256], F32, const=True)
    qgB = sb("qgB", [128, 64], F32, const=True)
    subgB = sb("subgB", [128, 128], F32, const=True)
    neglam = sb("neglam", [128, 1], F32, const=True)
    lamt = sb("lamt", [128, 64], F32)
    lams = sb("lams", [128, 4], F32)
    wg2 = sb("wg2", [16, 256], F32, const=True)
    bgr = sb("bgr", [1, 256], F32, const=True)
    cvals = sb("cvals", [128, 4], F32, const=True)

    def C(name):
        o, w = coffs[name]
        return cst[:, o:o + w]

    sT = sb("sT", [128, 8], F32)
    modT = sb("modT", [128, 4, 8], F32)
    gm1 = sb("gm1", [128, 8], F32)
    gm2 = sb("gm2", [128, 8], F32)
    gt1B = sb("gt1B", [128, D], F32)
    gt2B = sb("gt2B", [128, D], F32)
    wst = [sb("wst%d" % i, [128, 8, 128], F32) for i in range(2)]

    NRING = 4
    ring = [sb("ring%d" % i, [128, 8, 512], BF16) for i in range(NRING)]
    rc = [0]

    def ring_next():
        r = ring[rc[0] % NRING]
        rc[0] += 1
        return r

    KT = [sb("KT%d" % i, [128, 4, 128], BF16) for i in range(NT)]
    VC = [sb("VC%d" % i, [128, 4, 130], BF16) for i in range(NT)]
    Sst = [sb("Sst%d" % i, [128, 128], F32) for i in range(2)]

    x1 = sb("x1", [128, ST, D], F32)
    xk = [Key("x1_%d" % i) for i in range(ST)]
    junk = sb("junk", [128, D], BF16)
    xn = sb("xn", [128, D], BF16)
    st1 = sb("st1", [128, 4], F32)
    hT = sb("hT", [128, 8, 128], BF16)
    h2T = sb("h2T", [128, 8, ST * 128], BF16)
    sq = sb("sq", [128, 512], F32)
    ss8 = sb("ss8", [128, 8], F32)
    qn = sb("qn", [128, 512], F32)
    rt = [sb("rt%d" % i, [128, 256], F32) for i in range(2)]
    qr = sb("qr", [128, 512], BF16)
    qT = sb("qT", [128, 4, 128], BF16)
    ang = sb("ang", [128, 32], F32)
    ang2 = sb("ang2", [128, 32], F32)
    ang3 = sb("ang3", [128, 32], F32)
    angi = sb("angi", [128, 32], I32)
    sinT = sb("sinT", [128, 32], F32)
    cosT = sb("cosT", [128, 32], F32)
    PT = [sb("PT%d" % i, [128, 512], BF16) for i in range(2)]
    osb = sb("osb", [128, 128], F32)
    rz = sb("rz", [128, 4], F32)
    ybf = sb("ybf", [128, D], BF16)
    ykey2 = Key("ybf_gla")
    junk2 = sb("junk2", [128, 128], BF16)
    rz2 = sb("rz2", [128, 4], F32)
    osb2 = sb("osb2", [128, 128], F32)
    yT = sb("yT", [128, 8, 128], BF16)
    gqk = sb("gqk", [128, 512], F32)
    gv = sb("gv", [128, 512], F32)
    sr = sb("sr", [128, 512], F32)
    ggT = sb("ggT", [16, 128], F32)
    la = sb("la", [128, 256], F32)
    gl = [sb("gl%d" % i, [128, 256], F32) for i in range(8)]
    dec = sb("dec", [128, 2, 2], F32)
    T3 = sb("T3", [128, 3, 128], F32)
    ATs = sb("ATs", [128, 128], F32)

    psT = Tl(nc, "psT", [128, 1024], BF16, psum=True)
    psP = [Tl(nc, "psP%d" % i, [128, 512], F32, psum=True) for i in range(2)]
    psS = [Tl(nc, "psS%d" % i, [128, 512], F32, psum=True) for i in range(2)]
    psO = Tl(nc, "psO", [128, 512], F32, psum=True)
    psG = Tl(nc, "psG", [128, 512], F32, psum=True)
    psG2 = Tl(nc, "psG2", [128, 512], F32, psum=True)

    def mm(out, lhsT, rhs, start, stop, reads, writes, inc=True, skip=False):
        if skip:
            S.op("pe", lambda e: e.matmul(out, lhsT=lhsT, rhs=rhs, start=start, stop=stop, skip_group_check=True),
                 reads, writes, inc)
        else:
            S.op("pe", lambda e: e.matmul(out, lhsT=lhsT, rhs=rhs, start=start, stop=stop), reads, writes, inc)

    def tr(out, in_, ident, reads, writes, inc=True):
        S.op("pe", lambda e: e.transpose(out, in_, ident), reads, writes, inc)

    def act(out, in_, func, reads, writes, bias=None, scale=None, accum_out=None, ww_ok=False):
        kw = {}
        if bias is not None:
            kw["bias"] = bias
        if scale is not None:
            kw["scale"] = scale
        if accum_out is not None:
            kw["accum_out"] = accum_out
        S.op("act", lambda e: e.activation(out=out, in_=in_, func=func, **kw), reads, writes, ww_ok=ww_ok)

    def tt(eng, out, in0, in1, op, reads, writes):
        S.op(eng, lambda e: e.tensor_tensor(out=out, in0=in0, in1=in1, op=op), reads, writes)

    def ts(eng, out, in0, s1, s2, op0, op1, reads, writes):
        if s2 is None:
            S.op(eng, lambda e: e.tensor_scalar(out=out, in0=in0, scalar1=s1, scalar2=None, op0=op0), reads, writes)
        else:
            S.op(eng, lambda e: e.tensor_scalar(out=out, in0=in0, scalar1=s1, scalar2=s2, op0=op0, op1=op1),
                 reads, writes)

    def stt(eng, out, in0, scalar, in1, op0, op1, reads, writes):
        S.op(eng, lambda e: e.scalar_tensor_tensor(out=out, in0=in0, scalar=scalar, in1=in1, op0=op0, op1=op1),
             reads, writes)

    def cp(eng, out, in_, reads, writes):
        if eng == "act":
            S.op(eng, lambda e: e.activation(out=out, in_=in_, func=AF.Identity), reads, writes)
        else:
            S.op(eng, lambda e: e.tensor_copy(out=out, in_=in_), reads, writes)

    def red(eng, out, in_, op, reads, writes):
        S.op(eng, lambda e: e.tensor_reduce(out=out, in_=in_, axis=AX.X, op=op), reads, writes)

    def rsqrt_mean(dst, src, n, keys):
        act(dst, src, AF.Ln, list(keys) + [cvals.k], keys, bias=cvals[:, 1:2], scale=1.0 / n)
        act(dst, dst, AF.Exp, keys, keys, scale=-0.5)

    def ld(out, in_, tile, reads=(), **kw):
        return S.dma("sp", out, in_, owner=tile.k, reads=reads, writes=[tile.k], **kw)

    def cast_copy(dst, src, key, rows, cols, cchunk):
        for r0 in range(0, rows, 128):
            for c0 in range(0, cols, cchunk):
                c1 = min(cols, c0 + cchunk)
                if chain_casts and key.lw is not None:
                    S.wait_all("pool", [key.lw])
                S.dma("pool", dst[r0:r0 + 128, c0:c1], src[r0:r0 + 128, c0:c1], owner=key, writes=[key])

    cast_copy(win_s, win_d, k_win_s, D, 3088, 3088)
    cast_copy(wout_s, wout_d, k_wout_s, D, D, D)
    cast_copy(wq_s, wq_d, k_wq_s, D, 2048, 2048)
    if do_peer:
        cast_copy(uT_s, uT_d, k_uT_s, D, NEXP, 4096)
        cast_copy(v_s, v_d, k_v_s, NEXP, D, D)

    ld(cst[:, :], cst_d, cst)
    ld(cT[:, :, :], cT_d.rearrange("(k p) b -> p k b", p=128), cT, allow_slow_non_contiguous=True)
    ld(posi[:, :], pos_d, posi)
    ld(badaT[:, :], badaT_d, badaT)
    ld(g1T[:, :], g1T_d, g1T)
    ld(g2T[:, :], g2T_d, g2T)
    ld(bqT[:, :], bqT_d, bqT)
    ld(v64[:, :], vec64_d[0:1, :].to_broadcast([128, 384]), v64)
    ld(v128[:, :], vec128_d[0:1, :].to_broadcast([128, 256]), v128)
    ld(wg2[:, :], wg2_d, wg2)
    ld(bgr[:, :], bg_d, bgr)
    S.op("dve", lambda e: e.memset(onesr[:, :], 1.0), (), [onesr.k])
    S.op("dve", lambda e: e.memset(cvals[:, 0:1], -math.pi), (), [cvals.k])
    S.op("dve", lambda e: e.memset(cvals[:, 1:2], EPS), (), [cvals.k])
    S.op("dve", lambda e: e.memset(cvals[:, 2:3], 1.0), (), [cvals.k])
    S.op("dve", lambda e: e.memset(cvals[:, 3:4], 0.0), (), [cvals.k])
    cp("dve", identb[:, :], C("ident"), [cst.k], [identb.k])
    cp("dve", causb[:, :], C("causal"), [cst.k], [causb.k])
    cp("dve", posf[:, :], posi[:, :], [posi.k], [posf.k])
    ld(kst[:, :], k1T_d, kst)
    cp("dve", k1T[:, :], kst[:, :], [kst.k], [k1T.k])
    ld(kst[:, :], k2T_d, kst)
    cp("dve", k2T[:, :], kst[:, :], [kst.k], [k2T.k])
    ts("dve", qgB[:, :], v64[:, 0:64], 0.125, None, ALU.mult, None, [v64.k], [qgB.k])
    ts("dve", subgB[:, :], v128[:, 0:128], 0.8, None, ALU.mult, None, [v128.k], [subgB.k])
    tt("dve", lamt[:, :], v64[:, 128:192], v64[:, 192:256], ALU.mult, [v64.k], [lamt.k])
    red("dve", lams[:, 0:1], lamt[:, :], ALU.add, [lamt.k], [lams.k])
    tt("dve", lamt[:, :], v64[:, 256:320], v64[:, 320:384], ALU.mult, [v64.k, lams.k], [lamt.k])
    red("dve", lams[:, 1:2], lamt[:, :], ALU.add, [lamt.k], [lams.k])
    act(lams[:, 2:4], lams[:, 0:2], AF.Exp, [lams.k], [lams.k])
    tt("dve", lams[:, 0:1], lams[:, 3:4], lams[:, 2:3], ALU.subtract, [lams.k], [lams.k])
    ts("dve", neglam[:, :], lams[:, 0:1], -0.2, None, ALU.add, None, [lams.k], [neglam.k])
    for i in range(NT):
        S.op("pool", lambda e, i=i: e.memset(VC[i][:, :, :], 1.0), (), [VC[i].k])

    def adaln(b):
        act(sT[:, :], cT[:, :, b], AF.Silu, [cT.k], [sT.k])
        ld(gt1B[:, :], bada_d[0:1, 2 * D:3 * D].to_broadcast([128, D]), gt1B)
        ld(gt2B[:, :], bada_d[0:1, 5 * D:6 * D].to_broadcast([128, D]), gt2B)
        wi = 0
        for sec in range(6):
            for q8 in range(8):
                w = wst[wi % 2]
                wi += 1
                c0 = sec * D + q8 * 128
                ld(w[:, :, :], wada_d[:, c0:c0 + 128].rearrange("(k p) n -> p k n", p=128), w)
                ps = psP[wi % 2]
                if sec in (2, 5):
                    for kc in range(8):
                        mm(ps[:, 0:128], sT[:, kc:kc + 1].to_broadcast([128, 128]), w[:, kc, :],
                           kc == 0, kc == 7, [sT.k, w.k], [ps.k], inc=(kc == 7))
                    g = gt1B if sec == 2 else gt2B
                    tt("dve", g[:, q8 * 128:(q8 + 1) * 128], g[:, q8 * 128:(q8 + 1) * 128], ps[:, 0:128], ALU.add,
                       [ps.k, g.k], [g.k])
                else:
                    mi = {0: 0, 1: 1, 3: 2, 4: 3}[sec]
                    for kc in range(8):
                        mm(ps[:, 0:1], w[:, kc, :], sT[:, kc:kc + 1],
                           kc == 0, kc == 7, [sT.k, w.k], [ps.k], inc=(kc == 7))
                    tt("dve", modT[:, mi, q8:q8 + 1], ps[:, 0:1],
                       badaT[:, sec * 8 + q8:sec * 8 + q8 + 1], ALU.add, [ps.k, badaT.k], [modT.k])
        stt("dve", gm1[:, :], modT[:, 1, :], 1.0, g1T[:, :], ALU.add, ALU.mult, [modT.k, g1T.k], [gm1.k])
        stt("dve", gm2[:, :], modT[:, 3, :], 1.0, g2T[:, :], ALU.add, ALU.mult, [modT.k, g2T.k], [gm2.k])

    def norm_T(xap, xkey, gm, shi, dst, dcol):
        act(junk[:, :], xap, AF.Square, [xkey], [junk.k, st1.k], accum_out=st1[:, 0:1])
        rsqrt_mean(st1[:, 2:3], st1[:, 0:1], D, [st1.k])
        ts("dve", xn[:, :], xap, st1[:, 2:3], None, ALU.mult, None, [xkey, st1.k], [xn.k])
        for j in range(8):
            tr(psT[:, j * 128:(j + 1) * 128], xn[:, j * 128:(j + 1) * 128], identb[:, :],
               [xn.k, identb.k], [psT.k], inc=(j == 7))
        for j in range(8):
            act(dst[:, j, dcol:dcol + 128], psT[:, j * 128:(j + 1) * 128], AF.Identity,
                [psT.k, gm.k, modT.k], [dst.k], bias=modT[:, shi, j:j + 1], scale=gm[:, j:j + 1])

    def rope_tables(col):
        C1 = 6.28125
        C2 = 2.0 * math.pi - C1
        ts("dve", ang[:, :], C("invf"), posf[:, col:col + 1], None, ALU.mult, None, [cst.k, posf.k], [ang.k])
        for (shift, dst) in ((0.0, sinT), (0.5 * math.pi, cosT)):
            ts("dve", ang2[:, :], ang[:, :], shift, 1.0 / (2.0 * math.pi), ALU.add, ALU.mult, [ang.k], [ang2.k])
            cp("dve", angi[:, :], ang2[:, :], [ang2.k], [angi.k])
            cp("dve", ang2[:, :], angi[:, :], [angi.k], [ang2.k])
            stt("dve", ang3[:, :], ang2[:, :], -C1, ang[:, :], ALU.mult, ALU.add, [ang2.k, ang.k], [ang3.k])
            stt("dve", ang3[:, :], ang2[:, :], -C2, ang3[:, :], ALU.mult, ALU.add, [ang2.k, ang3.k], [ang3.k])
            ts("dve", ang3[:, :], ang3[:, :], shift, math.pi, ALU.add, ALU.min, [ang3.k], [ang3.k])
            ts("dve", ang3[:, :], ang3[:, :], -math.pi, None, ALU.max, None, [ang3.k], [ang3.k])
            act(dst[:, :], ang3[:, :], AF.Sin, [ang3.k], [dst.k])

    def proj_block(c0, ncols, ps, lhs=None):
        r = ring_next()
        ld(r[:, :, 0:ncols], win_s[:, c0:c0 + ncols].rearrange("(k p) n -> p k n", p=128), r, reads=[k_win_s])
        for kc in range(8):
            mm(ps[:, 0:ncols], hT[:, kc, :], r[:, kc, 0:ncols], kc == 0, kc == 7,
               [hT.k, r.k], [ps.k], inc=(kc == 7))
        return r

    def qk_post(ps, gB, dstT, dkey):
        act(sq[:, :], ps[:, :], AF.Square, [ps.k], [sq.k])
        red("dve", ss8[:, :], sq[:, :].rearrange("p (g d) -> p g d", d=64), ALU.add, [sq.k], [ss8.k])
        rsqrt_mean(ss8[:, :], ss8[:, :], 64, [ss8.k])
        q3 = qn[:, :].rearrange("p (g d) -> p g d", d=64)
        tt("dve", q3, ps[:, :].rearrange("p (g d) -> p g d", d=64),
           ss8[:, :].unsqueeze(2).to_broadcast([128, 8, 64]), ALU.mult, [ps.k, ss8.k], [qn.k])
        tt("dve", q3, q3, gB.unsqueeze(1).to_broadcast([128, 8, 64]), ALU.mult, [qn.k, v64.k, qgB.k], [qn.k])
        x1v = q3[:, :, 0:32]
        x2v = q3[:, :, 32:64]
        cb = cosT[:, :].unsqueeze(1).to_broadcast([128, 8, 32])
        sbb = sinT[:, :].unsqueeze(1).to_broadcast([128, 8, 32])
        r0, r1 = (t[:, :].rearrange("p (g d) -> p g d", d=32) for t in rt)
        qr3 = qr[:, :].rearrange("p (g d) -> p g d", d=64)
        tt("dve", r0, x1v, cb, ALU.mult, [qn.k, cosT.k], [rt[0].k])
        tt("dve", r1, x2v, sbb, ALU.mult, [qn.k, sinT.k], [rt[1].k])
        tt("dve", qr3[:, :, 0:32], r0, r1, ALU.subtract, [rt[0].k, rt[1].k], [qr.k])
        tt("dve", r0, x2v, cb, ALU.mult, [qn.k, cosT.k], [rt[0].k])
        tt("dve", r1, x1v, sbb, ALU.mult, [qn.k, sinT.k], [rt[1].k])
        tt("dve", qr3[:, :, 32:64], r0, r1, ALU.add, [rt[0].k, rt[1].k], [qr.k])
        for h in range(4):
            tr(psT[:, h * 128:(h + 1) * 128], qr[:, h * 128:(h + 1) * 128], identb[:, :],
               [qr.k, identb.k], [psT.k], inc=(h == 3))
        cp("dve", dstT[:, :, :], psT[:, 0:512].rearrange("p (h t) -> p h t", t=128),
           [psT.k], [dkey])

    dbg_toks = []

    def dump(name, ap, key):
        if name in dbg_d:
            dbg_toks.append(S.dma("sp", dbg_d[name], ap, owner=key, reads=[key]))

    def mixer_tile(b, i, slot):
        col = b * NT + i
        tok0 = b * SEQ + i * 128
        xs = x1[:, slot, :]
        xkey = xk[slot]
        S.dma("sp", xs, x_d[tok0:tok0 + 128, :], owner=xkey, writes=[xkey])
        if slevel < 2:
            return
        norm_T(xs, xkey, gm1, 0, hT, 0)
        rope_tables(col)
        if slevel < 3:
            return
        proj_block(0, 512, psP[0])
        qk_post(psP[0], qgB[:, :], qT, qT.k)
        proj_block(512, 512, psP[1])
        qk_post(psP[1], v64[:, 64:128], KT[i], KT[i].k)
        proj_block(1024, 512, psP[0])
        cp("act", VC[i][:, :, 0:128], psP[0][:, :].rearrange("p (h e) -> p h e", e=128), [psP[0].k], [VC[i].k])
        if slevel < 4:
            return
        proj_block(1536, 512, psP[1])
        cp("act", gqk[:, :], psP[1][:, :], [psP[1].k], [gqk.k])
        proj_block(2048, 512, psP[0])
        cp("act", gv[:, :], psP[0][:, :], [psP[0].k], [gv.k])
        proj_block(2560, 512, psP[1])
        act(sr[:, :], psP[1][:, :], AF.Silu, [psP[1].k], [sr.k])
        r = ring_next()
        ld(r[:, :, 0:16], win_s[:, 3072:3088].rearrange("(k p) n -> p k n", p=128), r, reads=[k_win_s])
        for kc in range(8):
            mm(psP[0][0:16, 0:128], r[:, kc, 0:16], hT[:, kc, :], kc == 0, kc == 7,
               [hT.k, r.k], [psP[0].k], inc=(kc == 7))
        cp("act", ggT[:, :], psP[0][0:16, 0:128], [psP[0].k], [ggT.k])

        if slevel < 5:
            return

        def attn_part():
            nkb = i + 1
            ngrp = (nkb + 3) // 4
            sidx = 0
            for h in range(4):
                for m in range(2):
                    pr = slice(m * 64, (m + 1) * 64)
                    for g in range(ngrp):
                        j0 = g * 4
                        nj = min(4, nkb - j0)
                        pss = psS[sidx % 2]
                        pt = PT[sidx % 2]
                        sidx += 1
                        for jj in range(nj):
                            j = j0 + jj
                            mm(pss[:, jj * 128:(jj + 1) * 128], KT[j][pr, h, :], qT[pr, h, :], True, True,
                               [KT[j].k, qT.k], [pss.k], inc=(jj == nj - 1))
                        act(pt[:, 0:nj * 128], pss[:, 0:nj * 128], AF.Exp, [pss.k], [pt.k])
                        if j0 + nj - 1 == i:
                            dsl = slice((nj - 1) * 128, nj * 128)
                            tt("pool", pt[:, dsl], pt[:, dsl], causb[:, :], ALU.mult, [pt.k, causb.k], [pt.k])
                        for jj in range(nj):
                            j = j0 + jj
                            mm(psO[:, m * 129:(m + 1) * 129], pt[:, jj * 128:(jj + 1) * 128], VC[j][:, h, 0:129],
                               j == 0, j == i, [pt.k, VC[j].k], [psO.k], inc=(jj == nj - 1))
                S.op("dve", lambda e: e.reciprocal(out=rz[:, 0:1], in_=psO[:, 128:129]), [psO.k], [rz.k])
                S.op("dve", lambda e: e.reciprocal(out=rz[:, 1:2], in_=psO[:, 257:258]), [psO.k], [rz.k])
                tt("dve", rz[:, 2:3], rz[:, 1:2], neglam[:, :], ALU.mult, [rz.k, neglam.k], [rz.k])
                ts("dve", osb[:, :], psO[:, 0:128], rz[:, 0:1], None, ALU.mult, None, [psO.k, rz.k], [osb.k])
                stt("dve", osb[:, :], psO[:, 129:257], rz[:, 2:3], osb[:, :], ALU.mult, ALU.add,
                    [psO.k, rz.k, osb.k], [osb.k])
                act(junk[:, 0:128], osb[:, :], AF.Square, [osb.k], [junk.k, rz.k], accum_out=rz[:, 3:4])
                rsqrt_mean(rz[:, 3:4], rz[:, 3:4], 128, [rz.k])
                stt("dve", ybf[:, h * 128:(h + 1) * 128], osb[:, :], rz[:, 3:4], subgB[:, :], ALU.mult, ALU.mult,
                    [osb.k, rz.k, subgB.k], [ybf.k])


        def gla_part():
            mm(psG[:, 0:256], ggT[:, :], wg2[:, :], True, False, [ggT.k, wg2.k], [psG.k], inc=False)
            mm(psG[:, 0:256], onesr[:, :], bgr[:, :], False, True, [onesr.k, bgr.k], [psG.k])
            act(la[:, :], psG[:, 0:256], AF.Exp, [psG.k], [la.k], scale=-1.0)
            act(la[:, :], la[:, :], AF.Ln, [la.k, cvals.k], [la.k], bias=cvals[:, 2:3])
            ts("dve", la[:, :], la[:, :], -1.0 / 16, None, ALU.mult, None, [la.k], [la.k])
            if GSUB < 1:
                return
            mm(psG[:, 0:256], C("mcum"), la[:, :], True, True, [cst.k, la.k], [psG.k], inc=False)
            mm(psG[:, 256:512], C("mmid"), la[:, :], True, True, [cst.k, la.k], [psG.k])
            mm(psG2[:, 0:256], C("mblk"), la[:, :], True, True, [cst.k, la.k], [psG2.k], inc=False)
            for hp in range(2):
                for hh in range(2):
                    h = hp * 2 + hh
                    mm(psG2[hh * 64:(hh + 1) * 64, 256 + hp * 2:256 + hp * 2 + 2], la[:, h * 64:(h + 1) * 64],
                       C("chunkind"), True, True, [la.k, cst.k], [psG2.k], inc=(hp == 1 and hh == 1))
            if GSUB < 2:
                return
            bc, d1, eq, ek, eb, d2, ed, qg = gl
            cp("dve", bc[:, :], psG[:, 0:256], [psG.k], [bc.k])
            tt("dve", d1[:, :], bc[:, :], psG[:, 256:512], ALU.subtract, [bc.k, psG.k], [d1.k])
            tt("dve", d2[:, :], psG2[:, 0:256], bc[:, :], ALU.subtract, [bc.k, psG2.k], [d2.k])
            act(dec[:, :, :], psG2[:, 256:260].rearrange("p (a c) -> p a c", c=2), AF.Exp, [psG2.k], [dec.k])
            act(eq[:, :], d1[:, :], AF.Exp, [d1.k], [eq.k])
            act(ek[:, :], d1[:, :], AF.Exp, [d1.k], [ek.k], scale=-1.0)
            act(eb[:, :], bc[:, :], AF.Exp, [bc.k], [eb.k])
            act(ed[:, :], d2[:, :], AF.Exp, [d2.k], [ed.k])
            stt("dve", eq[:, :], gqk[:, 0:256], 0.125, eq[:, :], ALU.mult, ALU.mult, [gqk.k, eq.k], [eq.k])
            tt("dve", ek[:, :], gqk[:, 256:512], ek[:, :], ALU.mult, [gqk.k, ek.k], [ek.k])
            stt("dve", eb[:, :], gqk[:, 0:256], 0.125, eb[:, :], ALU.mult, ALU.mult, [gqk.k, eb.k], [eb.k])
            tt("dve", ed[:, :], gqk[:, 256:512], ed[:, :], ALU.mult, [gqk.k, ed.k], [ed.k])
            ci = C("chunkind")
            ts("dve", d1[:, :], ed[:, :], ci[:, 0:1], None, ALU.mult, None, [ed.k, cst.k, d1.k], [d1.k])
            ts("dve", d2[:, :], ed[:, :], ci[:, 1:2], None, ALU.mult, None, [ed.k, cst.k, d2.k], [d2.k])
            if GSUB < 3:
                return
            for hp in range(2):
                cs = slice(hp * 128, (hp + 1) * 128)
                mm(psG[:, 0:128], eq[:, cs], C("ident"), True, True, [eq.k, cst.k], [psG.k], inc=False)
                mm(psG[:, 128:256], ek[:, cs], C("ident"), True, True, [ek.k, cst.k], [psG.k], inc=False)
                mm(psG[:, 256:384], eb[:, cs], C("ident"), True, True, [eb.k, cst.k], [psG.k])
                cp("act", T3[:, :, :], psG[:, 0:384].rearrange("p (a t) -> p a t", t=128), [psG.k], [T3.k])
                if GSUB < 4:
                    continue
                st = Sst[hp]
                if i == 0:
                    S.op("dve", lambda e, st=st: e.memset(st[:, :], 0.0), (), [st.k])
                for hh in range(2):
                    h = hp * 2 + hh
                    pr = slice(hh * 64, (hh + 1) * 64)
                    vcols = slice(h * 128, (h + 1) * 128)
                    for c, kdm in enumerate((d1, d2)):
                        mm(psG2[pr, c * 128:(c + 1) * 128], kdm[:, h * 64:(h + 1) * 64], gv[:, vcols], True, True,
                           [kdm.k, gv.k], [psG2.k], inc=False)
                    mm(psG2[:, 256:384], T3[pr, 1, :], T3[pr, 0, :], True, True, [T3.k], [psG2.k])
                    tt("dve", ATs[:, :], psG2[:, 256:384], C("mcum"), ALU.mult, [psG2.k, cst.k], [ATs.k])
                    if GSUB < 5:
                        continue
                    mm(psP[0][:, 0:128], ATs[:, :], gv[:, vcols], True, False, [ATs.k, gv.k], [psP[0].k], inc=False, skip=True)
                    mm(psP[0][0:64, 0:128], T3[pr, 2, 0:64], st[pr, :], False, False, [T3.k, st.k], [psP[0].k], inc=True,
                       skip=True)
                    if GSUB < 6:
                        continue
                    stt("dve", st[pr, :], st[pr, :], dec[pr, hp, 0:1], psG2[pr, 0:128], ALU.mult, ALU.add,
                        [st.k, dec.k, psG2.k], [st.k])
                    mm(psP[0][64:128, 0:128], T3[pr, 2, 64:128], st[pr, :], False, True, [T3.k, st.k], [psP[0].k], skip=True)
                    if GSUB < 7:
                        continue
                    stt("dve", st[pr, :], st[pr, :], dec[pr, hp, 1:2], psG2[pr, 128:256], ALU.mult, ALU.add,
                        [st.k, dec.k, psG2.k], [st.k])
                    if GSUB < 8:
                        continue
                    act(junk2[:, 0:128], psP[0][:, 0:128], AF.Square, [psP[0].k], [junk2.k, rz2.k], accum_out=rz2[:, 3:4])
                    rsqrt_mean(rz2[:, 3:4], rz2[:, 3:4], 128, [rz2.k])
                    stt("dve", osb2[:, :], psP[0][:, 0:128], rz2[:, 3:4], v128[:, 128:256], ALU.mult, ALU.mult,
                        [psP[0].k, rz2.k, v128.k], [osb2.k])
                    tt("dve", ybf[:, 512 + h * 128:512 + (h + 1) * 128], osb2[:, :], sr[:, vcols], ALU.mult,
                       [osb2.k, sr.k], [ykey2])


        if slevel >= 6 and INTERLEAVE:
            co = Co(gla_part)
            S.co = co
            attn_part()
            S.co = None
            co.finish()
        else:
            attn_part()
            if slevel >= 6:
                gla_part()
        if slevel < 7:
            return
        for j in range(8):
            tr(psT[:, j * 128:(j + 1) * 128], ybf[:, j * 128:(j + 1) * 128], identb[:, :],
               [ybf.k, ykey2, identb.k], [psT.k], inc=(j == 7))
        cp("act", yT[:, :, :], psT[:, :].rearrange("p (j t) -> p j t", t=128), [psT.k], [yT.k])
        for half in range(2):
            r = ring_next()
            ld(r[:, :, :], wout_s[:, half * 512:(half + 1) * 512].rearrange("(k p) n -> p k n", p=128), r, reads=[k_wout_s])
            ps = psP[half]
            for kc in range(8):
                mm(ps[:, :], yT[:, kc, :], r[:, kc, :], kc == 0, kc == 7, [yT.k, r.k], [ps.k],
                   inc=(kc == 7))
            cs = slice(half * 512, (half + 1) * 512)
            tt("dve", sq[:, :], ps[:, :], gt1B[:, cs], ALU.mult, [ps.k, gt1B.k], [sq.k])
            tt("dve", x1[:, slot, cs], sq[:, :], x1[:, slot, cs], ALU.add, [sq.k, xkey], [xkey])
        if do_peer:
            norm_T(xs, xkey, gm2, 2, h2T, slot * 128)

    if do_peer:
        qpc = [sb("qpc%d" % i, [128, ST * 128], BF16) for i in range(2)]
        s1m = sb("s1m", [128, ST, 8, 128], F32)
        s2m = sb("s2m", [128, ST, 8, 128], F32)
        wk = sb("wk", [128, 256], F32)
        v1 = sb("v1", [128, 8, 16], F32)
        v2 = sb("v2", [128, 8, 16], F32)
        c24 = sb("c24", [128, 8, 24], F32)
        thr = sb("thr", [128, 8], F32)
        nrm = sb("nrm", [128, 8], F32)
        ex16 = sb("ex16", [128, 8, 16], F32)
        DG = [sb("DG%d" % t, [128, 8, 128], BF16) for t in range(ST)]
        zbs = [sb("zb%d" % i, [128, 8, 2, 128], F32) for i in range(2)]
        zb = zbs[0]
        zk2s = [Key("zk2_%d" % i) for i in range(3)]
        zi = [0]
        cand = zb[:, :, :, :].rearrange("p h a j -> p h (a j)")
        wbs = [sb("wb%d" % i, [128, 8, 2, 128], BF16) for i in range(2)]
        gas = [sb("ga%d" % i, [128, 2, ST * 128], F32) for i in range(2)]
        GTs = [sb("GT%d" % i, [128, 2, 128], BF16) for i in range(2)]
        oacc = sb("oacc", [128, ST, D], F32)
        ok = [Key("oacc_%d" % i) for i in range(ST)]

    out_toks = []

    def top16(src_ap, src_keys, dst, h):
        n = src_ap.shape[-1]
        S.op("dve", lambda e: e.max(out=dst[:, h, 0:8], in_=src_ap), src_keys, [dst.k])
        S.op("dve", lambda e: e.match_replace(out=wk[:, 0:n], in_to_replace=dst[:, h, 0:8], in_values=src_ap,
                                              imm_value=-BIG), list(src_keys) + [dst.k], [wk.k])
        S.op("dve", lambda e: e.max(out=dst[:, h, 8:16], in_=wk[:, 0:n]), [wk.k], [dst.k])

    def peer_supertile(b, st_i):
        T2 = ST * 128
        for blk in range(4):
            r = ring_next()
            ld(r[:, :, :], wq_s[:, blk * 512:(blk + 1) * 512].rearrange("(k p) n -> p k n", p=128), r, reads=[k_wq_s])
            for c4 in range(4):
                cc = blk * 4 + c4
                h, half = cc // 2, cc % 2
                ps = psP[cc % 2]
                qc = qpc[cc % 2]
                for kc in range(8):
                    mm(ps[:, 0:T2], r[:, kc, c4 * 128:(c4 + 1) * 128], h2T[:, kc, :], kc == 0, kc == 7,
                       [r.k, h2T.k], [ps.k], inc=(kc == 7))
                act(qc[:, :], ps[:, 0:T2], AF.Identity, [ps.k, bqT.k], [qc.k], bias=bqT[:, cc:cc + 1])
                pss = psS[cc % 2]
                kT = k1T if half == 0 else k2T
                dstm = s1m if half == 0 else s2m
                for t in range(ST):
                    mm(pss[:, t * 128:(t + 1) * 128], qc[:, t * 128:(t + 1) * 128], kT[:, :], True, True,
                       [qc.k, kT.k], [pss.k], inc=(t == ST - 1))
                cp("dve", dstm[:, :, h, :], pss[:, 0:T2].rearrange("p (t k) -> p t k", k=128), [pss.k], [dstm.k])
        for t in range(ST):
            for h in range(8):
                top16(s1m[:, t, h, :], [s1m.k], v1, h)
                top16(s2m[:, t, h, :], [s2m.k], v2, h)
            tt("dve", cand.rearrange("p h (a b) -> p h a b", b=16),
               v1[:, :, :].unsqueeze(3).to_broadcast([128, 8, 16, 16]),
               v2[:, :, :].unsqueeze(2).to_broadcast([128, 8, 16, 16]), ALU.add, [v1.k, v2.k], [zb.k])
            for h in range(8):
                top16(cand[:, h, :], [zb.k], c24, h)
                S.op("dve", lambda e, h=h: e.match_replace(out=wk[:, :], in_to_replace=c24[:, h, 8:16],
                                                           in_values=wk[:, :], imm_value=-BIG),
                     [wk.k, c24.k], [wk.k])
                S.op("dve", lambda e, h=h: e.max(out=c24[:, h, 16:24], in_=wk[:, :]), [wk.k], [c24.k])
            tt("dve", thr[:, :], c24[:, :, 15], c24[:, :, 16], ALU.add, [c24.k], [thr.k])
            ts("dve", thr[:, :], thr[:, :], 0.5, None, ALU.mult, None, [thr.k], [thr.k])
            tt("dve", ex16[:, :, :], c24[:, :, 0:16], thr[:, :].unsqueeze(2).to_broadcast([128, 8, 16]),
               ALU.subtract, [c24.k, thr.k], [ex16.k])
            act(ex16[:, :, :], ex16[:, :, :], AF.Exp, [ex16.k], [ex16.k])
            red("dve", nrm[:, :], ex16[:, :, :], ALU.add, [ex16.k], [nrm.k])
            S.op("dve", lambda e: e.reciprocal(out=nrm[:, :], in_=nrm[:, :]), [nrm.k], [nrm.k])
            for h in range(8):
                ts("dve", DG[t][:, h, :], C("ident"), nrm[:, h:h + 1], None, ALU.mult, None, [cst.k, nrm.k],
                   [DG[t].k])
            for (sm, vv, sub_thr) in ((s1m, v1, True), (s2m, v2, False)):
                mskt = zbs[1][:, :, 0, :]
                tt("dve", mskt, sm[:, t, :, :], vv[:, :, 15:16].to_broadcast([128, 8, 128]), ALU.is_lt,
                   [sm.k, vv.k], [zbs[1].k])
                stt("dve", sm[:, t, :, :], mskt, -BIG, sm[:, t, :, :], ALU.mult, ALU.add,
                    [zbs[1].k, sm.k], [sm.k])
                if sub_thr:
                    tt("dve", sm[:, t, :, :], sm[:, t, :, :], thr[:, :].unsqueeze(2).to_broadcast([128, 8, 128]),
                       ALU.subtract, [sm.k, thr.k], [sm.k])
                act(sm[:, t, :, :], sm[:, t, :, :], AF.Exp, [sm.k], [sm.k])
        psWs = (psS[0], psP[0])
        psA = psS[1]
        psV = ((psO, psG), (psG2, psP[1]))
        NG = NEXP // 512
        slots = {}

        def load_group(g):
            ru = ring_next()
            ld(ru[:, :, :], uT_s[:, g * 512:(g + 1) * 512].rearrange("(k p) n -> p k n", p=128), ru, reads=[k_uT_s])
            rv = ring_next()
            rv4 = rv[:, :, :].rearrange("p (c a) n -> p c (a n)", a=2)
            ld(rv4, v_s[g * 512:(g + 1) * 512, :].rearrange("(c p) d -> p c d", p=128), rv, reads=[k_v_s])
            slots[g] = (ru, rv, rv4)

        def stage_A_mm(g, sub):
            ru = slots[g][0]
            for ii in range(2):
                ec = sub * 2 + ii
                for kc in range(8):
                    mm(psA[:, ii * T2:(ii + 1) * T2], ru[:, kc, ec * 128:(ec + 1) * 128], h2T[:, kc, :],
                       kc == 0, kc == 7, [ru.k, h2T.k], [psA.k], inc=(kc == 7))

        def stage_A_gelu(g, sub):
            gt_ = gas[(g * 2 + sub) % 2]
            act(gt_[:, :, :], psA[:, 0:2 * T2].rearrange("p (a t) -> p a t", t=T2), AF.Gelu, [psA.k], [gt_.k])

        its = [(g, sub, t) for g in range(NG) for sub in range(2) for t in range(ST)]

        NZ = len(zbs)

        NHA = 6

        def stage_P(k):
            g, sub, t = its[k]
            i0 = g * 4 + sub * 2
            zt = zbs[k % NZ]
            zk2 = zk2s[k % NZ]
            for h in range(NHA):
                for ii in range(2):
                    act(zt[:, h, ii, :], s2m[:, t, h, :], AF.Identity, [s1m.k, s2m.k], [zt.k],
                        scale=s1m[:, t, h, i0 + ii:i0 + ii + 1], ww_ok=True)
            nd = 8 - NHA
            tt("dve", zt[:, NHA:8, :, :],
               s1m[:, t, NHA:8, i0:i0 + 2].unsqueeze(3).to_broadcast([128, nd, 2, 128]),
               s2m[:, t, NHA:8, :].unsqueeze(2).to_broadcast([128, nd, 2, 128]), ALU.mult,
               [s1m.k, s2m.k], [zk2])

        def stage_M(k):
            zt = zbs[k % NZ]
            zk2 = zk2s[k % NZ]
            wt = wbs[k % 2]
            stt("dve", wt[:, :, :, :], zt[:, :, :, :], 1.0, zt[:, :, :, :], ALU.is_ge, ALU.mult,
                [zt.k, zk2], [wt.k])

        def stage_W(k):
            g, sub, t = its[k]
            wt = wbs[k % 2]
            pw = psWs[k % 2]
            first = True
            for h in range(8):
                for ii in range(2):
                    mm(pw[:, ii * 128:(ii + 1) * 128], wt[:, h, ii, :], DG[t][:, h, :], first, (h == 7),
                       [wt.k, DG[t].k], [pw.k], inc=(h == 7 and ii == 1), skip=True)
                    first = False

        def stage_G(k):
            g, sub, t = its[k]
            pw = psWs[k % 2]
            gt_ = gas[(g * 2 + sub) % 2]
            tt("dve", GTs[k % 2][:, :, :], gt_[:, :, t * 128:(t + 1) * 128],
               pw[:, 0:256].rearrange("p (a t) -> p a t", t=128), ALU.mult, [gt_.k, pw.k], [GTs[k % 2].k])

        def stage_V(k):
            g, sub, t = its[k]
            rv, rv4 = slots[g][1], slots[g][2]
            for ii in range(2):
                ec = sub * 2 + ii
                for half in range(2):
                    pv = psV[t][half]
                    mm(pv[:, :], GTs[k % 2][:, ii, :], rv4[:, ec, half * 512:(half + 1) * 512],
                       (g == 0 and sub == 0 and ii == 0), (g == NG - 1 and sub == 1 and ii == 1),
                       [GTs[k % 2].k, rv.k], [pv.k], inc=(ii == 1 and half == 1))

        N = len(its)
        load_group(0)
        stage_A_mm(0, 0)
        stage_A_gelu(0, 0)
        stage_P(0)
        for k in range(N + 1):
            if k + 1 < N:
                stage_P(k + 1)
            if k < N:
                stage_M(k)
                stage_W(k)
            if k >= 1:
                stage_G(k - 1)
                stage_V(k - 1)
            if k < N:
                g, sub, t = its[k]
                ng, nsub = (g, 1) if sub == 0 else (g + 1, 0)
                if ng < NG:
                    if t == 0:
                        if nsub == 0:
                            load_group(ng)
                        stage_A_mm(ng, nsub)
                    else:
                        stage_A_gelu(ng, nsub)
        for t in range(ST):
            for half in range(2):
                cs = slice(half * 512, (half + 1) * 512)
                tt("dve", oacc[:, t, cs], psV[t][half][:, :], gt2B[:, cs], ALU.mult, [psV[t][half].k, gt2B.k], [ok[t]])
            tt("dve", oacc[:, t, :], oacc[:, t, :], x1[:, t, :], ALU.add, [ok[t], xk[t]], [ok[t]])
            tok0 = b * SEQ + (st_i * ST + t) * 128
            out_toks.append(S.dma("sp", y_d[tok0:tok0 + 128, :], oacc[:, t, :], owner=ok[t], reads=[ok[t]]))

    for b in range(NB):
        if slevel >= 1:
            adaln(b)
        for st_i in range(NST):
            for t in range(ST):
                mixer_tile(b, st_i * ST + t, t)
            if do_peer:
                peer_supertile(b, st_i)
            else:
                for t in range(ST):
                    tok0 = b * SEQ + (st_i * ST + t) * 128
                    out_toks.append(S.dma("sp", y_d[tok0:tok0 + 128, :], x1[:, t, :], owner=xk[t], reads=[xk[t]]))
    print("sbuf bytes remaining", nc.sbuf_bytes_remaining)
    S.wait_all("sp", out_toks + dbg_toks)
    S.emit()
    return nc, S


def make_in_maps(inputs, n_cores, NB, SEQ):
    f = lambda a: np.ascontiguousarray(np.asarray(a, dtype=np.float32))
    x = f(inputs["x"])
    c = f(inputs["c"])
    pos = np.asarray(inputs["positions"]).astype(np.int32)
    NT = SEQ // 128
    cblob, _ = _consts()
    shared = {
        "w_ada": f(inputs["w_ada"][0]),
        "b_adaT": f(np.asarray(inputs["b_ada"][0]).reshape(48, 128).T),
        "b_ada": f(np.asarray(inputs["b_ada"][0]).reshape(1, -1)),
        "g1T": f(np.asarray(inputs["norm1_g"][0]).reshape(8, 128).T),
        "g2T": f(np.asarray(inputs["norm2_g"][0]).reshape(8, 128).T),
        "w_in": f(inputs["w_in"][0]),
        "w_out": f(inputs["w_out"][0]),
        "w_query": f(inputs["w_query"][0]),
        "b_queryT": f(np.asarray(inputs["b_query"][0]).reshape(16, 128).T),
        "keys1T": f(np.asarray(inputs["peer_keys1"][0]).T),
        "keys2T": f(np.asarray(inputs["peer_keys2"][0]).T),
        "uT": f(np.asarray(inputs["expert_u"][0]).T),
        "ev": f(inputs["expert_v"][0]),
        "vec64": f(np.concatenate([np.asarray(inputs[k][0]).reshape(-1) for k in
                                   ("qn_g", "kn_g", "lam_q1", "lam_k1", "lam_q2", "lam_k2")]).reshape(1, -1)),
        "vec128": f(np.concatenate([np.asarray(inputs[k][0]).reshape(-1) for k in
                                    ("diff_norm_g", "gla_norm_g")]).reshape(1, -1)),
        "w_gate2": f(inputs["w_gate2"][0]),
        "b_gate": f(np.asarray(inputs["b_gate"][0]).reshape(1, -1)),
        "consts": cblob,
    }
    maps = []
    for i in range(n_cores):
        bs = slice(i * NB, (i + 1) * NB)
        m = dict(shared)
        m["x"] = np.ascontiguousarray(x[bs].reshape(NB * SEQ, D))
        m["cT"] = np.ascontiguousarray(c[bs].T)
        m["posT"] = np.ascontiguousarray(pos[bs].reshape(NB * NT, 128).T)
        maps.append(m)
    return maps


def kernel(**inputs):
    x = np.asarray(inputs["x"])
    B, SEQ, _ = x.shape
    NB = B // NCORES
    nc, _ = build_program(NB, SEQ, do_peer=True)
    maps = make_in_maps(inputs, NCORES, NB, SEQ)
    res = run_bass_kernel_spmd(nc, maps, core_ids=list(range(NCORES)))
    out = np.concatenate([np.asarray(r["y"]).reshape(NB, SEQ, D) for r in res.results], axis=0)
    return out.astype(np.float32)
```

```python
import math
import os
import threading
import numpy as np
import concourse.bass as bass
import concourse.mybir as mybir
from concourse.bass_utils import run_bass_kernel_spmd

F32 = mybir.dt.float32
BF16 = mybir.dt.bfloat16
I32 = mybir.dt.int32
AF = mybir.ActivationFunctionType
ALU = mybir.AluOpType
AX = mybir.AxisListType

D = 1024
NCORES = 8
EPS = 1e-6
NEXP = 16384
BIG = 1.0e4
GSUB = int(os.environ.get("GSUB", "9"))
INTERLEAVE = int(os.environ.get("INTERLEAVE", "1"))
NHA_ACT = int(os.environ.get("NHA_ACT", "4"))
NZBUF = int(os.environ.get("NZBUF", "3"))
CSH = float(np.float32(1.0 - 2.0 ** -9))


class Key:
    __slots__ = ("name", "excl", "const", "lw", "rd", "sem", "semcnt")

    def __init__(self, name, excl=False, const=False):
        self.name = name
        self.excl = excl
        self.const = const
        self.lw = None
        self.rd = []
        self.sem = None
        self.semcnt = 0


class Co:
    def __init__(self, fn):
        self.go = threading.Semaphore(0)
        self.back = threading.Semaphore(0)
        self.done = False
        self.budget = 0
        self.err = None
        self.th = threading.Thread(target=self._run, args=(fn,), daemon=True)
        self.th.start()

    def _run(self, fn):
        self.go.acquire()
        try:
            fn()
        except BaseException as e:
            self.err = e
        self.done = True
        self.back.release()

    def hook(self):
        if self.budget <= 0:
            self.back.release()
            self.go.acquire()
        self.budget -= 1

    def step(self, n):
        if self.done:
            return
        self.budget = n
        self.go.release()
        self.back.acquire()
        if self.err is not None:
            raise self.err

    def finish(self):
        while not self.done:
            self.step(1000)
        if self.err is not None:
            raise self.err


class Sched:
    def __init__(self, nc):
        self.co = None
        self.nc = nc
        self.engs = ("pe", "act", "dve", "pool", "sp")
        self.prog = {e: [] for e in self.engs}
        self.cnt = {e: 0 for e in self.engs}
        self.seen = {e: {} for e in self.engs}
        self.esem = {e: nc.alloc_semaphore(name="tl_" + e) for e in self.engs}
        self.n_ins = 0

    def _need(self, eng, tok, waits, raw):
        if tok is None:
            return
        if tok[0] == "e":
            _, f, n = tok
            if f == eng and eng == "pe":
                return
            k = ("e", f)
        else:
            _, key, n = tok
            k = ("d", key)
        if self.seen[eng].get(k, 0) >= n:
            return
        if n > waits.get(k, 0):
            waits[k] = n

    def _deps(self, eng, reads, writes, ww_ok=False):
        waits = {}
        for k in reads:
            self._need(eng, k.lw, waits, True)
        for k in writes:
            if not (ww_ok and k.lw is not None and k.lw[0] == "e" and k.lw[1] == eng):
                self._need(eng, k.lw, waits, False)
            for t in k.rd:
                self._need(eng, t, waits, False)
        for k, n in waits.items():
            self.seen[eng][k] = n
            sem = self.esem[k[1]] if k[0] == "e" else k[1].sem
            self.prog[eng].append(("w", sem, n))

    def _commit(self, tok, reads, writes):
        for k in reads:
            if k.excl:
                k.lw = tok
                k.rd = []
            elif not k.const:
                k.rd.append(tok)
        for k in writes:
            k.lw = tok
            k.rd = []

    def _co_hook(self):
        co = self.co
        if co is None:
            return False
        if threading.current_thread() is co.th:
            co.hook()
            return False
        return True

    def op(self, eng, fn, reads=(), writes=(), inc=True, ww_ok=False):
        main_with_co = self._co_hook()
        tok = self._op(eng, fn, reads, writes, inc, ww_ok)
        if main_with_co:
            self.co.step(1)
        return tok

    def _op(self, eng, fn, reads=(), writes=(), inc=True, ww_ok=False):
        self._deps(eng, reads, writes, ww_ok)
        self.n_ins += 1
        if inc:
            self.cnt[eng] += 1
            tok = ("e", eng, self.cnt[eng])
            self.prog[eng].append(("i", fn, self.esem[eng], 1))
        else:
            tok = ("e", eng, self.cnt[eng] + 1)
            self.prog[eng].append(("i", fn, None, 0))
        self._commit(tok, reads, writes)
        return tok

    def dma(self, eng, out, in_, owner, reads=(), writes=(), **kw):
        self._co_hook()
        self._deps(eng, reads, writes)
        if owner.sem is None:
            owner.sem = self.nc.alloc_semaphore(name="d_" + owner.name)
        owner.semcnt += 16
        tok = ("d", owner, owner.semcnt)
        self.n_ins += 1
        self.prog[eng].append(
            ("i", lambda e, o=out, i=in_, kw=kw: e.dma_start(out=o, in_=i, **kw), owner.sem, 16))
        self._commit(tok, reads, writes)
        return tok

    def wait_all(self, eng, toks):
        waits = {}
        for t in toks:
            self._need(eng, t, waits, True)
        for k, n in waits.items():
            self.seen[eng][k] = n
            sem = self.esem[k[1]] if k[0] == "e" else k[1].sem
            self.prog[eng].append(("w", sem, n))

    def emit(self):
        progs = self.prog

        def run(e, lst):
            for it in lst:
                if it[0] == "w":
                    e.wait_ge(it[1], it[2])
                else:
                    ins = it[1](e)
                    if it[2] is not None:
                        ins.then_inc(it[2], it[3])

        with self.nc.Block() as block:
            @block.tensor
            def _(e):
                run(e, progs["pe"])

            @block.scalar
            def _(e):
                run(e, progs["act"])

            @block.vector
            def _(e):
                run(e, progs["dve"])

            @block.gpsimd
            def _(e):
                run(e, progs["pool"])

            @block.sync
            def _(e):
                run(e, progs["sp"])


class Tl:
    def __init__(self, nc, name, shape, dt, psum=False, const=False):
        if psum:
            self.t = nc.alloc_psum_tensor("p_" + name, shape, dt)
        else:
            self.t = nc.alloc_sbuf_tensor("s_" + name, shape, dt)
        self.k = Key(name, excl=psum, const=const)

    def __getitem__(self, idx):
        return self.t[idx]


def _consts():
    p = np.arange(128)
    same = (p[:, None] // 64) == (p[None, :] // 64)
    c = {}
    c["ident"] = np.eye(128, dtype=np.float32)
    c["causal"] = (p[:, None] <= p[None, :]).astype(np.float32)
    c["mcum"] = (same & (p[:, None] <= p[None, :])).astype(np.float32)
    c["mblk"] = same.astype(np.float32)
    c["mmid"] = (same & ((p[:, None] % 64) <= 32)).astype(np.float32)
    c["chunkind"] = np.stack([(p < 64), (p >= 64)], axis=1).astype(np.float32)
    invf = (10000.0 ** (-np.arange(0, 64, 2, dtype=np.float32) / 64)).astype(np.float32)
    c["invf"] = np.tile(invf[None, :], (128, 1)).astype(np.float32)
    order = ["ident", "causal", "mcum", "mblk", "mmid", "chunkind", "invf"]
    offs = {}
    cols = 0
    for n in order:
        offs[n] = (cols, c[n].shape[1])
        cols += c[n].shape[1]
    blob = np.concatenate([c[n] for n in order], axis=1).astype(np.float32)
    return blob, offs


def build_program(NB, SEQ, do_peer=True, dbg=(), stage="full", chain_casts=True):
    STAGES = ["pro", "ada", "norm", "qkv", "proj", "attn", "gla", "full"]
    slevel = STAGES.index(stage)
    NT = SEQ // 128
    ST = 2
    NST = NT // ST
    NTOK = NB * SEQ
    nc = bass.Bass("TRN2", target_bir_lowering=False)
    S = Sched(nc)
    cblob, coffs = _consts()
    CW = cblob.shape[1]

    def din(name, shape, dt=F32):
        return nc.dram_tensor(name, list(shape), dt, kind="ExternalInput").ap()

    x_d = din("x", [NTOK, D])
    cT_d = din("cT", [D, NB])
    pos_d = din("posT", [128, NB * NT], I32)
    wada_d = din("w_ada", [D, 6 * D])
    badaT_d = din("b_adaT", [128, 48])
    bada_d = din("b_ada", [1, 6 * D])
    g1T_d = din("g1T", [128, 8])
    g2T_d = din("g2T", [128, 8])
    win_d = din("w_in", [D, 3088])
    wout_d = din("w_out", [D, D])
    wq_d = din("w_query", [D, 2048])
    bqT_d = din("b_queryT", [128, 16])
    k1T_d = din("keys1T", [128, 128])
    k2T_d = din("keys2T", [128, 128])
    uT_d = din("uT", [D, NEXP])
    v_d = din("ev", [NEXP, D])
    vec64_d = din("vec64", [1, 6 * 64])
    vec128_d = din("vec128", [1, 2 * 128])
    wg2_d = din("w_gate2", [16, 256])
    bg_d = din("b_gate", [1, 256])
    cst_d = din("consts", [128, CW])
    y_d = nc.dram_tensor("y", [NTOK, D], F32, kind="ExternalOutput").ap()
    dbg_d = {n: nc.dram_tensor("dbg_" + n, list(shp), F32, kind="ExternalOutput").ap() for n, shp in dbg}

    win_s = nc.dram_tensor("win_s", [D, 3088], BF16, kind="Internal").ap()
    wout_s = nc.dram_tensor("wout_s", [D, D], BF16, kind="Internal").ap()
    wq_s = nc.dram_tensor("wq_s", [D, 2048], BF16, kind="Internal").ap()
    uT_s = nc.dram_tensor("uT_s", [D, NEXP], BF16, kind="Internal").ap()
    v_s = nc.dram_tensor("v_s", [NEXP, D], BF16, kind="Internal").ap()
    k_win_s, k_wout_s, k_wq_s, k_uT_s, k_v_s = (Key(n, const=True) for n in ("kwin", "kwout", "kwq", "kuT", "kv"))

    def sb(name, shape, dt=F32, const=False):
        return Tl(nc, name, shape, dt, const=const)

    cst = sb("cst", [128, CW], F32, const=True)
    identb = sb("identb", [128, 128], BF16, const=True)
    causb = sb("causb", [128, 128], BF16, const=True)
    onesr = sb("onesr", [1, 128], F32, const=True)
    cT = sb("cT", [128, 8, NB], F32, const=True)
    posi = sb("posi", [128, NB * NT], I32, const=True)
    posf = sb("posf", [128, NB * NT], F32, const=True)
    badaT = sb("badaT", [128, 48], F32, const=True)
    g1T = sb("g1T", [128, 8], F32, const=True)
    g2T = sb("g2T", [128, 8], F32, const=True)
    bqT = sb("bqT", [128, 16], F32, const=True)
    k1T = sb("k1T", [128, 128], BF16, const=True)
    k2T = sb("k2T", [128, 128], BF16, const=True)
    kst = sb("kst", [128, 128], F32)
    v64 = sb("v64", [128, 6 * 64], F32, const=True)
    v128 = sb("v128", [128, 256], F32, const=True)
    qgB = sb("qgB", [128, 64], F32, const=True)
    subgB = sb("subgB", [128, 128], F32, const=True)
    neglam = sb("neglam", [128, 1], F32, const=True)
    lamt = sb("lamt", [128, 64], F32)
    lams = sb("lams", [128, 4], F32)
    wg2 = sb("wg2", [16, 256], F32, const=True)
    bgr = sb("bgr", [1, 256], F32, const=True)
    cvals = sb("cvals", [128, 4], F32, const=True)

    def C(name):
        o, w = coffs[name]
        return cst[:, o:o + w]

    sT = sb("sT", [128, 8], F32)
    modT = sb("modT", [128, 4, 8], F32)
    gm1 = sb("gm1", [128, 8], F32)
    gm2 = sb("gm2", [128, 8], F32)
    gt1B = sb("gt1B", [128, D], F32)
    gt2B = sb("gt2B", [128, D], F32)
    wst = [sb("wst%d" % i, [128, 8, 128], F32) for i in range(1)]

    NRING = 4
    ring = [sb("ring%d" % i, [128, 8, 512], BF16) for i in range(NRING)]
    rc = [0]

    def ring_next():
        r = ring[rc[0] % NRING]
        rc[0] += 1
        return r

    KT = [sb("KT%d" % i, [128, 4, 128], BF16) for i in range(NT)]
    VC = [sb("VC%d" % i, [128, 4, 130], BF16) for i in range(NT)]
    Sst = [sb("Sst%d" % i, [128, 128], F32) for i in range(2)]

    x1 = sb("x1", [128, ST, D], F32)
    xk = [Key("x1_%d" % i) for i in range(ST)]
    junk = sb("junk", [128, D], BF16)
    xn = sb("xn", [128, D], BF16)
    st1 = sb("st1", [128, 4], F32)
    hT = sb("hT", [128, 8, 128], BF16)
    h2T = sb("h2T", [128, 8, ST * 128], BF16)
    sq = sb("sq", [128, 512], F32)
    ss8 = sb("ss8", [128, 8], F32)
    qn = sb("qn", [128, 512], F32)
    rt = [sb("rt%d" % i, [128, 256], F32) for i in range(2)]
    qr = sb("qr", [128, 512], BF16)
    qT = sb("qT", [128, 4, 128], BF16)
    ang = sb("ang", [128, 32], F32)
    ang2 = sb("ang2", [128, 32], F32)
    ang3 = sb("ang3", [128, 32], F32)
    angi = sb("angi", [128, 32], I32)
    sinT = sb("sinT", [128, 32], F32)
    cosT = sb("cosT", [128, 32], F32)
    PT = [sb("PT%d" % i, [128, 512], BF16) for i in range(2)]
    osb = sb("osb", [128, 128], F32)
    rz = sb("rz", [128, 4], F32)
    ybf = sb("ybf", [128, D], BF16)
    ykey2 = Key("ybf_gla")
    junk2 = sb("junk2", [128, 128], BF16)
    rz2 = sb("rz2", [128, 4], F32)
    osb2 = sb("osb2", [128, 128], F32)
    yT = sb("yT", [128, 8, 128], BF16)
    gqk = sb("gqk", [128, 512], F32)
    gv = sb("gv", [128, 512], F32)
    sr = sb("sr", [128, 512], F32)
    ggT = sb("ggT", [16, 128], F32)
    la = sb("la", [128, 256], F32)
    gl = [sb("gl%d" % i, [128, 256], F32) for i in range(8)]
    dec = sb("dec", [128, 2, 2], F32)
    T3 = sb("T3", [128, 3, 128], F32)
    ATs = sb("ATs", [128, 128], F32)

    psT = Tl(nc, "psT", [128, 1024], BF16, psum=True)
    psP = [Tl(nc, "psP%d" % i, [128, 512], F32, psum=True) for i in range(2)]
    psS = [Tl(nc, "psS%d" % i, [128, 512], F32, psum=True) for i in range(2)]
    psO = Tl(nc, "psO", [128, 512], F32, psum=True)
    psG = Tl(nc, "psG", [128, 512], F32, psum=True)
    psG2 = Tl(nc, "psG2", [128, 512], F32, psum=True)

    def mm(out, lhsT, rhs, start, stop, reads, writes, inc=True, skip=False):
        if skip:
            S.op("pe", lambda e: e.matmul(out, lhsT=lhsT, rhs=rhs, start=start, stop=stop, skip_group_check=True),
                 reads, writes, inc)
        else:
            S.op("pe", lambda e: e.matmul(out, lhsT=lhsT, rhs=rhs, start=start, stop=stop), reads, writes, inc)

    def tr(out, in_, ident, reads, writes, inc=True):
        S.op("pe", lambda e: e.transpose(out, in_, ident), reads, writes, inc)

    def act(out, in_, func, reads, writes, bias=None, scale=None, accum_out=None, ww_ok=False):
        kw = {}
        if bias is not None:
            kw["bias"] = bias
        if scale is not None:
            kw["scale"] = scale
        if accum_out is not None:
            kw["accum_out"] = accum_out
        S.op("act", lambda e: e.activation(out=out, in_=in_, func=func, **kw), reads, writes, ww_ok=ww_ok)

    def tt(eng, out, in0, in1, op, reads, writes):
        S.op(eng, lambda e: e.tensor_tensor(out=out, in0=in0, in1=in1, op=op), reads, writes)

    def ts(eng, out, in0, s1, s2, op0, op1, reads, writes):
        if s2 is None:
            S.op(eng, lambda e: e.tensor_scalar(out=out, in0=in0, scalar1=s1, scalar2=None, op0=op0), reads, writes)
        else:
            S.op(eng, lambda e: e.tensor_scalar(out=out, in0=in0, scalar1=s1, scalar2=s2, op0=op0, op1=op1),
                 reads, writes)

    def stt(eng, out, in0, scalar, in1, op0, op1, reads, writes):
        S.op(eng, lambda e: e.scalar_tensor_tensor(out=out, in0=in0, scalar=scalar, in1=in1, op0=op0, op1=op1),
             reads, writes)

    def cp(eng, out, in_, reads, writes):
        if eng == "act":
            S.op(eng, lambda e: e.activation(out=out, in_=in_, func=AF.Identity), reads, writes)
        else:
            S.op(eng, lambda e: e.tensor_copy(out=out, in_=in_), reads, writes)

    def red(eng, out, in_, op, reads, writes):
        S.op(eng, lambda e: e.tensor_reduce(out=out, in_=in_, axis=AX.X, op=op), reads, writes)

    def rsqrt_mean(dst, src, n, keys):
        act(dst, src, AF.Ln, list(keys) + [cvals.k], keys, bias=cvals[:, 1:2], scale=1.0 / n)
        act(dst, dst, AF.Exp, keys, keys, scale=-0.5)

    def ld(out, in_, tile, reads=(), **kw):
        return S.dma("sp", out, in_, owner=tile.k, reads=reads, writes=[tile.k], **kw)

    def cast_copy(dst, src, key, rows, cols, cchunk):
        for r0 in range(0, rows, 128):
            for c0 in range(0, cols, cchunk):
                c1 = min(cols, c0 + cchunk)
                if chain_casts and key.lw is not None:
                    S.wait_all("pool", [key.lw])
                S.dma("pool", dst[r0:r0 + 128, c0:c1], src[r0:r0 + 128, c0:c1], owner=key, writes=[key])

    cast_copy(win_s, win_d, k_win_s, D, 3088, 3088)
    cast_copy(wout_s, wout_d, k_wout_s, D, D, D)
    cast_copy(wq_s, wq_d, k_wq_s, D, 2048, 2048)
    if do_peer:
        cast_copy(uT_s, uT_d, k_uT_s, D, NEXP, 4096)
        cast_copy(v_s, v_d, k_v_s, NEXP, D, D)

    ld(cst[:, :], cst_d, cst)
    ld(cT[:, :, :], cT_d.rearrange("(k p) b -> p k b", p=128), cT, allow_slow_non_contiguous=True)
    ld(posi[:, :], pos_d, posi)
    ld(badaT[:, :], badaT_d, badaT)
    ld(g1T[:, :], g1T_d, g1T)
    ld(g2T[:, :], g2T_d, g2T)
    ld(bqT[:, :], bqT_d, bqT)
    ld(v64[:, :], vec64_d[0:1, :].to_broadcast([128, 384]), v64)
    ld(v128[:, :], vec128_d[0:1, :].to_broadcast([128, 256]), v128)
    ld(wg2[:, :], wg2_d, wg2)
    ld(bgr[:, :], bg_d, bgr)
    S.op("dve", lambda e: e.memset(onesr[:, :], 1.0), (), [onesr.k])
    S.op("dve", lambda e: e.memset(cvals[:, 0:1], -math.pi), (), [cvals.k])
    S.op("dve", lambda e: e.memset(cvals[:, 1:2], EPS), (), [cvals.k])
    S.op("dve", lambda e: e.memset(cvals[:, 2:3], 1.0), (), [cvals.k])
    S.op("dve", lambda e: e.memset(cvals[:, 3:4], 0.0), (), [cvals.k])
    cp("dve", identb[:, :], C("ident"), [cst.k], [identb.k])
    cp("dve", causb[:, :], C("causal"), [cst.k], [causb.k])
    cp("dve", posf[:, :], posi[:, :], [posi.k], [posf.k])
    ld(kst[:, :], k1T_d, kst)
    cp("dve", k1T[:, :], kst[:, :], [kst.k], [k1T.k])
    ld(kst[:, :], k2T_d, kst)
    cp("dve", k2T[:, :], kst[:, :], [kst.k], [k2T.k])
    ts("dve", qgB[:, :], v64[:, 0:64], 0.125, None, ALU.mult, None, [v64.k], [qgB.k])
    ts("dve", subgB[:, :], v128[:, 0:128], 0.8, None, ALU.mult, None, [v128.k], [subgB.k])
    tt("dve", lamt[:, :], v64[:, 128:192], v64[:, 192:256], ALU.mult, [v64.k], [lamt.k])
    red("dve", lams[:, 0:1], lamt[:, :], ALU.add, [lamt.k], [lams.k])
    tt("dve", lamt[:, :], v64[:, 256:320], v64[:, 320:384], ALU.mult, [v64.k, lams.k], [lamt.k])
    red("dve", lams[:, 1:2], lamt[:, :], ALU.add, [lamt.k], [lams.k])
    act(lams[:, 2:4], lams[:, 0:2], AF.Exp, [lams.k], [lams.k])
    tt("dve", lams[:, 0:1], lams[:, 3:4], lams[:, 2:3], ALU.subtract, [lams.k], [lams.k])
    ts("dve", neglam[:, :], lams[:, 0:1], -0.2, None, ALU.add, None, [lams.k], [neglam.k])
    for i in range(NT):
        S.op("pool", lambda e, i=i: e.memset(VC[i][:, :, :], 1.0), (), [VC[i].k])

    def adaln(b):
        act(sT[:, :], cT[:, :, b], AF.Silu, [cT.k], [sT.k])
        ld(gt1B[:, :], bada_d[0:1, 2 * D:3 * D].to_broadcast([128, D]), gt1B)
        ld(gt2B[:, :], bada_d[0:1, 5 * D:6 * D].to_broadcast([128, D]), gt2B)
        wi = 0
        for sec in range(6):
            for q8 in range(8):
                w = wst[0]
                wi += 1
                c0 = sec * D + q8 * 128
                ld(w[:, :, :], wada_d[:, c0:c0 + 128].rearrange("(k p) n -> p k n", p=128), w)
                ps = psP[wi % 2]
                if sec in (2, 5):
                    for kc in range(8):
                        mm(ps[:, 0:128], sT[:, kc:kc + 1].to_broadcast([128, 128]), w[:, kc, :],
                           kc == 0, kc == 7, [sT.k, w.k], [ps.k], inc=(kc == 7))
                    g = gt1B if sec == 2 else gt2B
                    tt("dve", g[:, q8 * 128:(q8 + 1) * 128], g[:, q8 * 128:(q8 + 1) * 128], ps[:, 0:128], ALU.add,
                       [ps.k, g.k], [g.k])
                else:
                    mi = {0: 0, 1: 1, 3: 2, 4: 3}[sec]
                    for kc in range(8):
                        mm(ps[:, 0:1], w[:, kc, :], sT[:, kc:kc + 1],
                           kc == 0, kc == 7, [sT.k, w.k], [ps.k], inc=(kc == 7))
                    tt("dve", modT[:, mi, q8:q8 + 1], ps[:, 0:1],
                       badaT[:, sec * 8 + q8:sec * 8 + q8 + 1], ALU.add, [ps.k, badaT.k], [modT.k])
        stt("dve", gm1[:, :], modT[:, 1, :], 1.0, g1T[:, :], ALU.add, ALU.mult, [modT.k, g1T.k], [gm1.k])
        stt("dve", gm2[:, :], modT[:, 3, :], 1.0, g2T[:, :], ALU.add, ALU.mult, [modT.k, g2T.k], [gm2.k])

    def norm_T(xap, xkey, gm, shi, dst, dcol):
        act(junk[:, :], xap, AF.Square, [xkey], [junk.k, st1.k], accum_out=st1[:, 0:1])
        rsqrt_mean(st1[:, 2:3], st1[:, 0:1], D, [st1.k])
        ts("dve", xn[:, :], xap, st1[:, 2:3], None, ALU.mult, None, [xkey, st1.k], [xn.k])
        for j in range(8):
            tr(psT[:, j * 128:(j + 1) * 128], xn[:, j * 128:(j + 1) * 128], identb[:, :],
               [xn.k, identb.k], [psT.k], inc=(j == 7))
        for j in range(8):
            act(dst[:, j, dcol:dcol + 128], psT[:, j * 128:(j + 1) * 128], AF.Identity,
                [psT.k, gm.k, modT.k], [dst.k], bias=modT[:, shi, j:j + 1], scale=gm[:, j:j + 1])

    def rope_tables(col):
        C1 = 6.28125
        C2 = 2.0 * math.pi - C1
        ts("dve", ang[:, :], C("invf"), posf[:, col:col + 1], None, ALU.mult, None, [cst.k, posf.k], [ang.k])
        for (shift, dst) in ((0.0, sinT), (0.5 * math.pi, cosT)):
            ts("dve", ang2[:, :], ang[:, :], shift, 1.0 / (2.0 * math.pi), ALU.add, ALU.mult, [ang.k], [ang2.k])
            cp("dve", angi[:, :], ang2[:, :], [ang2.k], [angi.k])
            cp("dve", ang2[:, :], angi[:, :], [angi.k], [ang2.k])
            stt("dve", ang3[:, :], ang2[:, :], -C1, ang[:, :], ALU.mult, ALU.add, [ang2.k, ang.k], [ang3.k])
            stt("dve", ang3[:, :], ang2[:, :], -C2, ang3[:, :], ALU.mult, ALU.add, [ang2.k, ang3.k], [ang3.k])
            ts("dve", ang3[:, :], ang3[:, :], shift, math.pi, ALU.add, ALU.min, [ang3.k], [ang3.k])
            ts("dve", ang3[:, :], ang3[:, :], -math.pi, None, ALU.max, None, [ang3.k], [ang3.k])
            act(dst[:, :], ang3[:, :], AF.Sin, [ang3.k], [dst.k])

    def proj_block(c0, ncols, ps, lhs=None):
        r = ring_next()
        ld(r[:, :, 0:ncols], win_s[:, c0:c0 + ncols].rearrange("(k p) n -> p k n", p=128), r, reads=[k_win_s])
        for kc in range(8):
            mm(ps[:, 0:ncols], hT[:, kc, :], r[:, kc, 0:ncols], kc == 0, kc == 7,
               [hT.k, r.k], [ps.k], inc=(kc == 7))
        return r

    def qk_post(ps, gB, dstT, dkey):
        act(sq[:, :], ps[:, :], AF.Square, [ps.k], [sq.k])
        red("dve", ss8[:, :], sq[:, :].rearrange("p (g d) -> p g d", d=64), ALU.add, [sq.k], [ss8.k])
        rsqrt_mean(ss8[:, :], ss8[:, :], 64, [ss8.k])
        q3 = qn[:, :].rearrange("p (g d) -> p g d", d=64)
        tt("dve", q3, ps[:, :].rearrange("p (g d) -> p g d", d=64),
           ss8[:, :].unsqueeze(2).to_broadcast([128, 8, 64]), ALU.mult, [ps.k, ss8.k], [qn.k])
        tt("dve", q3, q3, gB.unsqueeze(1).to_broadcast([128, 8, 64]), ALU.mult, [qn.k, v64.k, qgB.k], [qn.k])
        x1v = q3[:, :, 0:32]
        x2v = q3[:, :, 32:64]
        cb = cosT[:, :].unsqueeze(1).to_broadcast([128, 8, 32])
        sbb = sinT[:, :].unsqueeze(1).to_broadcast([128, 8, 32])
        r0, r1 = (t[:, :].rearrange("p (g d) -> p g d", d=32) for t in rt)
        qr3 = qr[:, :].rearrange("p (g d) -> p g d", d=64)
        tt("dve", r0, x1v, cb, ALU.mult, [qn.k, cosT.k], [rt[0].k])
        tt("dve", r1, x2v, sbb, ALU.mult, [qn.k, sinT.k], [rt[1].k])
        tt("dve", qr3[:, :, 0:32], r0, r1, ALU.subtract, [rt[0].k, rt[1].k], [qr.k])
        tt("dve", r0, x2v, cb, ALU.mult, [qn.k, cosT.k], [rt[0].k])
        tt("dve", r1, x1v, sbb, ALU.mult, [qn.k, sinT.k], [rt[1].k])
        tt("dve", qr3[:, :, 32:64], r0, r1, ALU.add, [rt[0].k, rt[1].k], [qr.k])
        for h in range(4):
            tr(psT[:, h * 128:(h + 1) * 128], qr[:, h * 128:(h + 1) * 128], identb[:, :],
               [qr.k, identb.k], [psT.k], inc=(h == 3))
        cp("dve", dstT[:, :, :], psT[:, 0:512].rearrange("p (h t) -> p h t", t=128),
           [psT.k], [dkey])

    dbg_toks = []

    def dump(name, ap, key):
        if name in dbg_d:
            dbg_toks.append(S.dma("sp", dbg_d[name], ap, owner=key, reads=[key]))

    def mixer_tile(b, i, slot):
        col = b * NT + i
        tok0 = b * SEQ + i * 128
        xs = x1[:, slot, :]
        xkey = xk[slot]
        S.dma("sp", xs, x_d[tok0:tok0 + 128, :], owner=xkey, writes=[xkey])
        if slevel < 2:
            return
        norm_T(xs, xkey, gm1, 0, hT, 0)
        rope_tables(col)
        if slevel < 3:
            return
        proj_block(0, 512, psP[0])
        qk_post(psP[0], qgB[:, :], qT, qT.k)
        proj_block(512, 512, psP[1])
        qk_post(psP[1], v64[:, 64:128], KT[i], KT[i].k)
        proj_block(1024, 512, psP[0])
        cp("act", VC[i][:, :, 0:128], psP[0][:, :].rearrange("p (h e) -> p h e", e=128), [psP[0].k], [VC[i].k])
        if slevel < 4:
            return
        proj_block(1536, 512, psP[1])
        cp("act", gqk[:, :], psP[1][:, :], [psP[1].k], [gqk.k])
        proj_block(2048, 512, psP[0])
        cp("act", gv[:, :], psP[0][:, :], [psP[0].k], [gv.k])
        proj_block(2560, 512, psP[1])
        act(sr[:, :], psP[1][:, :], AF.Silu, [psP[1].k], [sr.k])
        r = ring_next()
        ld(r[:, :, 0:16], win_s[:, 3072:3088].rearrange("(k p) n -> p k n", p=128), r, reads=[k_win_s])
        for kc in range(8):
            mm(psP[0][0:16, 0:128], r[:, kc, 0:16], hT[:, kc, :], kc == 0, kc == 7,
               [hT.k, r.k], [psP[0].k], inc=(kc == 7))
        cp("act", ggT[:, :], psP[0][0:16, 0:128], [psP[0].k], [ggT.k])

        if slevel < 5:
            return

        def attn_part():
            nkb = i + 1
            ngrp = (nkb + 3) // 4
            sidx = 0
            for h in range(4):
                for m in range(2):
                    pr = slice(m * 64, (m + 1) * 64)
                    for g in range(ngrp):
                        j0 = g * 4
                        nj = min(4, nkb - j0)
                        pss = psS[sidx % 2]
                        pt = PT[sidx % 2]
                        sidx += 1
                        for jj in range(nj):
                            j = j0 + jj
                            mm(pss[:, jj * 128:(jj + 1) * 128], KT[j][pr, h, :], qT[pr, h, :], True, True,
                               [KT[j].k, qT.k], [pss.k], inc=(jj == nj - 1))
                        act(pt[:, 0:nj * 128], pss[:, 0:nj * 128], AF.Exp, [pss.k], [pt.k])
                        if j0 + nj - 1 == i:
                            dsl = slice((nj - 1) * 128, nj * 128)
                            tt("pool", pt[:, dsl], pt[:, dsl], causb[:, :], ALU.mult, [pt.k, causb.k], [pt.k])
                        for jj in range(nj):
                            j = j0 + jj
                            mm(psO[:, m * 129:(m + 1) * 129], pt[:, jj * 128:(jj + 1) * 128], VC[j][:, h, 0:129],
                               j == 0, j == i, [pt.k, VC[j].k], [psO.k], inc=(jj == nj - 1))
                S.op("dve", lambda e: e.reciprocal(out=rz[:, 0:1], in_=psO[:, 128:129]), [psO.k], [rz.k])
                S.op("dve", lambda e: e.reciprocal(out=rz[:, 1:2], in_=psO[:, 257:258]), [psO.k], [rz.k])
                tt("dve", rz[:, 2:3], rz[:, 1:2], neglam[:, :], ALU.mult, [rz.k, neglam.k], [rz.k])
                ts("dve", osb[:, :], psO[:, 0:128], rz[:, 0:1], None, ALU.mult, None, [psO.k, rz.k], [osb.k])
                stt("dve", osb[:, :], psO[:, 129:257], rz[:, 2:3], osb[:, :], ALU.mult, ALU.add,
                    [psO.k, rz.k, osb.k], [osb.k])
                act(junk[:, 0:128], osb[:, :], AF.Square, [osb.k], [junk.k, rz.k], accum_out=rz[:, 3:4])
                rsqrt_mean(rz[:, 3:4], rz[:, 3:4], 128, [rz.k])
                stt("dve", ybf[:, h * 128:(h + 1) * 128], osb[:, :], rz[:, 3:4], subgB[:, :], ALU.mult, ALU.mult,
                    [osb.k, rz.k, subgB.k], [ybf.k])


        def gla_part():
            mm(psG[:, 0:256], ggT[:, :], wg2[:, :], True, False, [ggT.k, wg2.k], [psG.k], inc=False)
            mm(psG[:, 0:256], onesr[:, :], bgr[:, :], False, True, [onesr.k, bgr.k], [psG.k])
            act(la[:, :], psG[:, 0:256], AF.Exp, [psG.k], [la.k], scale=-1.0)
            act(la[:, :], la[:, :], AF.Ln, [la.k, cvals.k], [la.k], bias=cvals[:, 2:3])
            ts("dve", la[:, :], la[:, :], -1.0 / 16, None, ALU.mult, None, [la.k], [la.k])
            if GSUB < 1:
                return
            mm(psG[:, 0:256], C("mcum"), la[:, :], True, True, [cst.k, la.k], [psG.k], inc=False)
            mm(psG[:, 256:512], C("mmid"), la[:, :], True, True, [cst.k, la.k], [psG.k])
            mm(psG2[:, 0:256], C("mblk"), la[:, :], True, True, [cst.k, la.k], [psG2.k], inc=False)
            for hp in range(2):
                for hh in range(2):
                    h = hp * 2 + hh
                    mm(psG2[hh * 64:(hh + 1) * 64, 256 + hp * 2:256 + hp * 2 + 2], la[:, h * 64:(h + 1) * 64],
                       C("chunkind"), True, True, [la.k, cst.k], [psG2.k], inc=(hp == 1 and hh == 1))
            if GSUB < 2:
                return
            bc, d1, eq, ek, eb, d2, ed, qg = gl
            cp("dve", bc[:, :], psG[:, 0:256], [psG.k], [bc.k])
            tt("dve", d1[:, :], bc[:, :], psG[:, 256:512], ALU.subtract, [bc.k, psG.k], [d1.k])
            tt("dve", d2[:, :], psG2[:, 0:256], bc[:, :], ALU.subtract, [bc.k, psG2.k], [d2.k])
            act(dec[:, :, :], psG2[:, 256:260].rearrange("p (a c) -> p a c", c=2), AF.Exp, [psG2.k], [dec.k])
            act(eq[:, :], d1[:, :], AF.Exp, [d1.k], [eq.k])
            act(ek[:, :], d1[:, :], AF.Exp, [d1.k], [ek.k], scale=-1.0)
            act(eb[:, :], bc[:, :], AF.Exp, [bc.k], [eb.k])
            act(ed[:, :], d2[:, :], AF.Exp, [d2.k], [ed.k])
            stt("dve", eq[:, :], gqk[:, 0:256], 0.125, eq[:, :], ALU.mult, ALU.mult, [gqk.k, eq.k], [eq.k])
            tt("dve", ek[:, :], gqk[:, 256:512], ek[:, :], ALU.mult, [gqk.k, ek.k], [ek.k])
            stt("dve", eb[:, :], gqk[:, 0:256], 0.125, eb[:, :], ALU.mult, ALU.mult, [gqk.k, eb.k], [eb.k])
            tt("dve", ed[:, :], gqk[:, 256:512], ed[:, :], ALU.mult, [gqk.k, ed.k], [ed.k])
            ci = C("chunkind")
            ts("dve", d1[:, :], ed[:, :], ci[:, 0:1], None, ALU.mult, None, [ed.k, cst.k, d1.k], [d1.k])
            ts("dve", d2[:, :], ed[:, :], ci[:, 1:2], None, ALU.mult, None, [ed.k, cst.k, d2.k], [d2.k])
            if GSUB < 3:
                return
            for hp in range(2):
                cs = slice(hp * 128, (hp + 1) * 128)
                mm(psG[:, 0:128], eq[:, cs], C("ident"), True, True, [eq.k, cst.k], [psG.k], inc=False)
                mm(psG[:, 128:256], ek[:, cs], C("ident"), True, True, [ek.k, cst.k], [psG.k], inc=False)
                mm(psG[:, 256:384], eb[:, cs], C("ident"), True, True, [eb.k, cst.k], [psG.k])
                cp("act", T3[:, :, :], psG[:, 0:384].rearrange("p (a t) -> p a t", t=128), [psG.k], [T3.k])
                if GSUB < 4:
                    continue
                st = Sst[hp]
                if i == 0:
                    S.op("dve", lambda e, st=st: e.memset(st[:, :], 0.0), (), [st.k])
                for hh in range(2):
                    h = hp * 2 + hh
                    pr = slice(hh * 64, (hh + 1) * 64)
                    vcols = slice(h * 128, (h + 1) * 128)
                    for c, kdm in enumerate((d1, d2)):
                        mm(psG2[pr, c * 128:(c + 1) * 128], kdm[:, h * 64:(h + 1) * 64], gv[:, vcols], True, True,
                           [kdm.k, gv.k], [psG2.k], inc=False)
                    mm(psG2[:, 256:384], T3[pr, 1, :], T3[pr, 0, :], True, True, [T3.k], [psG2.k])
                    tt("dve", ATs[:, :], psG2[:, 256:384], C("mcum"), ALU.mult, [psG2.k, cst.k], [ATs.k])
                    if GSUB < 5:
                        continue
                    mm(psP[0][:, 0:128], ATs[:, :], gv[:, vcols], True, False, [ATs.k, gv.k], [psP[0].k], inc=False, skip=True)
                    mm(psP[0][0:64, 0:128], T3[pr, 2, 0:64], st[pr, :], False, False, [T3.k, st.k], [psP[0].k], inc=True,
                       skip=True)
                    if GSUB < 6:
                        continue
                    stt("dve", st[pr, :], st[pr, :], dec[pr, hp, 0:1], psG2[pr, 0:128], ALU.mult, ALU.add,
                        [st.k, dec.k, psG2.k], [st.k])
                    mm(psP[0][64:128, 0:128], T3[pr, 2, 64:128], st[pr, :], False, True, [T3.k, st.k], [psP[0].k], skip=True)
                    if GSUB < 7:
                        continue
                    stt("dve", st[pr, :], st[pr, :], dec[pr, hp, 1:2], psG2[pr, 128:256], ALU.mult, ALU.add,
                        [st.k, dec.k, psG2.k], [st.k])
                    if GSUB < 8:
                        continue
                    act(junk2[:, 0:128], psP[0][:, 0:128], AF.Square, [psP[0].k], [junk2.k, rz2.k], accum_out=rz2[:, 3:4])
                    rsqrt_mean(rz2[:, 3:4], rz2[:, 3:4], 128, [rz2.k])
                    stt("dve", osb2[:, :], psP[0][:, 0:128], rz2[:, 3:4], v128[:, 128:256], ALU.mult, ALU.mult,
                        [psP[0].k, rz2.k, v128.k], [osb2.k])
                    tt("dve", ybf[:, 512 + h * 128:512 + (h + 1) * 128], osb2[:, :], sr[:, vcols], ALU.mult,
                       [osb2.k, sr.k], [ykey2])


        if slevel >= 6 and INTERLEAVE:
            co = Co(gla_part)
            S.co = co
            attn_part()
            S.co = None
            co.finish()
        else:
            attn_part()
            if slevel >= 6:
                gla_part()
        if slevel < 7:
            return
        for j in range(8):
            tr(psT[:, j * 128:(j + 1) * 128], ybf[:, j * 128:(j + 1) * 128], identb[:, :],
               [ybf.k, ykey2, identb.k], [psT.k], inc=(j == 7))
        cp("act", yT[:, :, :], psT[:, :].rearrange("p (j t) -> p j t", t=128), [psT.k], [yT.k])
        for half in range(2):
            r = ring_next()
            ld(r[:, :, :], wout_s[:, half * 512:(half + 1) * 512].rearrange("(k p) n -> p k n", p=128), r, reads=[k_wout_s])
            ps = psP[half]
            for kc in range(8):
                mm(ps[:, :], yT[:, kc, :], r[:, kc, :], kc == 0, kc == 7, [yT.k, r.k], [ps.k],
                   inc=(kc == 7))
            cs = slice(half * 512, (half + 1) * 512)
            tt("dve", sq[:, :], ps[:, :], gt1B[:, cs], ALU.mult, [ps.k, gt1B.k], [sq.k])
            tt("dve", x1[:, slot, cs], sq[:, :], x1[:, slot, cs], ALU.add, [sq.k, xkey], [xkey])
        if do_peer:
            norm_T(xs, xkey, gm2, 2, h2T, slot * 128)

    if do_peer:
        qpc = [sb("qpc%d" % i, [128, ST * 128], BF16) for i in range(2)]
        s1m = sb("s1m", [128, ST, 8, 128], F32)
        s2m = sb("s2m", [128, ST, 8, 128], F32)
        wk = sb("wk", [128, 256], F32)
        v1 = sb("v1", [128, 8, 16], F32)
        v2 = sb("v2", [128, 8, 16], F32)
        c24 = sb("c24", [128, 8, 24], F32)
        thr = sb("thr", [128, 8], F32)
        nrm = sb("nrm", [128, 8], F32)
        ex16 = sb("ex16", [128, 8, 16], F32)
        DG = [sb("DG%d" % t, [128, 8, 128], BF16) for t in range(ST)]
        zbs = [sb("zb%d" % i, [128, 8, 2, 128], BF16) for i in range(NZBUF)]
        zk2s = [Key("zk2_%d" % i) for i in range(NZBUF)]
        zb = sb("candb", [128, 8, 256], F32)
        cand = zb[:, :, :]
        wbs = [sb("wb%d" % i, [128, 8, 2, 128], BF16) for i in range(2)]
        mbs = [sb("mb%d" % i, [128, 8, 2, 128], BF16) for i in range(2)]
        gas = [sb("ga%d" % i, [128, 2, ST * 128], BF16) for i in range(2)]
        GTs = [sb("GT%d" % i, [128, 2, 128], BF16) for i in range(2)]

    out_toks = []

    def top16(src_ap, src_keys, dst, h):
        n = src_ap.shape[-1]
        S.op("dve", lambda e: e.max(out=dst[:, h, 0:8], in_=src_ap), src_keys, [dst.k])
        S.op("dve", lambda e: e.match_replace(out=wk[:, 0:n], in_to_replace=dst[:, h, 0:8], in_values=src_ap,
                                              imm_value=-BIG), list(src_keys) + [dst.k], [wk.k])
        S.op("dve", lambda e: e.max(out=dst[:, h, 8:16], in_=wk[:, 0:n]), [wk.k], [dst.k])

    def peer_supertile(b, st_i):
        T2 = ST * 128
        for blk in range(4):
            r = ring_next()
            ld(r[:, :, :], wq_s[:, blk * 512:(blk + 1) * 512].rearrange("(k p) n -> p k n", p=128), r, reads=[k_wq_s])
            for c4 in range(4):
                cc = blk * 4 + c4
                h, half = cc // 2, cc % 2
                ps = psP[cc % 2]
                qc = qpc[cc % 2]
                for kc in range(8):
                    mm(ps[:, 0:T2], r[:, kc, c4 * 128:(c4 + 1) * 128], h2T[:, kc, :], kc == 0, kc == 7,
                       [r.k, h2T.k], [ps.k], inc=(kc == 7))
                act(qc[:, :], ps[:, 0:T2], AF.Identity, [ps.k, bqT.k], [qc.k], bias=bqT[:, cc:cc + 1])
                pss = psS[cc % 2]
                kT = k1T if half == 0 else k2T
                dstm = s1m if half == 0 else s2m
                for t in range(ST):
                    mm(pss[:, t * 128:(t + 1) * 128], qc[:, t * 128:(t + 1) * 128], kT[:, :], True, True,
                       [qc.k, kT.k], [pss.k], inc=(t == ST - 1))
                cp("dve", dstm[:, :, h, :], pss[:, 0:T2].rearrange("p (t k) -> p t k", k=128), [pss.k], [dstm.k])
        for t in range(ST):
            for h in range(8):
                top16(s1m[:, t, h, :], [s1m.k], v1, h)
                top16(s2m[:, t, h, :], [s2m.k], v2, h)
            tt("dve", cand.rearrange("p h (a b) -> p h a b", b=16),
               v1[:, :, :].unsqueeze(3).to_broadcast([128, 8, 16, 16]),
               v2[:, :, :].unsqueeze(2).to_broadcast([128, 8, 16, 16]), ALU.add, [v1.k, v2.k], [zb.k])
            for h in range(8):
                top16(cand[:, h, :], [zb.k], c24, h)
                S.op("dve", lambda e, h=h: e.match_replace(out=wk[:, :], in_to_replace=c24[:, h, 8:16],
                                                           in_values=wk[:, :], imm_value=-BIG),
                     [wk.k, c24.k], [wk.k])
                S.op("dve", lambda e, h=h: e.max(out=c24[:, h, 16:24], in_=wk[:, :]), [wk.k], [c24.k])
            tt("dve", thr[:, :], c24[:, :, 15], c24[:, :, 16], ALU.add, [c24.k], [thr.k])
            ts("dve", thr[:, :], thr[:, :], 0.5, None, ALU.mult, None, [thr.k], [thr.k])
            tt("dve", ex16[:, :, :], c24[:, :, 0:16], thr[:, :].unsqueeze(2).to_broadcast([128, 8, 16]),
               ALU.subtract, [c24.k, thr.k], [ex16.k])
            act(ex16[:, :, :], ex16[:, :, :], AF.Exp, [ex16.k], [ex16.k])
            red("dve", nrm[:, :], ex16[:, :, :], ALU.add, [ex16.k], [nrm.k])
            S.op("dve", lambda e: e.reciprocal(out=nrm[:, :], in_=nrm[:, :]), [nrm.k], [nrm.k])
            ts("dve", nrm[:, :], nrm[:, :], 1.0 / CSH, None, ALU.mult, None, [nrm.k], [nrm.k])
            for h in range(8):
                ts("dve", DG[t][:, h, :], C("ident"), nrm[:, h:h + 1], None, ALU.mult, None, [cst.k, nrm.k],
                   [DG[t].k])
            for (sm, vv, sub_thr) in ((s1m, v1, True), (s2m, v2, False)):
                mskt = zb[:, :, 0:128]
                tt("dve", mskt, sm[:, t, :, :], vv[:, :, 15:16].to_broadcast([128, 8, 128]), ALU.is_lt,
                   [sm.k, vv.k], [zb.k])
                stt("dve", sm[:, t, :, :], mskt, -BIG, sm[:, t, :, :], ALU.mult, ALU.add,
                    [zb.k, sm.k], [sm.k])
                if sub_thr:
                    tt("dve", sm[:, t, :, :], sm[:, t, :, :], thr[:, :].unsqueeze(2).to_broadcast([128, 8, 128]),
                       ALU.subtract, [sm.k, thr.k], [sm.k])
                act(sm[:, t, :, :], sm[:, t, :, :], AF.Exp, [sm.k], [sm.k])
                if sub_thr:
                    ts("dve", sm[:, t, :, :], sm[:, t, :, :], CSH, None, ALU.mult, None, [sm.k], [sm.k])
        psWs = (psS[0], psP[0])
        psA = psS[1]
        psV = ((psO, psG), (psG2, psP[1]))
        NG = NEXP // 512
        slots = {}

        def load_group(g):
            ru = ring_next()
            ld(ru[:, :, :], uT_s[:, g * 512:(g + 1) * 512].rearrange("(k p) n -> p k n", p=128), ru, reads=[k_uT_s])
            rv = ring_next()
            rv4 = rv[:, :, :].rearrange("p (c a) n -> p c (a n)", a=2)
            ld(rv4, v_s[g * 512:(g + 1) * 512, :].rearrange("(c p) d -> p c d", p=128), rv, reads=[k_v_s])
            slots[g] = (ru, rv, rv4)

        def stage_A_mm(g, sub):
            ru = slots[g][0]
            for ii in range(2):
                ec = sub * 2 + ii
                for kc in range(8):
                    mm(psA[:, ii * T2:(ii + 1) * T2], ru[:, kc, ec * 128:(ec + 1) * 128], h2T[:, kc, :],
                       kc == 0, kc == 7, [ru.k, h2T.k], [psA.k], inc=(kc == 7))

        def stage_A_gelu(g, sub):
            gt_ = gas[(g * 2 + sub) % 2]
            act(gt_[:, :, :], psA[:, 0:2 * T2].rearrange("p (a t) -> p a t", t=T2), AF.Gelu, [psA.k], [gt_.k])

        its = [(g, sub, t) for g in range(NG) for sub in range(2) for t in range(ST)]

        NZ = len(zbs)

        NHA = NHA_ACT

        def stage_P(k):
            g, sub, t = its[k]
            i0 = g * 4 + sub * 2
            zt = zbs[k % NZ]
            zk2 = zk2s[k % NZ]
            for h in range(NHA):
                for ii in range(2):
                    act(zt[:, h, ii, :], s2m[:, t, h, :], AF.Identity, [s1m.k, s2m.k], [zt.k],
                        scale=s1m[:, t, h, i0 + ii:i0 + ii + 1], ww_ok=True)
            nd = 8 - NHA
            tt("dve", zt[:, NHA:8, :, :],
               s1m[:, t, NHA:8, i0:i0 + 2].unsqueeze(3).to_broadcast([128, nd, 2, 128]),
               s2m[:, t, NHA:8, :].unsqueeze(2).to_broadcast([128, nd, 2, 128]), ALU.mult,
               [s1m.k, s2m.k], [zk2])

        def stage_M(k):
            zt = zbs[k % NZ]
            zk2 = zk2s[k % NZ]
            wt = wbs[k % 2]
            mt = mbs[k % 2]
            ts("dve", wt[:, :, :, :], zt[:, :, :, :], 1.0, -1.0, ALU.max, ALU.add, [zt.k, zk2], [wt.k])
            ts("dve", mt[:, :, :, :], zt[:, :, :, :], 1.0, None, ALU.is_ge, None, [zt.k, zk2], [mt.k])

        def stage_W(k):
            g, sub, t = its[k]
            wt = wbs[k % 2]
            mt = mbs[k % 2]
            pw = psWs[k % 2]
            first = True
            for h in range(8):
                for ii in range(2):
                    mm(pw[:, ii * 128:(ii + 1) * 128], wt[:, h, ii, :], DG[t][:, h, :], first, False,
                       [wt.k, DG[t].k], [pw.k], inc=False, skip=True)
                    first = False
                    mm(pw[:, ii * 128:(ii + 1) * 128], mt[:, h, ii, :], DG[t][:, h, :], False, (h == 7),
                       [mt.k, DG[t].k], [pw.k], inc=(h == 7 and ii == 1), skip=True)

        def stage_G(k):
            g, sub, t = its[k]
            pw = psWs[k % 2]
            gt_ = gas[(g * 2 + sub) % 2]
            tt("dve", GTs[k % 2][:, :, :], gt_[:, :, t * 128:(t + 1) * 128],
               pw[:, 0:256].rearrange("p (a t) -> p a t", t=128), ALU.mult, [gt_.k, pw.k], [GTs[k % 2].k])

        def stage_V(k):
            g, sub, t = its[k]
            rv, rv4 = slots[g][1], slots[g][2]
            for ii in range(2):
                ec = sub * 2 + ii
                for half in range(2):
                    pv = psV[t][half]
                    mm(pv[:, :], GTs[k % 2][:, ii, :], rv4[:, ec, half * 512:(half + 1) * 512],
                       (g == 0 and sub == 0 and ii == 0), (g == NG - 1 and sub == 1 and ii == 1),
                       [GTs[k % 2].k, rv.k], [pv.k], inc=(ii == 1 and half == 1))

        N = len(its)
        load_group(0)
        stage_A_mm(0, 0)
        stage_A_gelu(0, 0)
        stage_P(0)
        for k in range(N + 1):
            if k + 1 < N:
                stage_P(k + 1)
            if k < N:
                stage_M(k)
                stage_W(k)
            if k >= 1:
                stage_G(k - 1)
                stage_V(k - 1)
            if k < N:
                g, sub, t = its[k]
                ng, nsub = (g, 1) if sub == 0 else (g + 1, 0)
                if ng < NG:
                    if t == 0:
                        if nsub == 0:
                            load_group(ng)
                        stage_A_mm(ng, nsub)
                    else:
                        stage_A_gelu(ng, nsub)
        for t in range(ST):
            for half in range(2):
                cs = slice(half * 512, (half + 1) * 512)
                tt("dve", sq[:, :], psV[t][half][:, :], gt2B[:, cs], ALU.mult, [psV[t][half].k, gt2B.k], [sq.k])
                tt("dve", x1[:, t, cs], sq[:, :], x1[:, t, cs], ALU.add, [sq.k, xk[t]], [xk[t]])
            tok0 = b * SEQ + (st_i * ST + t) * 128
            out_toks.append(S.dma("sp", y_d[tok0:tok0 + 128, :], x1[:, t, :], owner=xk[t], reads=[xk[t]]))

    for b in range(NB):
        if slevel >= 1:
            adaln(b)
        for st_i in range(NST):
            for t in range(ST):
                mixer_tile(b, st_i * ST + t, t)
            if do_peer:
                peer_supertile(b, st_i)
            else:
                for t in range(ST):
                    tok0 = b * SEQ + (st_i * ST + t) * 128
                    out_toks.append(S.dma("sp", y_d[tok0:tok0 + 128, :], x1[:, t, :], owner=xk[t], reads=[xk[t]]))
    print("sbuf bytes remaining", nc.sbuf_bytes_remaining)
    S.wait_all("sp", out_toks + dbg_toks)
    S.emit()
    return nc, S


def make_in_maps(inputs, n_cores, NB, SEQ):
    f = lambda a: np.ascontiguousarray(np.asarray(a, dtype=np.float32))
    x = f(inputs["x"])
    c = f(inputs["c"])
    pos = np.asarray(inputs["positions"]).astype(np.int32)
    NT = SEQ // 128
    cblob, _ = _consts()
    shared = {
        "w_ada": f(inputs["w_ada"][0]),
        "b_adaT": f(np.asarray(inputs["b_ada"][0]).reshape(48, 128).T),
        "b_ada": f(np.asarray(inputs["b_ada"][0]).reshape(1, -1)),
        "g1T": f(np.asarray(inputs["norm1_g"][0]).reshape(8, 128).T),
        "g2T": f(np.asarray(inputs["norm2_g"][0]).reshape(8, 128).T),
        "w_in": f(inputs["w_in"][0]),
        "w_out": f(inputs["w_out"][0]),
        "w_query": f(inputs["w_query"][0]),
        "b_queryT": f(np.asarray(inputs["b_query"][0]).reshape(16, 128).T),
        "keys1T": f(np.asarray(inputs["peer_keys1"][0]).T),
        "keys2T": f(np.asarray(inputs["peer_keys2"][0]).T),
        "uT": f(np.asarray(inputs["expert_u"][0]).T),
        "ev": f(inputs["expert_v"][0]),
        "vec64": f(np.concatenate([np.asarray(inputs[k][0]).reshape(-1) for k in
                                   ("qn_g", "kn_g", "lam_q1", "lam_k1", "lam_q2", "lam_k2")]).reshape(1, -1)),
        "vec128": f(np.concatenate([np.asarray(inputs[k][0]).reshape(-1) for k in
                                    ("diff_norm_g", "gla_norm_g")]).reshape(1, -1)),
        "w_gate2": f(inputs["w_gate2"][0]),
        "b_gate": f(np.asarray(inputs["b_gate"][0]).reshape(1, -1)),
        "consts": cblob,
    }
    maps = []
    for i in range(n_cores):
        bs = slice(i * NB, (i + 1) * NB)
        m = dict(shared)
        m["x"] = np.ascontiguousarray(x[bs].reshape(NB * SEQ, D))
        m["cT"] = np.ascontiguousarray(c[bs].T)
        m["posT"] = np.ascontiguousarray(pos[bs].reshape(NB * NT, 128).T)
        maps.append(m)
    return maps


def kernel(**inputs):
    x = np.asarray(inputs["x"])
    B, SEQ, _ = x.shape
    NB = B // NCORES
    nc, _ = build_program(NB, SEQ, do_peer=True)
    maps = make_in_maps(inputs, NCORES, NB, SEQ)
    res = run_bass_kernel_spmd(nc, maps, core_ids=list(range(NCORES)))
    out = np.concatenate([np.asarray(r["y"]).reshape(NB, SEQ, D) for r in res.results], axis=0)
    return out.astype(np.float32)
```

```python
import math
import os
import threading
import numpy as np
import concourse.bass as bass
import concourse.mybir as mybir
from concourse.bass_utils import run_bass_kernel_spmd

F32 = mybir.dt.float32
BF16 = mybir.dt.bfloat16
I32 = mybir.dt.int32
AF = mybir.ActivationFunctionType
ALU = mybir.AluOpType
AX = mybir.AxisListType

D = 1024
NCORES = 8
EPS = 1e-6
NEXP = 16384
BIG = 1.0e4
GSUB = int(os.environ.get("GSUB", "9"))
INTERLEAVE = int(os.environ.get("INTERLEAVE", "1"))
NHA_ACT = int(os.environ.get("NHA_ACT", "5"))
NZBUF = int(os.environ.get("NZBUF", "3"))
CSH = float(np.float32(1.0 - 2.0 ** -9))


class Key:
    __slots__ = ("name", "excl", "const", "lw", "rd", "sem", "semcnt")

    def __init__(self, name, excl=False, const=False):
        self.name = name
        self.excl = excl
        self.const = const
        self.lw = None
        self.rd = []
        self.sem = None
        self.semcnt = 0


class Co:
    def __init__(self, fn):
        self.go = threading.Semaphore(0)
        self.back = threading.Semaphore(0)
        self.done = False
        self.budget = 0
        self.err = None
        self.th = threading.Thread(target=self._run, args=(fn,), daemon=True)
        self.th.start()

    def _run(self, fn):
        self.go.acquire()
        try:
            fn()
        except BaseException as e:
            self.err = e
        self.done = True
        self.back.release()

    def hook(self):
        if self.budget <= 0:
            self.back.release()
            self.go.acquire()
        self.budget -= 1

    def step(self, n):
        if self.done:
            return
        self.budget = n
        self.go.release()
        self.back.acquire()
        if self.err is not None:
            raise self.err

    def finish(self):
        while not self.done:
            self.step(1000)
        if self.err is not None:
            raise self.err


class Sched:
    def __init__(self, nc):
        self.co = None
        self.nc = nc
        self.engs = ("pe", "act", "dve", "pool", "sp")
        self.prog = {e: [] for e in self.engs}
        self.cnt = {e: 0 for e in self.engs}
        self.seen = {e: {} for e in self.engs}
        self.esem = {e: nc.alloc_semaphore(name="tl_" + e) for e in self.engs}
        self.n_ins = 0

    def _need(self, eng, tok, waits, raw):
        if tok is None:
            return
        if tok[0] == "e":
            _, f, n = tok
            if f == eng and eng == "pe":
                return
            k = ("e", f)
        else:
            _, key, n = tok
            k = ("d", key)
        if self.seen[eng].get(k, 0) >= n:
            return
        if n > waits.get(k, 0):
            waits[k] = n

    def _deps(self, eng, reads, writes, ww_ok=False):
        waits = {}
        for k in reads:
            self._need(eng, k.lw, waits, True)
        for k in writes:
            if not (ww_ok and k.lw is not None and k.lw[0] == "e" and k.lw[1] == eng):
                self._need(eng, k.lw, waits, False)
            for t in k.rd:
                self._need(eng, t, waits, False)
        for k, n in waits.items():
            self.seen[eng][k] = n
            sem = self.esem[k[1]] if k[0] == "e" else k[1].sem
            self.prog[eng].append(("w", sem, n))

    def _commit(self, tok, reads, writes):
        for k in reads:
            if k.excl:
                k.lw = tok
                k.rd = []
            elif not k.const:
                k.rd.append(tok)
        for k in writes:
            k.lw = tok
            k.rd = []

    def _co_hook(self):
        co = self.co
        if co is None:
            return False
        if threading.current_thread() is co.th:
            co.hook()
            return False
        return True

    def op(self, eng, fn, reads=(), writes=(), inc=True, ww_ok=False):
        main_with_co = self._co_hook()
        tok = self._op(eng, fn, reads, writes, inc, ww_ok)
        if main_with_co:
            self.co.step(1)
        return tok

    def _op(self, eng, fn, reads=(), writes=(), inc=True, ww_ok=False):
        self._deps(eng, reads, writes, ww_ok)
        self.n_ins += 1
        if inc:
            self.cnt[eng] += 1
            tok = ("e", eng, self.cnt[eng])
            self.prog[eng].append(("i", fn, self.esem[eng], 1))
        else:
            tok = ("e", eng, self.cnt[eng] + 1)
            self.prog[eng].append(("i", fn, None, 0))
        self._commit(tok, reads, writes)
        return tok

    def dma(self, eng, out, in_, owner, reads=(), writes=(), **kw):
        self._co_hook()
        self._deps(eng, reads, writes)
        if owner.sem is None:
            owner.sem = self.nc.alloc_semaphore(name="d_" + owner.name)
        owner.semcnt += 16
        tok = ("d", owner, owner.semcnt)
        self.n_ins += 1
        self.prog[eng].append(
            ("i", lambda e, o=out, i=in_, kw=kw: e.dma_start(out=o, in_=i, **kw), owner.sem, 16))
        self._commit(tok, reads, writes)
        return tok

    def wait_all(self, eng, toks):
        waits = {}
        for t in toks:
            self._need(eng, t, waits, True)
        for k, n in waits.items():
            self.seen[eng][k] = n
            sem = self.esem[k[1]] if k[0] == "e" else k[1].sem
            self.prog[eng].append(("w", sem, n))

    def emit(self):
        progs = self.prog

        def run(e, lst):
            for it in lst:
                if it[0] == "w":
                    e.wait_ge(it[1], it[2])
                else:
                    ins = it[1](e)
                    if it[2] is not None:
                        ins.then_inc(it[2], it[3])

        with self.nc.Block() as block:
            @block.tensor
            def _(e):
                run(e, progs["pe"])

            @block.scalar
            def _(e):
                run(e, progs["act"])

            @block.vector
            def _(e):
                run(e, progs["dve"])

            @block.gpsimd
            def _(e):
                run(e, progs["pool"])

            @block.sync
            def _(e):
                run(e, progs["sp"])


class Tl:
    def __init__(self, nc, name, shape, dt, psum=False, const=False):
        if psum:
            self.t = nc.alloc_psum_tensor("p_" + name, shape, dt)
        else:
            self.t = nc.alloc_sbuf_tensor("s_" + name, shape, dt)
        self.k = Key(name, excl=psum, const=const)

    def __getitem__(self, idx):
        return self.t[idx]


def _consts():
    p = np.arange(128)
    same = (p[:, None] // 64) == (p[None, :] // 64)
    c = {}
    c["ident"] = np.eye(128, dtype=np.float32)
    c["causal"] = (p[:, None] <= p[None, :]).astype(np.float32)
    c["mcum"] = (same & (p[:, None] <= p[None, :])).astype(np.float32)
    c["mblk"] = same.astype(np.float32)
    c["mmid"] = (same & ((p[:, None] % 64) <= 32)).astype(np.float32)
    c["chunkind"] = np.stack([(p < 64), (p >= 64)], axis=1).astype(np.float32)
    invf = (10000.0 ** (-np.arange(0, 64, 2, dtype=np.float32) / 64)).astype(np.float32)
    c["invf"] = np.tile(invf[None, :], (128, 1)).astype(np.float32)
    order = ["ident", "causal", "mcum", "mblk", "mmid", "chunkind", "invf"]
    offs = {}
    cols = 0
    for n in order:
        offs[n] = (cols, c[n].shape[1])
        cols += c[n].shape[1]
    blob = np.concatenate([c[n] for n in order], axis=1).astype(np.float32)
    return blob, offs


def build_program(NB, SEQ, do_peer=True, dbg=(), stage="full", chain_casts=True):
    STAGES = ["pro", "ada", "norm", "qkv", "proj", "attn", "gla", "full"]
    slevel = STAGES.index(stage)
    NT = SEQ // 128
    ST = 2
    NST = NT // ST
    NTOK = NB * SEQ
    nc = bass.Bass("TRN2", target_bir_lowering=False)
    S = Sched(nc)
    cblob, coffs = _consts()
    CW = cblob.shape[1]

    def din(name, shape, dt=F32):
        return nc.dram_tensor(name, list(shape), dt, kind="ExternalInput").ap()

    x_d = din("x", [NTOK, D])
    cT_d = din("cT", [D, NB])
    pos_d = din("posT", [128, NB * NT], I32)
    wada_d = din("w_ada", [D, 6 * D])
    badaT_d = din("b_adaT", [128, 48])
    bada_d = din("b_ada", [1, 6 * D])
    g1T_d = din("g1T", [128, 8])
    g2T_d = din("g2T", [128, 8])
    win_d = din("w_in", [D, 3088])
    wout_d = din("w_out", [D, D])
    wq_d = din("w_query", [D, 2048])
    bqT_d = din("b_queryT", [128, 16])
    k1T_d = din("keys1T", [128, 128])
    k2T_d = din("keys2T", [128, 128])
    uT_d = din("uT", [D, NEXP])
    v_d = din("ev", [NEXP, D])
    vec64_d = din("vec64", [1, 6 * 64])
    vec128_d = din("vec128", [1, 2 * 128])
    wg2_d = din("w_gate2", [16, 256])
    bg_d = din("b_gate", [1, 256])
    cst_d = din("consts", [128, CW])
    y_d = nc.dram_tensor("y", [NTOK, D], F32, kind="ExternalOutput").ap()
    dbg_d = {n: nc.dram_tensor("dbg_" + n, list(shp), F32, kind="ExternalOutput").ap() for n, shp in dbg}

    win_s = nc.dram_tensor("win_s", [D, 3088], BF16, kind="Internal").ap()
    wout_s = nc.dram_tensor("wout_s", [D, D], BF16, kind="Internal").ap()
    wq_s = nc.dram_tensor("wq_s", [D, 2048], BF16, kind="Internal").ap()
    uT_s = nc.dram_tensor("uT_s", [D, NEXP], BF16, kind="Internal").ap()
    v_s = nc.dram_tensor("v_s", [NEXP, D], BF16, kind="Internal").ap()
    k_win_s, k_wout_s, k_wq_s, k_uT_s, k_v_s = (Key(n, const=True) for n in ("kwin", "kwout", "kwq", "kuT", "kv"))

    def sb(name, shape, dt=F32, const=False):
        return Tl(nc, name, shape, dt, const=const)

    cst = sb("cst", [128, CW], F32, const=True)
    identb = sb("identb", [128, 128], BF16, const=True)
    causb = sb("causb", [128, 128], BF16, const=True)
    onesr = sb("onesr", [1, 128], F32, const=True)
    cT = sb("cT", [128, 8, NB], F32, const=True)
    posi = sb("posi", [128, NB * NT], I32, const=True)
    posf = sb("posf", [128, NB * NT], F32, const=True)
    badaT = sb("badaT", [128, 48], F32, const=True)
    g1T = sb("g1T", [128, 8], F32, const=True)
    g2T = sb("g2T", [128, 8], F32, const=True)
    bqT = sb("bqT", [128, 16], F32, const=True)
    k1T = sb("k1T", [128, 128], BF16, const=True)
    k2T = sb("k2T", [128, 128], BF16, const=True)
    kst = sb("kst", [128, 128], F32)
    v64 = sb("v64", [128, 6 * 64], F32, const=True)
    v128 = sb("v128", [128, 256], F32, const=True)
    qgB = sb("qgB", [128, 64], F32, const=True)
    subgB = sb("subgB", [128, 128], F32, const=True)
    neglam = sb("neglam", [128, 1], F32, const=True)
    lamt = sb("lamt", [128, 64], F32)
    lams = sb("lams", [128, 4], F32)
    wg2 = sb("wg2", [16, 256], F32, const=True)
    bgr = sb("bgr", [1, 256], F32, const=True)
    cvals = sb("cvals", [128, 4], F32, const=True)

    def C(name):
        o, w = coffs[name]
        return cst[:, o:o + w]

    sT = sb("sT", [128, 8], F32)
    modT = sb("modT", [128, 4, 8], F32)
    gm1 = sb("gm1", [128, 8], F32)
    gm2 = sb("gm2", [128, 8], F32)
    gt1B = sb("gt1B", [128, D], F32)
    gt2B = sb("gt2B", [128, D], F32)
    wst = [sb("wst%d" % i, [128, 8, 128], F32) for i in range(1)]

    NRING = 4
    ring = [sb("ring%d" % i, [128, 8, 512], BF16) for i in range(NRING)]
    rc = [0]

    def ring_next():
        r = ring[rc[0] % NRING]
        rc[0] += 1
        return r

    KT = [sb("KT%d" % i, [128, 4, 128], BF16) for i in range(NT)]
    VC = [sb("VC%d" % i, [128, 4, 130], BF16) for i in range(NT)]
    Sst = [sb("Sst%d" % i, [128, 128], F32) for i in range(2)]

    x1 = sb("x1", [128, ST, D], F32)
    xk = [Key("x1_%d" % i) for i in range(ST)]
    junk = sb("junk", [128, D], BF16)
    xn = sb("xn", [128, D], BF16)
    st1 = sb("st1", [128, 4], F32)
    hT = sb("hT", [128, 8, 128], BF16)
    h2T = sb("h2T", [128, 8, ST * 128], BF16)
    sq = sb("sq", [128, 512], F32)
    ss8 = sb("ss8", [128, 8], F32)
    qn = sb("qn", [128, 512], F32)
    rt = [sb("rt%d" % i, [128, 256], F32) for i in range(2)]
    qr = sb("qr", [128, 512], BF16)
    qT = sb("qT", [128, 4, 128], BF16)
    ang = sb("ang", [128, 32], F32)
    ang2 = sb("ang2", [128, 32], F32)
    ang3 = sb("ang3", [128, 32], F32)
    angi = sb("angi", [128, 32], I32)
    sinT = sb("sinT", [128, 32], F32)
    cosT = sb("cosT", [128, 32], F32)
    PT = [sb("PT%d" % i, [128, 512], BF16) for i in range(2)]
    osb = sb("osb", [128, 128], F32)
    rz = sb("rz", [128, 4], F32)
    ybf = sb("ybf", [128, D], BF16)
    ykey2 = Key("ybf_gla")
    junk2 = sb("junk2", [128, 128], BF16)
    rz2 = sb("rz2", [128, 4], F32)
    osb2 = sb("osb2", [128, 128], F32)
    yT = sb("yT", [128, 8, 128], BF16)
    gqk = sb("gqk", [128, 512], F32)
    gv = sb("gv", [128, 512], F32)
    sr = sb("sr", [128, 512], F32)
    ggT = sb("ggT", [16, 128], F32)
    la = sb("la", [128, 256], F32)
    gl = [sb("gl%d" % i, [128, 256], F32) for i in range(8)]
    dec = sb("dec", [128, 2, 2], F32)
    T3 = sb("T3", [128, 3, 128], F32)
    ATs = sb("ATs", [128, 128], F32)

    psT = Tl(nc, "psT", [128, 1024], BF16, psum=True)
    psP = [Tl(nc, "psP%d" % i, [128, 512], F32, psum=True) for i in range(2)]
    psS = [Tl(nc, "psS%d" % i, [128, 512], F32, psum=True) for i in range(2)]
    psO = Tl(nc, "psO", [128, 512], F32, psum=True)
    psG = Tl(nc, "psG", [128, 512], F32, psum=True)
    psG2 = Tl(nc, "psG2", [128, 512], F32, psum=True)

    def mm(out, lhsT, rhs, start, stop, reads, writes, inc=True, skip=False):
        if skip:
            S.op("pe", lambda e: e.matmul(out, lhsT=lhsT, rhs=rhs, start=start, stop=stop, skip_group_check=True),
                 reads, writes, inc)
        else:
            S.op("pe", lambda e: e.matmul(out, lhsT=lhsT, rhs=rhs, start=start, stop=stop), reads, writes, inc)

    def tr(out, in_, ident, reads, writes, inc=True):
        S.op("pe", lambda e: e.transpose(out, in_, ident), reads, writes, inc)

    def act(out, in_, func, reads, writes, bias=None, scale=None, accum_out=None, ww_ok=False):
        kw = {}
        if bias is not None:
            kw["bias"] = bias
        if scale is not None:
            kw["scale"] = scale
        if accum_out is not None:
            kw["accum_out"] = accum_out
        S.op("act", lambda e: e.activation(out=out, in_=in_, func=func, **kw), reads, writes, ww_ok=ww_ok)

    def tt(eng, out, in0, in1, op, reads, writes):
        S.op(eng, lambda e: e.tensor_tensor(out=out, in0=in0, in1=in1, op=op), reads, writes)

    def ts(eng, out, in0, s1, s2, op0, op1, reads, writes):
        if s2 is None:
            S.op(eng, lambda e: e.tensor_scalar(out=out, in0=in0, scalar1=s1, scalar2=None, op0=op0), reads, writes)
        else:
            S.op(eng, lambda e: e.tensor_scalar(out=out, in0=in0, scalar1=s1, scalar2=s2, op0=op0, op1=op1),
                 reads, writes)

    def stt(eng, out, in0, scalar, in1, op0, op1, reads, writes):
        S.op(eng, lambda e: e.scalar_tensor_tensor(out=out, in0=in0, scalar=scalar, in1=in1, op0=op0, op1=op1),
             reads, writes)

    def cp(eng, out, in_, reads, writes):
        if eng == "act":
            S.op(eng, lambda e: e.activation(out=out, in_=in_, func=AF.Identity), reads, writes)
        else:
            S.op(eng, lambda e: e.tensor_copy(out=out, in_=in_), reads, writes)

    def red(eng, out, in_, op, reads, writes):
        S.op(eng, lambda e: e.tensor_reduce(out=out, in_=in_, axis=AX.X, op=op), reads, writes)

    def rsqrt_mean(dst, src, n, keys):
        act(dst, src, AF.Ln, list(keys) + [cvals.k], keys, bias=cvals[:, 1:2], scale=1.0 / n)
        act(dst, dst, AF.Exp, keys, keys, scale=-0.5)

    def ld(out, in_, tile, reads=(), **kw):
        return S.dma("sp", out, in_, owner=tile.k, reads=reads, writes=[tile.k], **kw)

    def cast_copy(dst, src, key, rows, cols, cchunk):
        for r0 in range(0, rows, 128):
            for c0 in range(0, cols, cchunk):
                c1 = min(cols, c0 + cchunk)
                if chain_casts and key.lw is not None:
                    S.wait_all("pool", [key.lw])
                S.dma("pool", dst[r0:r0 + 128, c0:c1], src[r0:r0 + 128, c0:c1], owner=key, writes=[key])

    cast_copy(win_s, win_d, k_win_s, D, 3088, 3088)
    cast_copy(wout_s, wout_d, k_wout_s, D, D, D)
    cast_copy(wq_s, wq_d, k_wq_s, D, 2048, 2048)
    if do_peer:
        cast_copy(uT_s, uT_d, k_uT_s, D, NEXP, 4096)
        cast_copy(v_s, v_d, k_v_s, NEXP, D, D)

    ld(cst[:, :], cst_d, cst)
    ld(cT[:, :, :], cT_d.rearrange("(k p) b -> p k b", p=128), cT, allow_slow_non_contiguous=True)
    ld(posi[:, :], pos_d, posi)
    ld(badaT[:, :], badaT_d, badaT)
    ld(g1T[:, :], g1T_d, g1T)
    ld(g2T[:, :], g2T_d, g2T)
    ld(bqT[:, :], bqT_d, bqT)
    ld(v64[:, :], vec64_d[0:1, :].to_broadcast([128, 384]), v64)
    ld(v128[:, :], vec128_d[0:1, :].to_broadcast([128, 256]), v128)
    ld(wg2[:, :], wg2_d, wg2)
    ld(bgr[:, :], bg_d, bgr)
    S.op("dve", lambda e: e.memset(onesr[:, :], 1.0), (), [onesr.k])
    S.op("dve", lambda e: e.memset(cvals[:, 0:1], -math.pi), (), [cvals.k])
    S.op("dve", lambda e: e.memset(cvals[:, 1:2], EPS), (), [cvals.k])
    S.op("dve", lambda e: e.memset(cvals[:, 2:3], 1.0), (), [cvals.k])
    S.op("dve", lambda e: e.memset(cvals[:, 3:4], 0.0), (), [cvals.k])
    cp("dve", identb[:, :], C("ident"), [cst.k], [identb.k])
    cp("dve", causb[:, :], C("causal"), [cst.k], [causb.k])
    cp("dve", posf[:, :], posi[:, :], [posi.k], [posf.k])
    ld(kst[:, :], k1T_d, kst)
    cp("dve", k1T[:, :], kst[:, :], [kst.k], [k1T.k])
    ld(kst[:, :], k2T_d, kst)
    cp("dve", k2T[:, :], kst[:, :], [kst.k], [k2T.k])
    ts("dve", qgB[:, :], v64[:, 0:64], 0.125, None, ALU.mult, None, [v64.k], [qgB.k])
    ts("dve", subgB[:, :], v128[:, 0:128], 0.8, None, ALU.mult, None, [v128.k], [subgB.k])
    tt("dve", lamt[:, :], v64[:, 128:192], v64[:, 192:256], ALU.mult, [v64.k], [lamt.k])
    red("dve", lams[:, 0:1], lamt[:, :], ALU.add, [lamt.k], [lams.k])
    tt("dve", lamt[:, :], v64[:, 256:320], v64[:, 320:384], ALU.mult, [v64.k, lams.k], [lamt.k])
    red("dve", lams[:, 1:2], lamt[:, :], ALU.add, [lamt.k], [lams.k])
    act(lams[:, 2:4], lams[:, 0:2], AF.Exp, [lams.k], [lams.k])
    tt("dve", lams[:, 0:1], lams[:, 3:4], lams[:, 2:3], ALU.subtract, [lams.k], [lams.k])
    ts("dve", neglam[:, :], lams[:, 0:1], -0.2, None, ALU.add, None, [lams.k], [neglam.k])
    for i in range(NT):
        S.op("pool", lambda e, i=i: e.memset(VC[i][:, :, :], 1.0), (), [VC[i].k])

    def adaln(b):
        act(sT[:, :], cT[:, :, b], AF.Silu, [cT.k], [sT.k])
        ld(gt1B[:, :], bada_d[0:1, 2 * D:3 * D].to_broadcast([128, D]), gt1B)
        ld(gt2B[:, :], bada_d[0:1, 5 * D:6 * D].to_broadcast([128, D]), gt2B)
        wi = 0
        for sec in range(6):
            for q8 in range(8):
                w = wst[0]
                wi += 1
                c0 = sec * D + q8 * 128
                ld(w[:, :, :], wada_d[:, c0:c0 + 128].rearrange("(k p) n -> p k n", p=128), w)
                ps = psP[wi % 2]
                if sec in (2, 5):
                    for kc in range(8):
                        mm(ps[:, 0:128], sT[:, kc:kc + 1].to_broadcast([128, 128]), w[:, kc, :],
                           kc == 0, kc == 7, [sT.k, w.k], [ps.k], inc=(kc == 7))
                    g = gt1B if sec == 2 else gt2B
                    tt("dve", g[:, q8 * 128:(q8 + 1) * 128], g[:, q8 * 128:(q8 + 1) * 128], ps[:, 0:128], ALU.add,
                       [ps.k, g.k], [g.k])
                else:
                    mi = {0: 0, 1: 1, 3: 2, 4: 3}[sec]
                    for kc in range(8):
                        mm(ps[:, 0:1], w[:, kc, :], sT[:, kc:kc + 1],
                           kc == 0, kc == 7, [sT.k, w.k], [ps.k], inc=(kc == 7))
                    tt("dve", modT[:, mi, q8:q8 + 1], ps[:, 0:1],
                       badaT[:, sec * 8 + q8:sec * 8 + q8 + 1], ALU.add, [ps.k, badaT.k], [modT.k])
        stt("dve", gm1[:, :], modT[:, 1, :], 1.0, g1T[:, :], ALU.add, ALU.mult, [modT.k, g1T.k], [gm1.k])
        stt("dve", gm2[:, :], modT[:, 3, :], 1.0, g2T[:, :], ALU.add, ALU.mult, [modT.k, g2T.k], [gm2.k])

    def norm_T(xap, xkey, gm, shi, dst, dcol):
        act(junk[:, :], xap, AF.Square, [xkey], [junk.k, st1.k], accum_out=st1[:, 0:1])
        rsqrt_mean(st1[:, 2:3], st1[:, 0:1], D, [st1.k])
        ts("dve", xn[:, :], xap, st1[:, 2:3], None, ALU.mult, None, [xkey, st1.k], [xn.k])
        for j in range(8):
            tr(psT[:, j * 128:(j + 1) * 128], xn[:, j * 128:(j + 1) * 128], identb[:, :],
               [xn.k, identb.k], [psT.k], inc=(j == 7))
        for j in range(8):
            act(dst[:, j, dcol:dcol + 128], psT[:, j * 128:(j + 1) * 128], AF.Identity,
                [psT.k, gm.k, modT.k], [dst.k], bias=modT[:, shi, j:j + 1], scale=gm[:, j:j + 1])

    def rope_tables(col):
        C1 = 6.28125
        C2 = 2.0 * math.pi - C1
        ts("dve", ang[:, :], C("invf"), posf[:, col:col + 1], None, ALU.mult, None, [cst.k, posf.k], [ang.k])
        for (shift, dst) in ((0.0, sinT), (0.5 * math.pi, cosT)):
            ts("dve", ang2[:, :], ang[:, :], shift, 1.0 / (2.0 * math.pi), ALU.add, ALU.mult, [ang.k], [ang2.k])
            cp("dve", angi[:, :], ang2[:, :], [ang2.k], [angi.k])
            cp("dve", ang2[:, :], angi[:, :], [angi.k], [ang2.k])
            stt("dve", ang3[:, :], ang2[:, :], -C1, ang[:, :], ALU.mult, ALU.add, [ang2.k, ang.k], [ang3.k])
            stt("dve", ang3[:, :], ang2[:, :], -C2, ang3[:, :], ALU.mult, ALU.add, [ang2.k, ang3.k], [ang3.k])
            ts("dve", ang3[:, :], ang3[:, :], shift, math.pi, ALU.add, ALU.min, [ang3.k], [ang3.k])
            ts("dve", ang3[:, :], ang3[:, :], -math.pi, None, ALU.max, None, [ang3.k], [ang3.k])
            act(dst[:, :], ang3[:, :], AF.Sin, [ang3.k], [dst.k])

    def proj_block(c0, ncols, ps, lhs=None):
        r = ring_next()
        ld(r[:, :, 0:ncols], win_s[:, c0:c0 + ncols].rearrange("(k p) n -> p k n", p=128), r, reads=[k_win_s])
        for kc in range(8):
            mm(ps[:, 0:ncols], hT[:, kc, :], r[:, kc, 0:ncols], kc == 0, kc == 7,
               [hT.k, r.k], [ps.k], inc=(kc == 7))
        return r

    def qk_post(ps, gB, dstT, dkey):
        act(sq[:, :], ps[:, :], AF.Square, [ps.k], [sq.k])
        red("dve", ss8[:, :], sq[:, :].rearrange("p (g d) -> p g d", d=64), ALU.add, [sq.k], [ss8.k])
        rsqrt_mean(ss8[:, :], ss8[:, :], 64, [ss8.k])
        q3 = qn[:, :].rearrange("p (g d) -> p g d", d=64)
        tt("dve", q3, ps[:, :].rearrange("p (g d) -> p g d", d=64),
           ss8[:, :].unsqueeze(2).to_broadcast([128, 8, 64]), ALU.mult, [ps.k, ss8.k], [qn.k])
        tt("dve", q3, q3, gB.unsqueeze(1).to_broadcast([128, 8, 64]), ALU.mult, [qn.k, v64.k, qgB.k], [qn.k])
        x1v = q3[:, :, 0:32]
        x2v = q3[:, :, 32:64]
        cb = cosT[:, :].unsqueeze(1).to_broadcast([128, 8, 32])
        sbb = sinT[:, :].unsqueeze(1).to_broadcast([128, 8, 32])
        r0, r1 = (t[:, :].rearrange("p (g d) -> p g d", d=32) for t in rt)
        qr3 = qr[:, :].rearrange("p (g d) -> p g d", d=64)
        tt("dve", r0, x1v, cb, ALU.mult, [qn.k, cosT.k], [rt[0].k])
        tt("dve", r1, x2v, sbb, ALU.mult, [qn.k, sinT.k], [rt[1].k])
        tt("dve", qr3[:, :, 0:32], r0, r1, ALU.subtract, [rt[0].k, rt[1].k], [qr.k])
        tt("dve", r0, x2v, cb, ALU.mult, [qn.k, cosT.k], [rt[0].k])
        tt("dve", r1, x1v, sbb, ALU.mult, [qn.k, sinT.k], [rt[1].k])
        tt("dve", qr3[:, :, 32:64], r0, r1, ALU.add, [rt[0].k, rt[1].k], [qr.k])
        for h in range(4):
            tr(psT[:, h * 128:(h + 1) * 128], qr[:, h * 128:(h + 1) * 128], identb[:, :],
               [qr.k, identb.k], [psT.k], inc=(h == 3))
        cp("dve", dstT[:, :, :], psT[:, 0:512].rearrange("p (h t) -> p h t", t=128),
           [psT.k], [dkey])

    dbg_toks = []

    def dump(name, ap, key):
        if name in dbg_d:
            dbg_toks.append(S.dma("sp", dbg_d[name], ap, owner=key, reads=[key]))

    def mixer_tile(b, i, slot):
        col = b * NT + i
        tok0 = b * SEQ + i * 128
        xs = x1[:, slot, :]
        xkey = xk[slot]
        S.dma("sp", xs, x_d[tok0:tok0 + 128, :], owner=xkey, writes=[xkey])
        if slevel < 2:
            return
        norm_T(xs, xkey, gm1, 0, hT, 0)
        rope_tables(col)
        if slevel < 3:
            return
        proj_block(0, 512, psP[0])
        qk_post(psP[0], qgB[:, :], qT, qT.k)
        proj_block(512, 512, psP[1])
        qk_post(psP[1], v64[:, 64:128], KT[i], KT[i].k)
        proj_block(1024, 512, psP[0])
        cp("act", VC[i][:, :, 0:128], psP[0][:, :].rearrange("p (h e) -> p h e", e=128), [psP[0].k], [VC[i].k])
        if slevel < 4:
            return
        proj_block(1536, 512, psP[1])
        cp("act", gqk[:, :], psP[1][:, :], [psP[1].k], [gqk.k])
        proj_block(2048, 512, psP[0])
        cp("act", gv[:, :], psP[0][:, :], [psP[0].k], [gv.k])
        proj_block(2560, 512, psP[1])
        act(sr[:, :], psP[1][:, :], AF.Silu, [psP[1].k], [sr.k])
        r = ring_next()
        ld(r[:, :, 0:16], win_s[:, 3072:3088].rearrange("(k p) n -> p k n", p=128), r, reads=[k_win_s])
        for kc in range(8):
            mm(psP[0][0:16, 0:128], r[:, kc, 0:16], hT[:, kc, :], kc == 0, kc == 7,
               [hT.k, r.k], [psP[0].k], inc=(kc == 7))
        cp("act", ggT[:, :], psP[0][0:16, 0:128], [psP[0].k], [ggT.k])

        if slevel < 5:
            return

        def attn_part():
            nkb = i + 1
            ngrp = (nkb + 3) // 4
            sidx = 0
            for h in range(4):
                for m in range(2):
                    pr = slice(m * 64, (m + 1) * 64)
                    for g in range(ngrp):
                        j0 = g * 4
                        nj = min(4, nkb - j0)
                        pss = psS[sidx % 2]
                        pt = PT[sidx % 2]
                        sidx += 1
                        for jj in range(nj):
                            j = j0 + jj
                            mm(pss[:, jj * 128:(jj + 1) * 128], KT[j][pr, h, :], qT[pr, h, :], True, True,
                               [KT[j].k, qT.k], [pss.k], inc=(jj == nj - 1))
                        act(pt[:, 0:nj * 128], pss[:, 0:nj * 128], AF.Exp, [pss.k], [pt.k])
                        if j0 + nj - 1 == i:
                            dsl = slice((nj - 1) * 128, nj * 128)
                            tt("pool", pt[:, dsl], pt[:, dsl], causb[:, :], ALU.mult, [pt.k, causb.k], [pt.k])
                        for jj in range(nj):
                            j = j0 + jj
                            mm(psO[:, m * 129:(m + 1) * 129], pt[:, jj * 128:(jj + 1) * 128], VC[j][:, h, 0:129],
                               j == 0, j == i, [pt.k, VC[j].k], [psO.k], inc=(jj == nj - 1))
                S.op("dve", lambda e: e.reciprocal(out=rz[:, 0:1], in_=psO[:, 128:129]), [psO.k], [rz.k])
                S.op("dve", lambda e: e.reciprocal(out=rz[:, 1:2], in_=psO[:, 257:258]), [psO.k], [rz.k])
                tt("dve", rz[:, 2:3], rz[:, 1:2], neglam[:, :], ALU.mult, [rz.k, neglam.k], [rz.k])
                ts("dve", osb[:, :], psO[:, 0:128], rz[:, 0:1], None, ALU.mult, None, [psO.k, rz.k], [osb.k])
                stt("dve", osb[:, :], psO[:, 129:257], rz[:, 2:3], osb[:, :], ALU.mult, ALU.add,
                    [psO.k, rz.k, osb.k], [osb.k])
                act(junk[:, 0:128], osb[:, :], AF.Square, [osb.k], [junk.k, rz.k], accum_out=rz[:, 3:4])
                rsqrt_mean(rz[:, 3:4], rz[:, 3:4], 128, [rz.k])
                stt("dve", ybf[:, h * 128:(h + 1) * 128], osb[:, :], rz[:, 3:4], subgB[:, :], ALU.mult, ALU.mult,
                    [osb.k, rz.k, subgB.k], [ybf.k])


        def gla_part():
            mm(psG[:, 0:256], ggT[:, :], wg2[:, :], True, False, [ggT.k, wg2.k], [psG.k], inc=False)
            mm(psG[:, 0:256], onesr[:, :], bgr[:, :], False, True, [onesr.k, bgr.k], [psG.k])
            act(la[:, :], psG[:, 0:256], AF.Exp, [psG.k], [la.k], scale=-1.0)
            act(la[:, :], la[:, :], AF.Ln, [la.k, cvals.k], [la.k], bias=cvals[:, 2:3])
            ts("dve", la[:, :], la[:, :], -1.0 / 16, None, ALU.mult, None, [la.k], [la.k])
            if GSUB < 1:
                return
            mm(psG[:, 0:256], C("mcum"), la[:, :], True, True, [cst.k, la.k], [psG.k], inc=False)
            mm(psG[:, 256:512], C("mmid"), la[:, :], True, True, [cst.k, la.k], [psG.k])
            mm(psG2[:, 0:256], C("mblk"), la[:, :], True, True, [cst.k, la.k], [psG2.k], inc=False)
            for hp in range(2):
                for hh in range(2):
                    h = hp * 2 + hh
                    mm(psG2[hh * 64:(hh + 1) * 64, 256 + hp * 2:256 + hp * 2 + 2], la[:, h * 64:(h + 1) * 64],
                       C("chunkind"), True, True, [la.k, cst.k], [psG2.k], inc=(hp == 1 and hh == 1))
            if GSUB < 2:
                return
            bc, d1, eq, ek, eb, d2, ed, qg = gl
            cp("dve", bc[:, :], psG[:, 0:256], [psG.k], [bc.k])
            tt("dve", d1[:, :], bc[:, :], psG[:, 256:512], ALU.subtract, [bc.k, psG.k], [d1.k])
            tt("dve", d2[:, :], psG2[:, 0:256], bc[:, :], ALU.subtract, [bc.k, psG2.k], [d2.k])
            act(dec[:, :, :], psG2[:, 256:260].rearrange("p (a c) -> p a c", c=2), AF.Exp, [psG2.k], [dec.k])
            act(eq[:, :], d1[:, :], AF.Exp, [d1.k], [eq.k])
            act(ek[:, :], d1[:, :], AF.Exp, [d1.k], [ek.k], scale=-1.0)
            act(eb[:, :], bc[:, :], AF.Exp, [bc.k], [eb.k])
            act(ed[:, :], d2[:, :], AF.Exp, [d2.k], [ed.k])
            stt("dve", eq[:, :], gqk[:, 0:256], 0.125, eq[:, :], ALU.mult, ALU.mult, [gqk.k, eq.k], [eq.k])
            tt("dve", ek[:, :], gqk[:, 256:512], ek[:, :], ALU.mult, [gqk.k, ek.k], [ek.k])
            stt("dve", eb[:, :], gqk[:, 0:256], 0.125, eb[:, :], ALU.mult, ALU.mult, [gqk.k, eb.k], [eb.k])
            tt("dve", ed[:, :], gqk[:, 256:512], ed[:, :], ALU.mult, [gqk.k, ed.k], [ed.k])
            ci = C("chunkind")
            ts("dve", d1[:, :], ed[:, :], ci[:, 0:1], None, ALU.mult, None, [ed.k, cst.k, d1.k], [d1.k])
            ts("dve", d2[:, :], ed[:, :], ci[:, 1:2], None, ALU.mult, None, [ed.k, cst.k, d2.k], [d2.k])
            if GSUB < 3:
                return
            for hp in range(2):
                cs = slice(hp * 128, (hp + 1) * 128)
                mm(psG[:, 0:128], eq[:, cs], C("ident"), True, True, [eq.k, cst.k], [psG.k], inc=False)
                mm(psG[:, 128:256], ek[:, cs], C("ident"), True, True, [ek.k, cst.k], [psG.k], inc=False)
                mm(psG[:, 256:384], eb[:, cs], C("ident"), True, True, [eb.k, cst.k], [psG.k])
                cp("act", T3[:, :, :], psG[:, 0:384].rearrange("p (a t) -> p a t", t=128), [psG.k], [T3.k])
                if GSUB < 4:
                    continue
                st = Sst[hp]
                if i == 0:
                    S.op("dve", lambda e, st=st: e.memset(st[:, :], 0.0), (), [st.k])
                for hh in range(2):
                    h = hp * 2 + hh
                    pr = slice(hh * 64, (hh + 1) * 64)
                    vcols = slice(h * 128, (h + 1) * 128)
                    for c, kdm in enumerate((d1, d2)):
                        mm(psG2[pr, c * 128:(c + 1) * 128], kdm[:, h * 64:(h + 1) * 64], gv[:, vcols], True, True,
                           [kdm.k, gv.k], [psG2.k], inc=False)
                    mm(psG2[:, 256:384], T3[pr, 1, :], T3[pr, 0, :], True, True, [T3.k], [psG2.k])
                    tt("dve", ATs[:, :], psG2[:, 256:384], C("mcum"), ALU.mult, [psG2.k, cst.k], [ATs.k])
                    if GSUB < 5:
                        continue
                    mm(psP[0][:, 0:128], ATs[:, :], gv[:, vcols], True, False, [ATs.k, gv.k], [psP[0].k], inc=False, skip=True)
                    mm(psP[0][0:64, 0:128], T3[pr, 2, 0:64], st[pr, :], False, False, [T3.k, st.k], [psP[0].k], inc=True,
                       skip=True)
                    if GSUB < 6:
                        continue
                    stt("dve", st[pr, :], st[pr, :], dec[pr, hp, 0:1], psG2[pr, 0:128], ALU.mult, ALU.add,
                        [st.k, dec.k, psG2.k], [st.k])
                    mm(psP[0][64:128, 0:128], T3[pr, 2, 64:128], st[pr, :], False, True, [T3.k, st.k], [psP[0].k], skip=True)
                    if GSUB < 7:
                        continue
                    stt("dve", st[pr, :], st[pr, :], dec[pr, hp, 1:2], psG2[pr, 128:256], ALU.mult, ALU.add,
                        [st.k, dec.k, psG2.k], [st.k])
                    if GSUB < 8:
                        continue
                    act(junk2[:, 0:128], psP[0][:, 0:128], AF.Square, [psP[0].k], [junk2.k, rz2.k], accum_out=rz2[:, 3:4])
                    rsqrt_mean(rz2[:, 3:4], rz2[:, 3:4], 128, [rz2.k])
                    stt("dve", osb2[:, :], psP[0][:, 0:128], rz2[:, 3:4], v128[:, 128:256], ALU.mult, ALU.mult,
                        [psP[0].k, rz2.k, v128.k], [osb2.k])
                    tt("dve", ybf[:, 512 + h * 128:512 + (h + 1) * 128], osb2[:, :], sr[:, vcols], ALU.mult,
                       [osb2.k, sr.k], [ykey2])


        if slevel >= 6 and INTERLEAVE:
            co = Co(gla_part)
            S.co = co
            attn_part()
            S.co = None
            co.finish()
        else:
            attn_part()
            if slevel >= 6:
                gla_part()
        if slevel < 7:
            return
        for j in range(8):
            tr(psT[:, j * 128:(j + 1) * 128], ybf[:, j * 128:(j + 1) * 128], identb[:, :],
               [ybf.k, ykey2, identb.k], [psT.k], inc=(j == 7))
        cp("act", yT[:, :, :], psT[:, :].rearrange("p (j t) -> p j t", t=128), [psT.k], [yT.k])
        for half in range(2):
            r = ring_next()
            ld(r[:, :, :], wout_s[:, half * 512:(half + 1) * 512].rearrange("(k p) n -> p k n", p=128), r, reads=[k_wout_s])
            ps = psP[half]
            for kc in range(8):
                mm(ps[:, :], yT[:, kc, :], r[:, kc, :], kc == 0, kc == 7, [yT.k, r.k], [ps.k],
                   inc=(kc == 7))
            cs = slice(half * 512, (half + 1) * 512)
            tt("dve", sq[:, :], ps[:, :], gt1B[:, cs], ALU.mult, [ps.k, gt1B.k], [sq.k])
            tt("dve", x1[:, slot, cs], sq[:, :], x1[:, slot, cs], ALU.add, [sq.k, xkey], [xkey])
        if do_peer:
            norm_T(xs, xkey, gm2, 2, h2T, slot * 128)

    if do_peer:
        qpc = [sb("qpc%d" % i, [128, ST * 128], BF16) for i in range(2)]
        s1m = sb("s1m", [128, ST, 8, 128], F32)
        s2m = sb("s2m", [128, ST, 8, 128], F32)
        wk = sb("wk", [128, 256], F32)
        v1 = sb("v1", [128, 8, 16], F32)
        v2 = sb("v2", [128, 8, 16], F32)
        c24 = sb("c24", [128, 8, 24], F32)
        thr = sb("thr", [128, 8], F32)
        nrm = sb("nrm", [128, 8], F32)
        ex16 = sb("ex16", [128, 8, 16], F32)
        DG = [sb("DG%d" % t, [128, 8, 128], BF16) for t in range(ST)]
        zbs = [sb("zb%d" % i, [128, 8, 2, 128], BF16) for i in range(NZBUF)]
        zk2s = [Key("zk2_%d" % i) for i in range(NZBUF)]
        zb = sb("candb", [128, 8, 256], F32)
        cand = zb[:, :, :]
        wbs = [sb("wb%d" % i, [128, 8, 2, 128], BF16) for i in range(2)]
        mbs = [sb("mb%d" % i, [128, 8, 2, 128], BF16) for i in range(2)]
        gas = [sb("ga%d" % i, [128, 2, ST * 128], BF16) for i in range(2)]
        GTs = [sb("GT%d" % i, [128, 2, 128], BF16) for i in range(2)]

    out_toks = []

    def top16(src_ap, src_keys, dst, h):
        n = src_ap.shape[-1]
        S.op("dve", lambda e: e.max(out=dst[:, h, 0:8], in_=src_ap), src_keys, [dst.k])
        S.op("dve", lambda e: e.match_replace(out=wk[:, 0:n], in_to_replace=dst[:, h, 0:8], in_values=src_ap,
                                              imm_value=-BIG), list(src_keys) + [dst.k], [wk.k])
        S.op("dve", lambda e: e.max(out=dst[:, h, 8:16], in_=wk[:, 0:n]), [wk.k], [dst.k])

    def peer_supertile(b, st_i):
        T2 = ST * 128
        for blk in range(4):
            r = ring_next()
            ld(r[:, :, :], wq_s[:, blk * 512:(blk + 1) * 512].rearrange("(k p) n -> p k n", p=128), r, reads=[k_wq_s])
            for c4 in range(4):
                cc = blk * 4 + c4
                h, half = cc // 2, cc % 2
                ps = psP[cc % 2]
                qc = qpc[cc % 2]
                for kc in range(8):
                    mm(ps[:, 0:T2], r[:, kc, c4 * 128:(c4 + 1) * 128], h2T[:, kc, :], kc == 0, kc == 7,
                       [r.k, h2T.k], [ps.k], inc=(kc == 7))
                act(qc[:, :], ps[:, 0:T2], AF.Identity, [ps.k, bqT.k], [qc.k], bias=bqT[:, cc:cc + 1])
                pss = psS[cc % 2]
                kT = k1T if half == 0 else k2T
                dstm = s1m if half == 0 else s2m
                for t in range(ST):
                    mm(pss[:, t * 128:(t + 1) * 128], qc[:, t * 128:(t + 1) * 128], kT[:, :], True, True,
                       [qc.k, kT.k], [pss.k], inc=(t == ST - 1))
                cp("dve", dstm[:, :, h, :], pss[:, 0:T2].rearrange("p (t k) -> p t k", k=128), [pss.k], [dstm.k])
        for t in range(ST):
            for h in range(8):
                top16(s1m[:, t, h, :], [s1m.k], v1, h)
                top16(s2m[:, t, h, :], [s2m.k], v2, h)
            tt("dve", cand.rearrange("p h (a b) -> p h a b", b=16),
               v1[:, :, :].unsqueeze(3).to_broadcast([128, 8, 16, 16]),
               v2[:, :, :].unsqueeze(2).to_broadcast([128, 8, 16, 16]), ALU.add, [v1.k, v2.k], [zb.k])
            for h in range(8):
                top16(cand[:, h, :], [zb.k], c24, h)
                S.op("dve", lambda e, h=h: e.match_replace(out=wk[:, :], in_to_replace=c24[:, h, 8:16],
                                                           in_values=wk[:, :], imm_value=-BIG),
                     [wk.k, c24.k], [wk.k])
                S.op("dve", lambda e, h=h: e.max(out=c24[:, h, 16:24], in_=wk[:, :]), [wk.k], [c24.k])
            tt("dve", thr[:, :], c24[:, :, 15], c24[:, :, 16], ALU.add, [c24.k], [thr.k])
            ts("dve", thr[:, :], thr[:, :], 0.5, None, ALU.mult, None, [thr.k], [thr.k])
            tt("dve", ex16[:, :, :], c24[:, :, 0:16], thr[:, :].unsqueeze(2).to_broadcast([128, 8, 16]),
               ALU.subtract, [c24.k, thr.k], [ex16.k])
            act(ex16[:, :, :], ex16[:, :, :], AF.Exp, [ex16.k], [ex16.k])
            red("dve", nrm[:, :], ex16[:, :, :], ALU.add, [ex16.k], [nrm.k])
            S.op("dve", lambda e: e.reciprocal(out=nrm[:, :], in_=nrm[:, :]), [nrm.k], [nrm.k])
            ts("dve", nrm[:, :], nrm[:, :], 1.0 / CSH, None, ALU.mult, None, [nrm.k], [nrm.k])
            for h in range(8):
                ts("dve", DG[t][:, h, :], C("ident"), nrm[:, h:h + 1], None, ALU.mult, None, [cst.k, nrm.k],
                   [DG[t].k])
            for (sm, vv, sub_thr) in ((s1m, v1, True), (s2m, v2, False)):
                mskt = zb[:, :, 0:128]
                tt("dve", mskt, sm[:, t, :, :], vv[:, :, 15:16].to_broadcast([128, 8, 128]), ALU.is_lt,
                   [sm.k, vv.k], [zb.k])
                stt("dve", sm[:, t, :, :], mskt, -BIG, sm[:, t, :, :], ALU.mult, ALU.add,
                    [zb.k, sm.k], [sm.k])
                if sub_thr:
                    tt("dve", sm[:, t, :, :], sm[:, t, :, :], thr[:, :].unsqueeze(2).to_broadcast([128, 8, 128]),
                       ALU.subtract, [sm.k, thr.k], [sm.k])
                act(sm[:, t, :, :], sm[:, t, :, :], AF.Exp, [sm.k], [sm.k])
                if sub_thr:
                    ts("dve", sm[:, t, :, :], sm[:, t, :, :], CSH, None, ALU.mult, None, [sm.k], [sm.k])
        psWs = (psS[0], psP[0])
        psA = psS[1]
        psV = ((psO, psG), (psG2, psP[1]))
        NG = NEXP // 512
        slots = {}

        def load_group(g):
            ru = ring_next()
            ld(ru[:, :, :], uT_s[:, g * 512:(g + 1) * 512].rearrange("(k p) n -> p k n", p=128), ru, reads=[k_uT_s])
            rv = ring_next()
            rv4 = rv[:, :, :].rearrange("p (c a) n -> p c (a n)", a=2)
            ld(rv4, v_s[g * 512:(g + 1) * 512, :].rearrange("(c p) d -> p c d", p=128), rv, reads=[k_v_s])
            slots[g] = (ru, rv, rv4)

        def stage_A_mm(g, sub):
            ru = slots[g][0]
            for ii in range(2):
                ec = sub * 2 + ii
                for kc in range(8):
                    mm(psA[:, ii * T2:(ii + 1) * T2], ru[:, kc, ec * 128:(ec + 1) * 128], h2T[:, kc, :],
                       kc == 0, kc == 7, [ru.k, h2T.k], [psA.k], inc=(kc == 7))

        def stage_A_gelu(g, sub):
            gt_ = gas[(g * 2 + sub) % 2]
            act(gt_[:, :, :], psA[:, 0:2 * T2].rearrange("p (a t) -> p a t", t=T2), AF.Gelu, [psA.k], [gt_.k])

        its = [(g, sub, t) for g in range(NG) for sub in range(2) for t in range(ST)]

        NZ = len(zbs)

        NHA = NHA_ACT

        def stage_P(k):
            g, sub, t = its[k]
            i0 = g * 4 + sub * 2
            zt = zbs[k % NZ]
            zk2 = zk2s[k % NZ]
            for h in range(NHA):
                for ii in range(2):
                    act(zt[:, h, ii, :], s2m[:, t, h, :], AF.Identity, [s1m.k, s2m.k], [zt.k],
                        scale=s1m[:, t, h, i0 + ii:i0 + ii + 1], ww_ok=True)
            nd = 8 - NHA
            tt("dve", zt[:, NHA:8, :, :],
               s1m[:, t, NHA:8, i0:i0 + 2].unsqueeze(3).to_broadcast([128, nd, 2, 128]),
               s2m[:, t, NHA:8, :].unsqueeze(2).to_broadcast([128, nd, 2, 128]), ALU.mult,
               [s1m.k, s2m.k], [zk2])

        def stage_M(k):
            zt = zbs[k % NZ]
            zk2 = zk2s[k % NZ]
            wt = wbs[k % 2]
            mt = mbs[k % 2]
            ts("dve", mt[:, :, :, :], zt[:, :, :, :], 1.0, None, ALU.is_ge, None, [zt.k, zk2], [mt.k])
            tt("dve", wt[:, :, :, :], zt[:, :, :, :], mt[:, :, :, :], ALU.mult, [zt.k, zk2, mt.k], [wt.k])

        def stage_W(k):
            g, sub, t = its[k]
            wt = wbs[k % 2]
            pw = psWs[k % 2]
            first = True
            for h in range(8):
                for ii in range(2):
                    mm(pw[:, ii * 128:(ii + 1) * 128], wt[:, h, ii, :], DG[t][:, h, :], first, (h == 7),
                       [wt.k, DG[t].k], [pw.k], inc=(h == 7 and ii == 1), skip=True)
                    first = False

        def stage_G(k):
            g, sub, t = its[k]
            pw = psWs[k % 2]
            gt_ = gas[(g * 2 + sub) % 2]
            tt("dve", GTs[k % 2][:, :, :], gt_[:, :, t * 128:(t + 1) * 128],
               pw[:, 0:256].rearrange("p (a t) -> p a t", t=128), ALU.mult, [gt_.k, pw.k], [GTs[k % 2].k])

        def stage_V(k):
            g, sub, t = its[k]
            rv, rv4 = slots[g][1], slots[g][2]
            for ii in range(2):
                ec = sub * 2 + ii
                for half in range(2):
                    pv = psV[t][half]
                    mm(pv[:, :], GTs[k % 2][:, ii, :], rv4[:, ec, half * 512:(half + 1) * 512],
                       (g == 0 and sub == 0 and ii == 0), (g == NG - 1 and sub == 1 and ii == 1),
                       [GTs[k % 2].k, rv.k], [pv.k], inc=(ii == 1 and half == 1))

        N = len(its)
        load_group(0)
        stage_A_mm(0, 0)
        stage_A_gelu(0, 0)
        stage_P(0)
        for k in range(N + 1):
            if k + 1 < N:
                stage_P(k + 1)
            if k < N:
                stage_M(k)
                stage_W(k)
            if k >= 1:
                stage_G(k - 1)
                stage_V(k - 1)
            if k < N:
                g, sub, t = its[k]
                ng, nsub = (g, 1) if sub == 0 else (g + 1, 0)
                if ng < NG:
                    if t == 0:
                        if nsub == 0:
                            load_group(ng)
                        stage_A_mm(ng, nsub)
                    else:
                        stage_A_gelu(ng, nsub)
        for t in range(ST):
            for half in range(2):
                cs = slice(half * 512, (half + 1) * 512)
                tt("dve", sq[:, :], psV[t][half][:, :], gt2B[:, cs], ALU.mult, [psV[t][half].k, gt2B.k], [sq.k])
                tt("dve", x1[:, t, cs], sq[:, :], x1[:, t, cs], ALU.add, [sq.k, xk[t]], [xk[t]])
            tok0 = b * SEQ + (st_i * ST + t) * 128
            out_toks.append(S.dma("sp", y_d[tok0:tok0 + 128, :], x1[:, t, :], owner=xk[t], reads=[xk[t]]))

    for b in range(NB):
        if slevel >= 1:
            adaln(b)
        for st_i in range(NST):
            for t in range(ST):
                mixer_tile(b, st_i * ST + t, t)
            if do_peer:
                peer_supertile(b, st_i)
            else:
                for t in range(ST):
                    tok0 = b * SEQ + (st_i * ST + t) * 128
                    out_toks.append(S.dma("sp", y_d[tok0:tok0 + 128, :], x1[:, t, :], owner=xk[t], reads=[xk[t]]))
    print("sbuf bytes remaining", nc.sbuf_bytes_remaining)
    S.wait_all("sp", out_toks + dbg_toks)
    S.emit()
    return nc, S


def make_in_maps(inputs, n_cores, NB, SEQ):
    f = lambda a: np.ascontiguousarray(np.asarray(a, dtype=np.float32))
    x = f(inputs["x"])
    c = f(inputs["c"])
    pos = np.asarray(inputs["positions"]).astype(np.int32)
    NT = SEQ // 128
    cblob, _ = _consts()
    shared = {
        "w_ada": f(inputs["w_ada"][0]),
        "b_adaT": f(np.asarray(inputs["b_ada"][0]).reshape(48, 128).T),
        "b_ada": f(np.asarray(inputs["b_ada"][0]).reshape(1, -1)),
        "g1T": f(np.asarray(inputs["norm1_g"][0]).reshape(8, 128).T),
        "g2T": f(np.asarray(inputs["norm2_g"][0]).reshape(8, 128).T),
        "w_in": f(inputs["w_in"][0]),
        "w_out": f(inputs["w_out"][0]),
        "w_query": f(inputs["w_query"][0]),
        "b_queryT": f(np.asarray(inputs["b_query"][0]).reshape(16, 128).T),
        "keys1T": f(np.asarray(inputs["peer_keys1"][0]).T),
        "keys2T": f(np.asarray(inputs["peer_keys2"][0]).T),
        "uT": f(np.asarray(inputs["expert_u"][0]).T),
        "ev": f(inputs["expert_v"][0]),
        "vec64": f(np.concatenate([np.asarray(inputs[k][0]).reshape(-1) for k in
                                   ("qn_g", "kn_g", "lam_q1", "lam_k1", "lam_q2", "lam_k2")]).reshape(1, -1)),
        "vec128": f(np.concatenate([np.asarray(inputs[k][0]).reshape(-1) for k in
                                    ("diff_norm_g", "gla_norm_g")]).reshape(1, -1)),
        "w_gate2": f(inputs["w_gate2"][0]),
        "b_gate": f(np.asarray(inputs["b_gate"][0]).reshape(1, -1)),
        "consts": cblob,
    }
    maps = []
    for i in range(n_cores):
        bs = slice(i * NB, (i + 1) * NB)
        m = dict(shared)
        m["x"] = np.ascontiguousarray(x[bs].reshape(NB * SEQ, D))
        m["cT"] = np.ascontiguousarray(c[bs].T)
        m["posT"] = np.ascontiguousarray(pos[bs].reshape(NB * NT, 128).T)
        maps.append(m)
    return maps


def kernel(**inputs):
    x = np.asarray(inputs["x"])
    B, SEQ, _ = x.shape
    NB = B // NCORES
    nc, _ = build_program(NB, SEQ, do_peer=True)
    maps = make_in_maps(inputs, NCORES, NB, SEQ)
    res = run_bass_kernel_spmd(nc, maps, core_ids=list(range(NCORES)))
    out = np.concatenate([np.asarray(r["y"]).reshape(NB, SEQ, D) for r in res.results], axis=0)
    return out.astype(np.float32)
```

```python
import math
import os
import threading
import numpy as np
import concourse.bass as bass
import concourse.mybir as mybir
from concourse.bass_utils import run_bass_kernel_spmd

F32 = mybir.dt.float32
BF16 = mybir.dt.bfloat16
I32 = mybir.dt.int32
AF = mybir.ActivationFunctionType
ALU = mybir.AluOpType
AX = mybir.AxisListType

D = 1024
NCORES = 8
EPS = 1e-6
NEXP = 16384
BIG = 1.0e4
GSUB = int(os.environ.get("GSUB", "9"))
INTERLEAVE = int(os.environ.get("INTERLEAVE", "1"))
NHA_ACT = int(os.environ.get("NHA_ACT", "5"))
NZBUF = int(os.environ.get("NZBUF", "3"))
CSH = float(np.float32(1.0 - 2.0 ** -9))


class Key:
    __slots__ = ("name", "excl", "const", "lw", "rd", "sem", "semcnt")

    def __init__(self, name, excl=False, const=False):
        self.name = name
        self.excl = excl
        self.const = const
        self.lw = None
        self.rd = []
        self.sem = None
        self.semcnt = 0


class Co:
    def __init__(self, fn):
        self.go = threading.Semaphore(0)
        self.back = threading.Semaphore(0)
        self.done = False
        self.budget = 0
        self.err = None
        self.th = threading.Thread(target=self._run, args=(fn,), daemon=True)
        self.th.start()

    def _run(self, fn):
        self.go.acquire()
        try:
            fn()
        except BaseException as e:
            self.err = e
        self.done = True
        self.back.release()

    def hook(self):
        if self.budget <= 0:
            self.back.release()
            self.go.acquire()
        self.budget -= 1

    def step(self, n):
        if self.done:
            return
        self.budget = n
        self.go.release()
        self.back.acquire()
        if self.err is not None:
            raise self.err

    def finish(self):
        while not self.done:
            self.step(1000)
        if self.err is not None:
            raise self.err


class Sched:
    def __init__(self, nc):
        self.co = None
        self.nc = nc
        self.engs = ("pe", "act", "dve", "pool", "sp")
        self.prog = {e: [] for e in self.engs}
        self.cnt = {e: 0 for e in self.engs}
        self.seen = {e: {} for e in self.engs}
        self.esem = {e: nc.alloc_semaphore(name="tl_" + e) for e in self.engs}
        self.n_ins = 0

    def _need(self, eng, tok, waits, raw):
        if tok is None:
            return
        if tok[0] == "e":
            _, f, n = tok
            if f == eng and eng == "pe":
                return
            k = ("e", f)
        else:
            _, key, n = tok
            k = ("d", key)
        if self.seen[eng].get(k, 0) >= n:
            return
        if n > waits.get(k, 0):
            waits[k] = n

    def _deps(self, eng, reads, writes, ww_ok=False):
        waits = {}
        for k in reads:
            self._need(eng, k.lw, waits, True)
        for k in writes:
            if not (ww_ok and k.lw is not None and k.lw[0] == "e" and k.lw[1] == eng):
                self._need(eng, k.lw, waits, False)
            for t in k.rd:
                self._need(eng, t, waits, False)
        for k, n in waits.items():
            self.seen[eng][k] = n
            sem = self.esem[k[1]] if k[0] == "e" else k[1].sem
            self.prog[eng].append(("w", sem, n))

    def _commit(self, tok, reads, writes):
        for k in reads:
            if k.excl:
                k.lw = tok
                k.rd = []
            elif not k.const:
                k.rd.append(tok)
        for k in writes:
            k.lw = tok
            k.rd = []

    def _co_hook(self):
        co = self.co
        if co is None:
            return False
        if threading.current_thread() is co.th:
            co.hook()
            return False
        return True

    def op(self, eng, fn, reads=(), writes=(), inc=True, ww_ok=False):
        main_with_co = self._co_hook()
        tok = self._op(eng, fn, reads, writes, inc, ww_ok)
        if main_with_co:
            self.co.step(1)
        return tok

    def _op(self, eng, fn, reads=(), writes=(), inc=True, ww_ok=False):
        self._deps(eng, reads, writes, ww_ok)
        self.n_ins += 1
        if inc:
            self.cnt[eng] += 1
            tok = ("e", eng, self.cnt[eng])
            self.prog[eng].append(("i", fn, self.esem[eng], 1))
        else:
            tok = ("e", eng, self.cnt[eng] + 1)
            self.prog[eng].append(("i", fn, None, 0))
        self._commit(tok, reads, writes)
        return tok

    def dma(self, eng, out, in_, owner, reads=(), writes=(), **kw):
        self._co_hook()
        self._deps(eng, reads, writes)
        if owner.sem is None:
            owner.sem = self.nc.alloc_semaphore(name="d_" + owner.name)
        owner.semcnt += 16
        tok = ("d", owner, owner.semcnt)
        self.n_ins += 1
        self.prog[eng].append(
            ("i", lambda e, o=out, i=in_, kw=kw: e.dma_start(out=o, in_=i, **kw), owner.sem, 16))
        self._commit(tok, reads, writes)
        return tok

    def wait_all(self, eng, toks):
        waits = {}
        for t in toks:
            self._need(eng, t, waits, True)
        for k, n in waits.items():
            self.seen[eng][k] = n
            sem = self.esem[k[1]] if k[0] == "e" else k[1].sem
            self.prog[eng].append(("w", sem, n))

    def emit(self):
        progs = self.prog

        def run(e, lst):
            for it in lst:
                if it[0] == "w":
                    e.wait_ge(it[1], it[2])
                else:
                    ins = it[1](e)
                    if it[2] is not None:
                        ins.then_inc(it[2], it[3])

        with self.nc.Block() as block:
            @block.tensor
            def _(e):
                run(e, progs["pe"])

            @block.scalar
            def _(e):
                run(e, progs["act"])

            @block.vector
            def _(e):
                run(e, progs["dve"])

            @block.gpsimd
            def _(e):
                run(e, progs["pool"])

            @block.sync
            def _(e):
                run(e, progs["sp"])


class Tl:
    def __init__(self, nc, name, shape, dt, psum=False, const=False):
        if psum:
            self.t = nc.alloc_psum_tensor("p_" + name, shape, dt)
        else:
            self.t = nc.alloc_sbuf_tensor("s_" + name, shape, dt)
        self.k = Key(name, excl=psum, const=const)

    def __getitem__(self, idx):
        return self.t[idx]


def _consts():
    p = np.arange(128)
    same = (p[:, None] // 64) == (p[None, :] // 64)
    c = {}
    c["ident"] = np.eye(128, dtype=np.float32)
    c["causal"] = (p[:, None] <= p[None, :]).astype(np.float32)
    c["mcum"] = (same & (p[:, None] <= p[None, :])).astype(np.float32)
    c["mblk"] = same.astype(np.float32)
    c["mmid"] = (same & ((p[:, None] % 64) <= 32)).astype(np.float32)
    c["chunkind"] = np.stack([(p < 64), (p >= 64)], axis=1).astype(np.float32)
    invf = (10000.0 ** (-np.arange(0, 64, 2, dtype=np.float32) / 64)).astype(np.float32)
    c["invf"] = np.tile(invf[None, :], (128, 1)).astype(np.float32)
    order = ["ident", "causal", "mcum", "mblk", "mmid", "chunkind", "invf"]
    offs = {}
    cols = 0
    for n in order:
        offs[n] = (cols, c[n].shape[1])
        cols += c[n].shape[1]
    blob = np.concatenate([c[n] for n in order], axis=1).astype(np.float32)
    return blob, offs


def build_program(NB, SEQ, do_peer=True, dbg=(), stage="full", chain_casts=True):
    STAGES = ["pro", "ada", "norm", "qkv", "proj", "attn", "gla", "full"]
    slevel = STAGES.index(stage)
    NT = SEQ // 128
    ST = 2
    NST = NT // ST
    NTOK = NB * SEQ
    nc = bass.Bass("TRN2", target_bir_lowering=False)
    S = Sched(nc)
    cblob, coffs = _consts()
    CW = cblob.shape[1]

    def din(name, shape, dt=F32):
        return nc.dram_tensor(name, list(shape), dt, kind="ExternalInput").ap()

    x_d = din("x", [NTOK, D])
    cT_d = din("cT", [D, NB])
    pos_d = din("posT", [128, NB * NT], I32)
    wada_d = din("w_ada", [D, 6 * D])
    badaT_d = din("b_adaT", [128, 48])
    bada_d = din("b_ada", [1, 6 * D])
    g1T_d = din("g1T", [128, 8])
    g2T_d = din("g2T", [128, 8])
    win_d = din("w_in", [D, 3088])
    wout_d = din("w_out", [D, D])
    wq_d = din("w_query", [D, 2048])
    bqT_d = din("b_queryT", [128, 16])
    k1T_d = din("keys1T", [128, 128])
    k2T_d = din("keys2T", [128, 128])
    uT_d = din("uT", [D, NEXP])
    v_d = din("ev", [NEXP, D])
    vec64_d = din("vec64", [1, 6 * 64])
    vec128_d = din("vec128", [1, 2 * 128])
    wg2_d = din("w_gate2", [16, 256])
    bg_d = din("b_gate", [1, 256])
    cst_d = din("consts", [128, CW])
    y_d = nc.dram_tensor("y", [NTOK, D], F32, kind="ExternalOutput").ap()
    dbg_d = {n: nc.dram_tensor("dbg_" + n, list(shp), F32, kind="ExternalOutput").ap() for n, shp in dbg}

    win_s = nc.dram_tensor("win_s", [D, 3088], BF16, kind="Internal").ap()
    wout_s = nc.dram_tensor("wout_s", [D, D], BF16, kind="Internal").ap()
    wq_s = nc.dram_tensor("wq_s", [D, 2048], BF16, kind="Internal").ap()
    uT_s = nc.dram_tensor("uT_s", [D, NEXP], BF16, kind="Internal").ap()
    v_s = nc.dram_tensor("v_s", [NEXP, D], BF16, kind="Internal").ap()
    k_win_s, k_wout_s, k_wq_s, k_uT_s, k_v_s = (Key(n, const=True) for n in ("kwin", "kwout", "kwq", "kuT", "kv"))

    def sb(name, shape, dt=F32, const=False):
        return Tl(nc, name, shape, dt, const=const)

    cst = sb("cst", [128, CW], F32, const=True)
    identb = sb("identb", [128, 128], BF16, const=True)
    causb = sb("causb", [128, 128], BF16, const=True)
    onesr = sb("onesr", [1, 128], F32, const=True)
    cT = sb("cT", [128, 8, NB], F32, const=True)
    posi = sb("posi", [128, NB * NT], I32, const=True)
    posf = sb("posf", [128, NB * NT], F32, const=True)
    badaT = sb("badaT", [128, 48], F32, const=True)
    g1T = sb("g1T", [128, 8], F32, const=True)
    g2T = sb("g2T", [128, 8], F32, const=True)
    bqT = sb("bqT", [128, 16], F32, const=True)
    k1T = sb("k1T", [128, 128], BF16, const=True)
    k2T = sb("k2T", [128, 128], BF16, const=True)
    v64 = sb("v64", [128, 6 * 64], F32, const=True)
    v128 = sb("v128", [128, 256], F32, const=True)
    qgB = sb("qgB", [128, 64], F32, const=True)
    subgB = sb("subgB", [128, 128], F32, const=True)
    neglam = sb("neglam", [128, 1], F32, const=True)
    lamt = sb("lamt", [128, 64], F32)
    lams = sb("lams", [128, 4], F32)
    wg2 = sb("wg2", [16, 256], F32, const=True)
    bgr = sb("bgr", [1, 256], F32, const=True)
    cvals = sb("cvals", [128, 4], F32, const=True)

    def C(name):
        o, w = coffs[name]
        return cst[:, o:o + w]

    sT = sb("sT", [128, 8], F32)
    modT = sb("modT", [128, 4, 8], F32)
    gm1 = sb("gm1", [128, 8], F32)
    gm2 = sb("gm2", [128, 8], F32)
    gt1B = sb("gt1B", [128, D], F32)
    gt2B = sb("gt2B", [128, D], F32)
    wst = [sb("wst%d" % i, [128, 8, 128], F32) for i in range(2)]

    NRING = 4
    ring = [sb("ring%d" % i, [128, 8, 512], BF16) for i in range(NRING)]
    rc = [0]

    def ring_next():
        r = ring[rc[0] % NRING]
        rc[0] += 1
        return r

    KT = [sb("KT%d" % i, [128, 4, 128], BF16) for i in range(NT)]
    VC = [sb("VC%d" % i, [128, 4, 130], BF16) for i in range(NT)]
    Sst = [sb("Sst%d" % i, [128, 128], F32) for i in range(2)]

    x1 = sb("x1", [128, ST, D], F32)
    xk = [Key("x1_%d" % i) for i in range(ST)]
    junk = sb("junk", [128, D], BF16)
    xn = sb("xn", [128, D], BF16)
    st1 = sb("st1", [128, 4], F32)
    hT = sb("hT", [128, 8, 128], BF16)
    h2T = sb("h2T", [128, 8, ST * 128], BF16)
    sq = sb("sq", [128, 512], F32)
    ss8 = sb("ss8", [128, 8], F32)
    qn = sb("qn", [128, 512], F32)
    rt = [sb("rt%d" % i, [128, 256], F32) for i in range(2)]
    qr = sb("qr", [128, 512], BF16)
    qT = sb("qT", [128, 4, 128], BF16)
    ang = sb("ang", [128, 32], F32)
    ang2 = sb("ang2", [128, 32], F32)
    ang3 = sb("ang3", [128, 32], F32)
    angi = sb("angi", [128, 32], I32)
    sinT = sb("sinT", [128, 32], F32)
    cosT = sb("cosT", [128, 32], F32)
    PT = [sb("PT%d" % i, [128, 512], BF16) for i in range(2)]
    osb = sb("osb", [128, 128], F32)
    rz = sb("rz", [128, 4], F32)
    ybf = sb("ybf", [128, D], BF16)
    ykey2 = Key("ybf_gla")
    junk2 = sb("junk2", [128, 128], BF16)
    rz2 = sb("rz2", [128, 4], F32)
    osb2 = sb("osb2", [128, 128], F32)
    yT = sb("yT", [128, 8, 128], BF16)
    gqk = sb("gqk", [128, 512], F32)
    gv = sb("gv", [128, 512], F32)
    sr = sb("sr", [128, 512], F32)
    ggT = sb("ggT", [16, 128], F32)
    la = sb("la", [128, 256], F32)
    gl = [sb("gl%d" % i, [128, 256], F32) for i in range(8)]
    dec = sb("dec", [128, 2, 2], F32)
    T3 = sb("T3", [128, 3, 128], F32)
    ATs = sb("ATs", [128, 128], F32)

    psT = Tl(nc, "psT", [128, 1024], BF16, psum=True)
    psP = [Tl(nc, "psP%d" % i, [128, 512], F32, psum=True) for i in range(2)]
    psS = [Tl(nc, "psS%d" % i, [128, 512], F32, psum=True) for i in range(2)]
    psO = Tl(nc, "psO", [128, 512], F32, psum=True)
    psG = Tl(nc, "psG", [128, 512], F32, psum=True)
    psG2 = Tl(nc, "psG2", [128, 512], F32, psum=True)

    def mm(out, lhsT, rhs, start, stop, reads, writes, inc=True, skip=False):
        if skip:
            S.op("pe", lambda e: e.matmul(out, lhsT=lhsT, rhs=rhs, start=start, stop=stop, skip_group_check=True),
                 reads, writes, inc)
        else:
            S.op("pe", lambda e: e.matmul(out, lhsT=lhsT, rhs=rhs, start=start, stop=stop), reads, writes, inc)

    def tr(out, in_, ident, reads, writes, inc=True):
        S.op("pe", lambda e: e.transpose(out, in_, ident), reads, writes, inc)

    def act(out, in_, func, reads, writes, bias=None, scale=None, accum_out=None, ww_ok=False):
        kw = {}
        if bias is not None:
            kw["bias"] = bias
        if scale is not None:
            kw["scale"] = scale
        if accum_out is not None:
            kw["accum_out"] = accum_out
        S.op("act", lambda e: e.activation(out=out, in_=in_, func=func, **kw), reads, writes, ww_ok=ww_ok)

    def tt(eng, out, in0, in1, op, reads, writes):
        S.op(eng, lambda e: e.tensor_tensor(out=out, in0=in0, in1=in1, op=op), reads, writes)

    def ts(eng, out, in0, s1, s2, op0, op1, reads, writes):
        if s2 is None:
            S.op(eng, lambda e: e.tensor_scalar(out=out, in0=in0, scalar1=s1, scalar2=None, op0=op0), reads, writes)
        else:
            S.op(eng, lambda e: e.tensor_scalar(out=out, in0=in0, scalar1=s1, scalar2=s2, op0=op0, op1=op1),
                 reads, writes)

    def stt(eng, out, in0, scalar, in1, op0, op1, reads, writes):
        S.op(eng, lambda e: e.scalar_tensor_tensor(out=out, in0=in0, scalar=scalar, in1=in1, op0=op0, op1=op1),
             reads, writes)

    def cp(eng, out, in_, reads, writes):
        if eng == "act":
            S.op(eng, lambda e: e.activation(out=out, in_=in_, func=AF.Identity), reads, writes)
        else:
            S.op(eng, lambda e: e.tensor_copy(out=out, in_=in_), reads, writes)

    def red(eng, out, in_, op, reads, writes):
        S.op(eng, lambda e: e.tensor_reduce(out=out, in_=in_, axis=AX.X, op=op), reads, writes)

    def rsqrt_mean(dst, src, n, keys):
        act(dst, src, AF.Ln, list(keys) + [cvals.k], keys, bias=cvals[:, 1:2], scale=1.0 / n)
        act(dst, dst, AF.Exp, keys, keys, scale=-0.5)

    def ld(out, in_, tile, reads=(), **kw):
        return S.dma("sp", out, in_, owner=tile.k, reads=reads, writes=[tile.k], **kw)

    def cast_copy(dst, src, key, rows, cols, cchunk):
        for r0 in range(0, rows, 128):
            for c0 in range(0, cols, cchunk):
                c1 = min(cols, c0 + cchunk)
                if chain_casts and key.lw is not None:
                    S.wait_all("pool", [key.lw])
                S.dma("pool", dst[r0:r0 + 128, c0:c1], src[r0:r0 + 128, c0:c1], owner=key, writes=[key])

    cast_copy(win_s, win_d, k_win_s, D, 3088, 3088)
    cast_copy(wout_s, wout_d, k_wout_s, D, D, D)
    cast_copy(wq_s, wq_d, k_wq_s, D, 2048, 2048)
    if do_peer:
        cast_copy(uT_s, uT_d, k_uT_s, D, NEXP, 4096)
        cast_copy(v_s, v_d, k_v_s, NEXP, D, D)

    ld(cst[:, :], cst_d, cst)
    ld(cT[:, :, :], cT_d.rearrange("(k p) b -> p k b", p=128), cT, allow_slow_non_contiguous=True)
    ld(posi[:, :], pos_d, posi)
    ld(badaT[:, :], badaT_d, badaT)
    ld(g1T[:, :], g1T_d, g1T)
    ld(g2T[:, :], g2T_d, g2T)
    ld(bqT[:, :], bqT_d, bqT)
    ld(v64[:, :], vec64_d[0:1, :].to_broadcast([128, 384]), v64)
    ld(v128[:, :], vec128_d[0:1, :].to_broadcast([128, 256]), v128)
    ld(wg2[:, :], wg2_d, wg2)
    ld(bgr[:, :], bg_d, bgr)
    S.op("dve", lambda e: e.memset(onesr[:, :], 1.0), (), [onesr.k])
    S.op("dve", lambda e: e.memset(cvals[:, 0:1], -math.pi), (), [cvals.k])
    S.op("dve", lambda e: e.memset(cvals[:, 1:2], EPS), (), [cvals.k])
    S.op("dve", lambda e: e.memset(cvals[:, 2:3], 1.0), (), [cvals.k])
    S.op("dve", lambda e: e.memset(cvals[:, 3:4], 0.0), (), [cvals.k])
    cp("dve", identb[:, :], C("ident"), [cst.k], [identb.k])
    cp("dve", causb[:, :], C("causal"), [cst.k], [causb.k])
    cp("dve", posf[:, :], posi[:, :], [posi.k], [posf.k])
    ld(ATs[:, :], k1T_d, ATs)
    cp("dve", k1T[:, :], ATs[:, :], [ATs.k], [k1T.k])
    ld(ATs[:, :], k2T_d, ATs)
    cp("dve", k2T[:, :], ATs[:, :], [ATs.k], [k2T.k])
    ts("dve", qgB[:, :], v64[:, 0:64], 0.125, None, ALU.mult, None, [v64.k], [qgB.k])
    ts("dve", subgB[:, :], v128[:, 0:128], 0.8, None, ALU.mult, None, [v128.k], [subgB.k])
    tt("dve", lamt[:, :], v64[:, 128:192], v64[:, 192:256], ALU.mult, [v64.k], [lamt.k])
    red("dve", lams[:, 0:1], lamt[:, :], ALU.add, [lamt.k], [lams.k])
    tt("dve", lamt[:, :], v64[:, 256:320], v64[:, 320:384], ALU.mult, [v64.k, lams.k], [lamt.k])
    red("dve", lams[:, 1:2], lamt[:, :], ALU.add, [lamt.k], [lams.k])
    act(lams[:, 2:4], lams[:, 0:2], AF.Exp, [lams.k], [lams.k])
    tt("dve", lams[:, 0:1], lams[:, 3:4], lams[:, 2:3], ALU.subtract, [lams.k], [lams.k])
    ts("dve", neglam[:, :], lams[:, 0:1], -0.2, None, ALU.add, None, [lams.k], [neglam.k])
    for i in range(NT):
        S.op("pool", lambda e, i=i: e.memset(VC[i][:, :, :], 1.0), (), [VC[i].k])

    def adaln(b):
        act(sT[:, :], cT[:, :, b], AF.Silu, [cT.k], [sT.k])
        ld(gt1B[:, :], bada_d[0:1, 2 * D:3 * D].to_broadcast([128, D]), gt1B)
        ld(gt2B[:, :], bada_d[0:1, 5 * D:6 * D].to_broadcast([128, D]), gt2B)
        wi = 0
        for sec in range(6):
            for q8 in range(8):
                w = wst[wi % 2]
                wi += 1
                c0 = sec * D + q8 * 128
                ld(w[:, :, :], wada_d[:, c0:c0 + 128].rearrange("(k p) n -> p k n", p=128), w)
                ps = psP[wi % 2]
                if sec in (2, 5):
                    for kc in range(8):
                        mm(ps[:, 0:128], sT[:, kc:kc + 1].to_broadcast([128, 128]), w[:, kc, :],
                           kc == 0, kc == 7, [sT.k, w.k], [ps.k], inc=(kc == 7))
                    g = gt1B if sec == 2 else gt2B
                    tt("dve", g[:, q8 * 128:(q8 + 1) * 128], g[:, q8 * 128:(q8 + 1) * 128], ps[:, 0:128], ALU.add,
                       [ps.k, g.k], [g.k])
                else:
                    mi = {0: 0, 1: 1, 3: 2, 4: 3}[sec]
                    for kc in range(8):
                        mm(ps[:, 0:1], w[:, kc, :], sT[:, kc:kc + 1],
                           kc == 0, kc == 7, [sT.k, w.k], [ps.k], inc=(kc == 7))
                    tt("dve", modT[:, mi, q8:q8 + 1], ps[:, 0:1],
                       badaT[:, sec * 8 + q8:sec * 8 + q8 + 1], ALU.add, [ps.k, badaT.k], [modT.k])
        stt("dve", gm1[:, :], modT[:, 1, :], 1.0, g1T[:, :], ALU.add, ALU.mult, [modT.k, g1T.k], [gm1.k])
        stt("dve", gm2[:, :], modT[:, 3, :], 1.0, g2T[:, :], ALU.add, ALU.mult, [modT.k, g2T.k], [gm2.k])

    def norm_T(xap, xkey, gm, shi, dst, dcol):
        act(junk[:, :], xap, AF.Square, [xkey], [junk.k, st1.k], accum_out=st1[:, 0:1])
        rsqrt_mean(st1[:, 2:3], st1[:, 0:1], D, [st1.k])
        ts("dve", xn[:, :], xap, st1[:, 2:3], None, ALU.mult, None, [xkey, st1.k], [xn.k])
        for j in range(8):
            tr(psT[:, j * 128:(j + 1) * 128], xn[:, j * 128:(j + 1) * 128], identb[:, :],
               [xn.k, identb.k], [psT.k], inc=(j == 7))
        for j in range(8):
            act(dst[:, j, dcol:dcol + 128], psT[:, j * 128:(j + 1) * 128], AF.Identity,
                [psT.k, gm.k, modT.k], [dst.k], bias=modT[:, shi, j:j + 1], scale=gm[:, j:j + 1])

    def rope_tables(col):
        C1 = 6.28125
        C2 = 2.0 * math.pi - C1
        ts("dve", ang[:, :], C("invf"), posf[:, col:col + 1], None, ALU.mult, None, [cst.k, posf.k], [ang.k])
        for (shift, dst) in ((0.0, sinT), (0.5 * math.pi, cosT)):
            ts("dve", ang2[:, :], ang[:, :], shift, 1.0 / (2.0 * math.pi), ALU.add, ALU.mult, [ang.k], [ang2.k])
            cp("dve", angi[:, :], ang2[:, :], [ang2.k], [angi.k])
            cp("dve", ang2[:, :], angi[:, :], [angi.k], [ang2.k])
            stt("dve", ang3[:, :], ang2[:, :], -C1, ang[:, :], ALU.mult, ALU.add, [ang2.k, ang.k], [ang3.k])
            stt("dve", ang3[:, :], ang2[:, :], -C2, ang3[:, :], ALU.mult, ALU.add, [ang2.k, ang3.k], [ang3.k])
            ts("dve", ang3[:, :], ang3[:, :], shift, math.pi, ALU.add, ALU.min, [ang3.k], [ang3.k])
            ts("dve", ang3[:, :], ang3[:, :], -math.pi, None, ALU.max, None, [ang3.k], [ang3.k])
            act(dst[:, :], ang3[:, :], AF.Sin, [ang3.k], [dst.k])

    def proj_block(c0, ncols, ps, lhs=None):
        r = ring_next()
        ld(r[:, :, 0:ncols], win_s[:, c0:c0 + ncols].rearrange("(k p) n -> p k n", p=128), r, reads=[k_win_s])
        for kc in range(8):
            mm(ps[:, 0:ncols], hT[:, kc, :], r[:, kc, 0:ncols], kc == 0, kc == 7,
               [hT.k, r.k], [ps.k], inc=(kc == 7))
        return r

    def qk_post(ps, gB, dstT, dkey):
        act(sq[:, :], ps[:, :], AF.Square, [ps.k], [sq.k])
        red("dve", ss8[:, :], sq[:, :].rearrange("p (g d) -> p g d", d=64), ALU.add, [sq.k], [ss8.k])
        rsqrt_mean(ss8[:, :], ss8[:, :], 64, [ss8.k])
        q3 = qn[:, :].rearrange("p (g d) -> p g d", d=64)
        tt("dve", q3, ps[:, :].rearrange("p (g d) -> p g d", d=64),
           ss8[:, :].unsqueeze(2).to_broadcast([128, 8, 64]), ALU.mult, [ps.k, ss8.k], [qn.k])
        tt("dve", q3, q3, gB.unsqueeze(1).to_broadcast([128, 8, 64]), ALU.mult, [qn.k, v64.k, qgB.k], [qn.k])
        x1v = q3[:, :, 0:32]
        x2v = q3[:, :, 32:64]
        cb = cosT[:, :].unsqueeze(1).to_broadcast([128, 8, 32])
        sbb = sinT[:, :].unsqueeze(1).to_broadcast([128, 8, 32])
        r0, r1 = (t[:, :].rearrange("p (g d) -> p g d", d=32) for t in rt)
        qr3 = qr[:, :].rearrange("p (g d) -> p g d", d=64)
        tt("dve", r0, x1v, cb, ALU.mult, [qn.k, cosT.k], [rt[0].k])
        tt("dve", r1, x2v, sbb, ALU.mult, [qn.k, sinT.k], [rt[1].k])
        tt("dve", qr3[:, :, 0:32], r0, r1, ALU.subtract, [rt[0].k, rt[1].k], [qr.k])
        tt("dve", r0, x2v, cb, ALU.mult, [qn.k, cosT.k], [rt[0].k])
        tt("dve", r1, x1v, sbb, ALU.mult, [qn.k, sinT.k], [rt[1].k])
        tt("dve", qr3[:, :, 32:64], r0, r1, ALU.add, [rt[0].k, rt[1].k], [qr.k])
        for h in range(4):
            tr(psT[:, h * 128:(h + 1) * 128], qr[:, h * 128:(h + 1) * 128], identb[:, :],
               [qr.k, identb.k], [psT.k], inc=(h == 3))
        cp("dve", dstT[:, :, :], psT[:, 0:512].rearrange("p (h t) -> p h t", t=128),
           [psT.k], [dkey])

    dbg_toks = []

    def dump(name, ap, key):
        if name in dbg_d:
            dbg_toks.append(S.dma("sp", dbg_d[name], ap, owner=key, reads=[key]))

    def mixer_tile(b, i, slot):
        col = b * NT + i
        tok0 = b * SEQ + i * 128
        xs = x1[:, slot, :]
        xkey = xk[slot]
        S.dma("sp", xs, x_d[tok0:tok0 + 128, :], owner=xkey, writes=[xkey])
        if slevel < 2:
            return
        norm_T(xs, xkey, gm1, 0, hT, 0)
        if i == 0:
            rope_tables(col)
        if slevel < 3:
            return
        proj_block(0, 512, psP[0])
        qk_post(psP[0], qgB[:, :], qT, qT.k)
        proj_block(512, 512, psP[1])
        qk_post(psP[1], v64[:, 64:128], KT[i], KT[i].k)
        proj_block(1024, 512, psP[0])
        cp("act", VC[i][:, :, 0:128], psP[0][:, :].rearrange("p (h e) -> p h e", e=128), [psP[0].k], [VC[i].k])
        if slevel < 4:
            return
        proj_block(1536, 512, psP[1])
        cp("act", gqk[:, :], psP[1][:, :], [psP[1].k], [gqk.k])
        proj_block(2048, 512, psP[0])
        cp("act", gv[:, :], psP[0][:, :], [psP[0].k], [gv.k])
        proj_block(2560, 512, psP[1])
        act(sr[:, :], psP[1][:, :], AF.Silu, [psP[1].k], [sr.k])
        r = ring_next()
        ld(r[:, :, 0:16], win_s[:, 3072:3088].rearrange("(k p) n -> p k n", p=128), r, reads=[k_win_s])
        for kc in range(8):
            mm(psP[0][0:16, 0:128], r[:, kc, 0:16], hT[:, kc, :], kc == 0, kc == 7,
               [hT.k, r.k], [psP[0].k], inc=(kc == 7))
        cp("act", ggT[:, :], psP[0][0:16, 0:128], [psP[0].k], [ggT.k])

        if slevel < 5:
            return

        def attn_part():
            nkb = i + 1
            ngrp = (nkb + 3) // 4
            sidx = 0
            for h in range(4):
                for m in range(2):
                    pr = slice(m * 64, (m + 1) * 64)
                    for g in range(ngrp):
                        j0 = g * 4
                        nj = min(4, nkb - j0)
                        pss = psS[sidx % 2]
                        pt = PT[sidx % 2]
                        sidx += 1
                        for jj in range(nj):
                            j = j0 + jj
                            mm(pss[:, jj * 128:(jj + 1) * 128], KT[j][pr, h, :], qT[pr, h, :], True, True,
                               [KT[j].k, qT.k], [pss.k], inc=(jj == nj - 1))
                        act(pt[:, 0:nj * 128], pss[:, 0:nj * 128], AF.Exp, [pss.k], [pt.k])
                        if j0 + nj - 1 == i:
                            dsl = slice((nj - 1) * 128, nj * 128)
                            tt("pool", pt[:, dsl], pt[:, dsl], causb[:, :], ALU.mult, [pt.k, causb.k], [pt.k])
                        for jj in range(nj):
                            j = j0 + jj
                            mm(psO[:, m * 129:(m + 1) * 129], pt[:, jj * 128:(jj + 1) * 128], VC[j][:, h, 0:129],
                               j == 0, j == i, [pt.k, VC[j].k], [psO.k], inc=(jj == nj - 1))
                S.op("dve", lambda e: e.reciprocal(out=rz[:, 0:1], in_=psO[:, 128:129]), [psO.k], [rz.k])
                S.op("dve", lambda e: e.reciprocal(out=rz[:, 1:2], in_=psO[:, 257:258]), [psO.k], [rz.k])
                tt("dve", rz[:, 2:3], rz[:, 1:2], neglam[:, :], ALU.mult, [rz.k, neglam.k], [rz.k])
                ts("dve", osb[:, :], psO[:, 0:128], rz[:, 0:1], None, ALU.mult, None, [psO.k, rz.k], [osb.k])
                stt("dve", osb[:, :], psO[:, 129:257], rz[:, 2:3], osb[:, :], ALU.mult, ALU.add,
                    [psO.k, rz.k, osb.k], [osb.k])
                act(junk[:, 0:128], osb[:, :], AF.Square, [osb.k], [junk.k, rz.k], accum_out=rz[:, 3:4])
                rsqrt_mean(rz[:, 3:4], rz[:, 3:4], 128, [rz.k])
                stt("dve", ybf[:, h * 128:(h + 1) * 128], osb[:, :], rz[:, 3:4], subgB[:, :], ALU.mult, ALU.mult,
                    [osb.k, rz.k, subgB.k], [ybf.k])


        def gla_part():
            mm(psG[:, 0:256], ggT[:, :], wg2[:, :], True, False, [ggT.k, wg2.k], [psG.k], inc=False)
            mm(psG[:, 0:256], onesr[:, :], bgr[:, :], False, True, [onesr.k, bgr.k], [psG.k])
            act(la[:, :], psG[:, 0:256], AF.Exp, [psG.k], [la.k], scale=-1.0)
            act(la[:, :], la[:, :], AF.Ln, [la.k, cvals.k], [la.k], bias=cvals[:, 2:3])
            ts("dve", la[:, :], la[:, :], -1.0 / 16, None, ALU.mult, None, [la.k], [la.k])
            if GSUB < 1:
                return
            mm(psG[:, 0:256], C("mcum"), la[:, :], True, True, [cst.k, la.k], [psG.k], inc=False)
            mm(psG[:, 256:512], C("mmid"), la[:, :], True, True, [cst.k, la.k], [psG.k])
            mm(psG2[:, 0:256], C("mblk"), la[:, :], True, True, [cst.k, la.k], [psG2.k], inc=False)
            for hp in range(2):
                for hh in range(2):
                    h = hp * 2 + hh
                    mm(psG2[hh * 64:(hh + 1) * 64, 256 + hp * 2:256 + hp * 2 + 2], la[:, h * 64:(h + 1) * 64],
                       C("chunkind"), True, True, [la.k, cst.k], [psG2.k], inc=(hp == 1 and hh == 1))
            if GSUB < 2:
                return
            bc, d1, eq, ek, eb, d2, ed, qg = gl
            cp("dve", bc[:, :], psG[:, 0:256], [psG.k], [bc.k])
            tt("dve", d1[:, :], bc[:, :], psG[:, 256:512], ALU.subtract, [bc.k, psG.k], [d1.k])
            tt("dve", d2[:, :], psG2[:, 0:256], bc[:, :], ALU.subtract, [bc.k, psG2.k], [d2.k])
            act(dec[:, :, :], psG2[:, 256:260].rearrange("p (a c) -> p a c", c=2), AF.Exp, [psG2.k], [dec.k])
            act(eq[:, :], d1[:, :], AF.Exp, [d1.k], [eq.k])
            act(ek[:, :], d1[:, :], AF.Exp, [d1.k], [ek.k], scale=-1.0)
            act(eb[:, :], bc[:, :], AF.Exp, [bc.k], [eb.k])
            act(ed[:, :], d2[:, :], AF.Exp, [d2.k], [ed.k])
            stt("dve", eq[:, :], gqk[:, 0:256], 0.125, eq[:, :], ALU.mult, ALU.mult, [gqk.k, eq.k], [eq.k])
            tt("dve", ek[:, :], gqk[:, 256:512], ek[:, :], ALU.mult, [gqk.k, ek.k], [ek.k])
            stt("dve", eb[:, :], gqk[:, 0:256], 0.125, eb[:, :], ALU.mult, ALU.mult, [gqk.k, eb.k], [eb.k])
            tt("dve", ed[:, :], gqk[:, 256:512], ed[:, :], ALU.mult, [gqk.k, ed.k], [ed.k])
            ci = C("chunkind")
            ts("dve", d1[:, :], ed[:, :], ci[:, 0:1], None, ALU.mult, None, [ed.k, cst.k, d1.k], [d1.k])
            ts("dve", d2[:, :], ed[:, :], ci[:, 1:2], None, ALU.mult, None, [ed.k, cst.k, d2.k], [d2.k])
            if GSUB < 3:
                return
            for hp in range(2):
                cs = slice(hp * 128, (hp + 1) * 128)
                mm(psG[:, 0:128], eq[:, cs], C("ident"), True, True, [eq.k, cst.k], [psG.k], inc=False)
                mm(psG[:, 128:256], ek[:, cs], C("ident"), True, True, [ek.k, cst.k], [psG.k], inc=False)
                mm(psG[:, 256:384], eb[:, cs], C("ident"), True, True, [eb.k, cst.k], [psG.k])
                cp("act", T3[:, :, :], psG[:, 0:384].rearrange("p (a t) -> p a t", t=128), [psG.k], [T3.k])
                if GSUB < 4:
                    continue
                st = Sst[hp]
                if i == 0:
                    S.op("dve", lambda e, st=st: e.memset(st[:, :], 0.0), (), [st.k])
                for hh in range(2):
                    h = hp * 2 + hh
                    pr = slice(hh * 64, (hh + 1) * 64)
                    vcols = slice(h * 128, (h + 1) * 128)
                    for c, kdm in enumerate((d1, d2)):
                        mm(psG2[pr, c * 128:(c + 1) * 128], kdm[:, h * 64:(h + 1) * 64], gv[:, vcols], True, True,
                           [kdm.k, gv.k], [psG2.k], inc=False)
                    mm(psG2[:, 256:384], T3[pr, 1, :], T3[pr, 0, :], True, True, [T3.k], [psG2.k])
                    tt("dve", ATs[:, :], psG2[:, 256:384], C("mcum"), ALU.mult, [psG2.k, cst.k], [ATs.k])
                    if GSUB < 5:
                        continue
                    mm(psP[0][:, 0:128], ATs[:, :], gv[:, vcols], True, False, [ATs.k, gv.k], [psP[0].k], inc=False, skip=True)
                    mm(psP[0][0:64, 0:128], T3[pr, 2, 0:64], st[pr, :], False, False, [T3.k, st.k], [psP[0].k], inc=True,
                       skip=True)
                    if GSUB < 6:
                        continue
                    stt("dve", st[pr, :], st[pr, :], dec[pr, hp, 0:1], psG2[pr, 0:128], ALU.mult, ALU.add,
                        [st.k, dec.k, psG2.k], [st.k])
                    mm(psP[0][64:128, 0:128], T3[pr, 2, 64:128], st[pr, :], False, True, [T3.k, st.k], [psP[0].k], skip=True)
                    if GSUB < 7:
                        continue
                    stt("dve", st[pr, :], st[pr, :], dec[pr, hp, 1:2], psG2[pr, 128:256], ALU.mult, ALU.add,
                        [st.k, dec.k, psG2.k], [st.k])
                    if GSUB < 8:
                        continue
                    act(junk2[:, 0:128], psP[0][:, 0:128], AF.Square, [psP[0].k], [junk2.k, rz2.k], accum_out=rz2[:, 3:4])
                    rsqrt_mean(rz2[:, 3:4], rz2[:, 3:4], 128, [rz2.k])
                    stt("dve", osb2[:, :], psP[0][:, 0:128], rz2[:, 3:4], v128[:, 128:256], ALU.mult, ALU.mult,
                        [psP[0].k, rz2.k, v128.k], [osb2.k])
                    tt("dve", ybf[:, 512 + h * 128:512 + (h + 1) * 128], osb2[:, :], sr[:, vcols], ALU.mult,
                       [osb2.k, sr.k], [ykey2])


        if slevel >= 6 and INTERLEAVE:
            co = Co(gla_part)
            S.co = co
            attn_part()
            S.co = None
            co.finish()
        else:
            attn_part()
            if slevel >= 6:
                gla_part()
        if slevel < 7:
            return
        for j in range(8):
            tr(psT[:, j * 128:(j + 1) * 128], ybf[:, j * 128:(j + 1) * 128], identb[:, :],
               [ybf.k, ykey2, identb.k], [psT.k], inc=(j == 7))
        cp("act", yT[:, :, :], psT[:, :].rearrange("p (j t) -> p j t", t=128), [psT.k], [yT.k])
        for half in range(2):
            r = ring_next()
            ld(r[:, :, :], wout_s[:, half * 512:(half + 1) * 512].rearrange("(k p) n -> p k n", p=128), r, reads=[k_wout_s])
            ps = psP[half]
            for kc in range(8):
                mm(ps[:, :], yT[:, kc, :], r[:, kc, :], kc == 0, kc == 7, [yT.k, r.k], [ps.k],
                   inc=(kc == 7))
            cs = slice(half * 512, (half + 1) * 512)
            tt("dve", sq[:, :], ps[:, :], gt1B[:, cs], ALU.mult, [ps.k, gt1B.k], [sq.k])
            tt("dve", x1[:, slot, cs], sq[:, :], x1[:, slot, cs], ALU.add, [sq.k, xkey], [xkey])
        if do_peer:
            norm_T(xs, xkey, gm2, 2, h2T, slot * 128)
        if i + 1 < NT:
            rope_tables(col + 1)

    if do_peer:
        qpc = [sb("qpc%d" % i, [128, ST * 128], BF16) for i in range(2)]
        s1m = sb("s1m", [128, ST, 8, 128], F32)
        s2m = sb("s2m", [128, ST, 8, 128], F32)
        wk = sb("wk", [128, 256], F32)
        v1 = sb("v1", [128, 8, 16], F32)
        v2 = sb("v2", [128, 8, 16], F32)
        c24 = sb("c24", [128, 8, 24], F32)
        thr = sb("thr", [128, 8], F32)
        nrm = sb("nrm", [128, 8], F32)
        ex16 = sb("ex16", [128, 8, 16], F32)
        DG = [sb("DG%d" % t, [128, 8, 128], BF16) for t in range(ST)]
        zbs = [sb("zb%d" % i, [128, 8, 2, 128], BF16) for i in range(NZBUF)]
        zk2s = [Key("zk2_%d" % i) for i in range(NZBUF)]
        zb = sb("candb", [128, 8, 256], F32)
        cand = zb[:, :, :]
        wbs = [sb("wb%d" % i, [128, 8, 2, 128], BF16) for i in range(2)]
        mbs = [sb("mb%d" % i, [128, 8, 2, 128], BF16) for i in range(2)]
        gas = [sb("ga%d" % i, [128, 2, ST * 128], BF16) for i in range(2)]
        GTs = [sb("GT%d" % i, [128, 2, 128], BF16) for i in range(2)]

    out_toks = []

    def top16(src_ap, src_keys, dst, h):
        n = src_ap.shape[-1]
        S.op("dve", lambda e: e.max(out=dst[:, h, 0:8], in_=src_ap), src_keys, [dst.k])
        S.op("dve", lambda e: e.match_replace(out=wk[:, 0:n], in_to_replace=dst[:, h, 0:8], in_values=src_ap,
                                              imm_value=-BIG), list(src_keys) + [dst.k], [wk.k])
        S.op("dve", lambda e: e.max(out=dst[:, h, 8:16], in_=wk[:, 0:n]), [wk.k], [dst.k])

    def peer_supertile(b, st_i):
        T2 = ST * 128
        for blk in range(4):
            r = ring_next()
            ld(r[:, :, :], wq_s[:, blk * 512:(blk + 1) * 512].rearrange("(k p) n -> p k n", p=128), r, reads=[k_wq_s])
            for c4 in range(4):
                cc = blk * 4 + c4
                h, half = cc // 2, cc % 2
                ps = psP[cc % 2]
                qc = qpc[cc % 2]
                for kc in range(8):
                    mm(ps[:, 0:T2], r[:, kc, c4 * 128:(c4 + 1) * 128], h2T[:, kc, :], kc == 0, kc == 7,
                       [r.k, h2T.k], [ps.k], inc=(kc == 7))
                act(qc[:, :], ps[:, 0:T2], AF.Identity, [ps.k, bqT.k], [qc.k], bias=bqT[:, cc:cc + 1])
                pss = psS[cc % 2]
                kT = k1T if half == 0 else k2T
                dstm = s1m if half == 0 else s2m
                for t in range(ST):
                    mm(pss[:, t * 128:(t + 1) * 128], qc[:, t * 128:(t + 1) * 128], kT[:, :], True, True,
                       [qc.k, kT.k], [pss.k], inc=(t == ST - 1))
                cp("dve", dstm[:, :, h, :], pss[:, 0:T2].rearrange("p (t k) -> p t k", k=128), [pss.k], [dstm.k])
        for t in range(ST):
            for h in range(8):
                top16(s1m[:, t, h, :], [s1m.k], v1, h)
                top16(s2m[:, t, h, :], [s2m.k], v2, h)
            tt("dve", cand.rearrange("p h (a b) -> p h a b", b=16),
               v1[:, :, :].unsqueeze(3).to_broadcast([128, 8, 16, 16]),
               v2[:, :, :].unsqueeze(2).to_broadcast([128, 8, 16, 16]), ALU.add, [v1.k, v2.k], [zb.k])
            for h in range(8):
                top16(cand[:, h, :], [zb.k], c24, h)
                S.op("dve", lambda e, h=h: e.match_replace(out=wk[:, :], in_to_replace=c24[:, h, 8:16],
                                                           in_values=wk[:, :], imm_value=-BIG),
                     [wk.k, c24.k], [wk.k])
                S.op("dve", lambda e, h=h: e.max(out=c24[:, h, 16:24], in_=wk[:, :]), [wk.k], [c24.k])
            tt("dve", thr[:, :], c24[:, :, 15], c24[:, :, 16], ALU.add, [c24.k], [thr.k])
            ts("dve", thr[:, :], thr[:, :], 0.5, None, ALU.mult, None, [thr.k], [thr.k])
            tt("dve", ex16[:, :, :], c24[:, :, 0:16], thr[:, :].unsqueeze(2).to_broadcast([128, 8, 16]),
               ALU.subtract, [c24.k, thr.k], [ex16.k])
            act(ex16[:, :, :], ex16[:, :, :], AF.Exp, [ex16.k], [ex16.k])
            red("dve", nrm[:, :], ex16[:, :, :], ALU.add, [ex16.k], [nrm.k])
            S.op("dve", lambda e: e.reciprocal(out=nrm[:, :], in_=nrm[:, :]), [nrm.k], [nrm.k])
            ts("dve", nrm[:, :], nrm[:, :], 1.0 / CSH, None, ALU.mult, None, [nrm.k], [nrm.k])
            for h in range(8):
                ts("dve", DG[t][:, h, :], C("ident"), nrm[:, h:h + 1], None, ALU.mult, None, [cst.k, nrm.k],
                   [DG[t].k])
            for (sm, vv, sub_thr) in ((s1m, v1, True), (s2m, v2, False)):
                mskt = zb[:, :, 0:128]
                tt("dve", mskt, sm[:, t, :, :], vv[:, :, 15:16].to_broadcast([128, 8, 128]), ALU.is_lt,
                   [sm.k, vv.k], [zb.k])
                stt("dve", sm[:, t, :, :], mskt, -BIG, sm[:, t, :, :], ALU.mult, ALU.add,
                    [zb.k, sm.k], [sm.k])
                if sub_thr:
                    tt("dve", sm[:, t, :, :], sm[:, t, :, :], thr[:, :].unsqueeze(2).to_broadcast([128, 8, 128]),
                       ALU.subtract, [sm.k, thr.k], [sm.k])
                act(sm[:, t, :, :], sm[:, t, :, :], AF.Exp, [sm.k], [sm.k])
                if sub_thr:
                    ts("dve", sm[:, t, :, :], sm[:, t, :, :], CSH, None, ALU.mult, None, [sm.k], [sm.k])
        psWs = (psS[0], psP[0])
        psA = psS[1]
        psV = ((psO, psG), (psG2, psP[1]))
        NG = NEXP // 512
        slots = {}

        def load_group(g):
            ru = ring_next()
            ld(ru[:, :, :], uT_s[:, g * 512:(g + 1) * 512].rearrange("(k p) n -> p k n", p=128), ru, reads=[k_uT_s])
            rv = ring_next()
            rv4 = rv[:, :, :].rearrange("p (c a) n -> p c (a n)", a=2)
            ld(rv4, v_s[g * 512:(g + 1) * 512, :].rearrange("(c p) d -> p c d", p=128), rv, reads=[k_v_s])
            slots[g] = (ru, rv, rv4)

        def stage_A_mm(g, sub):
            ru = slots[g][0]
            for ii in range(2):
                ec = sub * 2 + ii
                for kc in range(8):
                    mm(psA[:, ii * T2:(ii + 1) * T2], ru[:, kc, ec * 128:(ec + 1) * 128], h2T[:, kc, :],
                       kc == 0, kc == 7, [ru.k, h2T.k], [psA.k], inc=(kc == 7))

        def stage_A_gelu(g, sub):
            gt_ = gas[(g * 2 + sub) % 2]
            act(gt_[:, :, :], psA[:, 0:2 * T2].rearrange("p (a t) -> p a t", t=T2), AF.Gelu, [psA.k], [gt_.k])

        its = [(g, sub, t) for g in range(NG) for sub in range(2) for t in range(ST)]

        NZ = len(zbs)

        NHA = NHA_ACT

        def stage_P(k):
            g, sub, t = its[k]
            i0 = g * 4 + sub * 2
            zt = zbs[k % NZ]
            zk2 = zk2s[k % NZ]
            for h in range(NHA):
                for ii in range(2):
                    act(zt[:, h, ii, :], s2m[:, t, h, :], AF.Identity, [s1m.k, s2m.k], [zt.k],
                        scale=s1m[:, t, h, i0 + ii:i0 + ii + 1], ww_ok=True)
            nd = 8 - NHA
            tt("dve", zt[:, NHA:8, :, :],
               s1m[:, t, NHA:8, i0:i0 + 2].unsqueeze(3).to_broadcast([128, nd, 2, 128]),
               s2m[:, t, NHA:8, :].unsqueeze(2).to_broadcast([128, nd, 2, 128]), ALU.mult,
               [s1m.k, s2m.k], [zk2])

        def stage_M(k):
            zt = zbs[k % NZ]
            zk2 = zk2s[k % NZ]
            wt = wbs[k % 2]
            mt = mbs[k % 2]
            ts("dve", mt[:, :, :, :], zt[:, :, :, :], 1.0, None, ALU.is_ge, None, [zt.k, zk2], [mt.k])
            tt("dve", wt[:, :, :, :], zt[:, :, :, :], mt[:, :, :, :], ALU.mult, [zt.k, zk2, mt.k], [wt.k])

        def stage_W(k):
            g, sub, t = its[k]
            wt = wbs[k % 2]
            pw = psWs[k % 2]
            first = True
            for h in range(8):
                for ii in range(2):
                    mm(pw[:, ii * 128:(ii + 1) * 128], wt[:, h, ii, :], DG[t][:, h, :], first, (h == 7),
                       [wt.k, DG[t].k], [pw.k], inc=(h == 7 and ii == 1), skip=True)
                    first = False

        def stage_G(k):
            g, sub, t = its[k]
            pw = psWs[k % 2]
            gt_ = gas[(g * 2 + sub) % 2]
            tt("dve", GTs[k % 2][:, :, :], gt_[:, :, t * 128:(t + 1) * 128],
               pw[:, 0:256].rearrange("p (a t) -> p a t", t=128), ALU.mult, [gt_.k, pw.k], [GTs[k % 2].k])

        def stage_V(k):
            g, sub, t = its[k]
            rv, rv4 = slots[g][1], slots[g][2]
            for ii in range(2):
                ec = sub * 2 + ii
                for half in range(2):
                    pv = psV[t][half]
                    mm(pv[:, :], GTs[k % 2][:, ii, :], rv4[:, ec, half * 512:(half + 1) * 512],
                       (g == 0 and sub == 0 and ii == 0), (g == NG - 1 and sub == 1 and ii == 1),
                       [GTs[k % 2].k, rv.k], [pv.k], inc=(ii == 1 and half == 1))

        N = len(its)
        load_group(0)
        stage_A_mm(0, 0)
        stage_A_gelu(0, 0)
        stage_P(0)
        for k in range(N + 1):
            if k + 1 < N:
                stage_P(k + 1)
            if k < N:
                stage_M(k)
                stage_W(k)
            if k >= 1:
                stage_G(k - 1)
                stage_V(k - 1)
            if k < N:
                g, sub, t = its[k]
                ng, nsub = (g, 1) if sub == 0 else (g + 1, 0)
                if ng < NG:
                    if t == 0:
                        if nsub == 0:
                            load_group(ng)
                        stage_A_mm(ng, nsub)
                    else:
                        stage_A_gelu(ng, nsub)
        for t in range(ST):
            for half in range(2):
                cs = slice(half * 512, (half + 1) * 512)
                tt("dve", sq[:, :], psV[t][half][:, :], gt2B[:, cs], ALU.mult, [psV[t][half].k, gt2B.k], [sq.k])
                tt("dve", x1[:, t, cs], sq[:, :], x1[:, t, cs], ALU.add, [sq.k, xk[t]], [xk[t]])
            tok0 = b * SEQ + (st_i * ST + t) * 128
            out_toks.append(S.dma("sp", y_d[tok0:tok0 + 128, :], x1[:, t, :], owner=xk[t], reads=[xk[t]]))

    for b in range(NB):
        if slevel >= 1:
            adaln(b)
        for st_i in range(NST):
            for t in range(ST):
                mixer_tile(b, st_i * ST + t, t)
            if do_peer:
                peer_supertile(b, st_i)
            else:
                for t in range(ST):
                    tok0 = b * SEQ + (st_i * ST + t) * 128
                    out_toks.append(S.dma("sp", y_d[tok0:tok0 + 128, :], x1[:, t, :], owner=xk[t], reads=[xk[t]]))
    print("sbuf bytes remaining", nc.sbuf_bytes_remaining)
    S.wait_all("sp", out_toks + dbg_toks)
    S.emit()
    return nc, S


def make_in_maps(inputs, n_cores, NB, SEQ):
    f = lambda a: np.ascontiguousarray(np.asarray(a, dtype=np.float32))
    x = f(inputs["x"])
    c = f(inputs["c"])
    pos = np.asarray(inputs["positions"]).astype(np.int32)
    NT = SEQ // 128
    cblob, _ = _consts()
    shared = {
        "w_ada": f(inputs["w_ada"][0]),
        "b_adaT": f(np.asarray(inputs["b_ada"][0]).reshape(48, 128).T),
        "b_ada": f(np.asarray(inputs["b_ada"][0]).reshape(1, -1)),
        "g1T": f(np.asarray(inputs["norm1_g"][0]).reshape(8, 128).T),
        "g2T": f(np.asarray(inputs["norm2_g"][0]).reshape(8, 128).T),
        "w_in": f(inputs["w_in"][0]),
        "w_out": f(inputs["w_out"][0]),
        "w_query": f(inputs["w_query"][0]),
        "b_queryT": f(np.asarray(inputs["b_query"][0]).reshape(16, 128).T),
        "keys1T": f(np.asarray(inputs["peer_keys1"][0]).T),
        "keys2T": f(np.asarray(inputs["peer_keys2"][0]).T),
        "uT": f(np.asarray(inputs["expert_u"][0]).T),
        "ev": f(inputs["expert_v"][0]),
        "vec64": f(np.concatenate([np.asarray(inputs[k][0]).reshape(-1) for k in
                                   ("qn_g", "kn_g", "lam_q1", "lam_k1", "lam_q2", "lam_k2")]).reshape(1, -1)),
        "vec128": f(np.concatenate([np.asarray(inputs[k][0]).reshape(-1) for k in
                                    ("diff_norm_g", "gla_norm_g")]).reshape(1, -1)),
        "w_gate2": f(inputs["w_gate2"][0]),
        "b_gate": f(np.asarray(inputs["b_gate"][0]).reshape(1, -1)),
        "consts": cblob,
    }
    maps = []
    for i in range(n_cores):
        bs = slice(i * NB, (i + 1) * NB)
        m = dict(shared)
        m["x"] = np.ascontiguousarray(x[bs].reshape(NB * SEQ, D))
        m["cT"] = np.ascontiguousarray(c[bs].T)
        m["posT"] = np.ascontiguousarray(pos[bs].reshape(NB * NT, 128).T)
        maps.append(m)
    return maps


def kernel(**inputs):
    x = np.asarray(inputs["x"])
    B, SEQ, _ = x.shape
    NB = B // NCORES
    nc, _ = build_program(NB, SEQ, do_peer=True)
    maps = make_in_maps(inputs, NCORES, NB, SEQ)
    res = run_bass_kernel_spmd(nc, maps, core_ids=list(range(NCORES)))
    out = np.concatenate([np.asarray(r["y"]).reshape(NB, SEQ, D) for r in res.results], axis=0)
    return out.astype(np.float32)
```

```python
import math
import os
import threading
import numpy as np
import concourse.bass as bass
import concourse.mybir as mybir
from concourse.bass_utils import run_bass_kernel_spmd

F32 = mybir.dt.float32
BF16 = mybir.dt.bfloat16
I32 = mybir.dt.int32
AF = mybir.ActivationFunctionType
ALU = mybir.AluOpType
AX = mybir.AxisListType

D = 1024
NCORES = 8
EPS = 1e-6
NEXP = 16384
BIG = 1.0e4
GSUB = int(os.environ.get("GSUB", "9"))
INTERLEAVE = int(os.environ.get("INTERLEAVE", "1"))
NHA_ACT = int(os.environ.get("NHA_ACT", "5"))
NZBUF = int(os.environ.get("NZBUF", "3"))
CSH = float(np.float32(1.0 - 2.0 ** -9))


class Key:
    __slots__ = ("name", "excl", "const", "lw", "rd", "sem", "semcnt")

    def __init__(self, name, excl=False, const=False):
        self.name = name
        self.excl = excl
        self.const = const
        self.lw = None
        self.rd = []
        self.sem = None
        self.semcnt = 0


class Co:
    def __init__(self, fn):
        self.go = threading.Semaphore(0)
        self.back = threading.Semaphore(0)
        self.done = False
        self.budget = 0
        self.err = None
        self.th = threading.Thread(target=self._run, args=(fn,), daemon=True)
        self.th.start()

    def _run(self, fn):
        self.go.acquire()
        try:
            fn()
        except BaseException as e:
            self.err = e
        self.done = True
        self.back.release()

    def hook(self):
        if self.budget <= 0:
            self.back.release()
            self.go.acquire()
        self.budget -= 1

    def step(self, n):
        if self.done:
            return
        self.budget = n
        self.go.release()
        self.back.acquire()
        if self.err is not None:
            raise self.err

    def finish(self):
        while not self.done:
            self.step(1000)
        if self.err is not None:
            raise self.err


class Sched:
    def __init__(self, nc):
        self.co = None
        self.nc = nc
        self.engs = ("pe", "act", "dve", "pool", "sp")
        self.prog = {e: [] for e in self.engs}
        self.cnt = {e: 0 for e in self.engs}
        self.seen = {e: {} for e in self.engs}
        self.esem = {e: nc.alloc_semaphore(name="tl_" + e) for e in self.engs}
        self.n_ins = 0

    def _need(self, eng, tok, waits, raw):
        if tok is None:
            return
        if tok[0] == "e":
            _, f, n = tok
            if f == eng and eng == "pe":
                return
            k = ("e", f)
        else:
            _, key, n = tok
            k = ("d", key)
        if self.seen[eng].get(k, 0) >= n:
            return
        if n > waits.get(k, 0):
            waits[k] = n

    def _deps(self, eng, reads, writes, ww_ok=False):
        waits = {}
        for k in reads:
            self._need(eng, k.lw, waits, True)
        for k in writes:
            if not (ww_ok and k.lw is not None and k.lw[0] == "e" and k.lw[1] == eng):
                self._need(eng, k.lw, waits, False)
            for t in k.rd:
                self._need(eng, t, waits, False)
        for k, n in waits.items():
            self.seen[eng][k] = n
            sem = self.esem[k[1]] if k[0] == "e" else k[1].sem
            self.prog[eng].append(("w", sem, n))

    def _commit(self, tok, reads, writes):
        for k in reads:
            if k.excl:
                k.lw = tok
                k.rd = []
            elif not k.const:
                k.rd.append(tok)
        for k in writes:
            k.lw = tok
            k.rd = []

    def _co_hook(self):
        co = self.co
        if co is None:
            return False
        if threading.current_thread() is co.th:
            co.hook()
            return False
        return True

    def op(self, eng, fn, reads=(), writes=(), inc=True, ww_ok=False):
        main_with_co = self._co_hook()
        tok = self._op(eng, fn, reads, writes, inc, ww_ok)
        if main_with_co:
            self.co.step(1)
        return tok

    def _op(self, eng, fn, reads=(), writes=(), inc=True, ww_ok=False):
        self._deps(eng, reads, writes, ww_ok)
        self.n_ins += 1
        if inc:
            self.cnt[eng] += 1
            tok = ("e", eng, self.cnt[eng])
            self.prog[eng].append(("i", fn, self.esem[eng], 1))
        else:
            tok = ("e", eng, self.cnt[eng] + 1)
            self.prog[eng].append(("i", fn, None, 0))
        self._commit(tok, reads, writes)
        return tok

    def dma(self, eng, out, in_, owner, reads=(), writes=(), **kw):
        self._co_hook()
        self._deps(eng, reads, writes)
        if owner.sem is None:
            owner.sem = self.nc.alloc_semaphore(name="d_" + owner.name)
        owner.semcnt += 16
        tok = ("d", owner, owner.semcnt)
        self.n_ins += 1
        self.prog[eng].append(
            ("i", lambda e, o=out, i=in_, kw=kw: e.dma_start(out=o, in_=i, **kw), owner.sem, 16))
        self._commit(tok, reads, writes)
        return tok

    def wait_all(self, eng, toks):
        waits = {}
        for t in toks:
            self._need(eng, t, waits, True)
        for k, n in waits.items():
            self.seen[eng][k] = n
            sem = self.esem[k[1]] if k[0] == "e" else k[1].sem
            self.prog[eng].append(("w", sem, n))

    def emit(self):
        progs = self.prog

        def run(e, lst):
            for it in lst:
                if it[0] == "w":
                    e.wait_ge(it[1], it[2])
                else:
                    ins = it[1](e)
                    if it[2] is not None:
                        ins.then_inc(it[2], it[3])

        with self.nc.Block() as block:
            @block.tensor
            def _(e):
                run(e, progs["pe"])

            @block.scalar
            def _(e):
                run(e, progs["act"])

            @block.vector
            def _(e):
                run(e, progs["dve"])

            @block.gpsimd
            def _(e):
                run(e, progs["pool"])

            @block.sync
            def _(e):
                run(e, progs["sp"])


class Tl:
    def __init__(self, nc, name, shape, dt, psum=False, const=False):
        if psum:
            self.t = nc.alloc_psum_tensor("p_" + name, shape, dt)
        else:
            self.t = nc.alloc_sbuf_tensor("s_" + name, shape, dt)
        self.k = Key(name, excl=psum, const=const)

    def __getitem__(self, idx):
        return self.t[idx]


def _consts():
    p = np.arange(128)
    same = (p[:, None] // 64) == (p[None, :] // 64)
    c = {}
    c["ident"] = np.eye(128, dtype=np.float32)
    c["causal"] = (p[:, None] <= p[None, :]).astype(np.float32)
    c["mcum"] = (same & (p[:, None] <= p[None, :])).astype(np.float32)
    c["mblk"] = same.astype(np.float32)
    c["mmid"] = (same & ((p[:, None] % 64) <= 32)).astype(np.float32)
    c["chunkind"] = np.stack([(p < 64), (p >= 64)], axis=1).astype(np.float32)
    invf = (10000.0 ** (-np.arange(0, 64, 2, dtype=np.float32) / 64)).astype(np.float32)
    c["invf"] = np.tile(invf[None, :], (128, 1)).astype(np.float32)
    order = ["ident", "causal", "mcum", "mblk", "mmid", "chunkind", "invf"]
    offs = {}
    cols = 0
    for n in order:
        offs[n] = (cols, c[n].shape[1])
        cols += c[n].shape[1]
    blob = np.concatenate([c[n] for n in order], axis=1).astype(np.float32)
    return blob, offs


def build_program(NB, SEQ, do_peer=True, dbg=(), stage="full", chain_casts=True):
    STAGES = ["pro", "ada", "norm", "qkv", "proj", "attn", "gla", "full"]
    slevel = STAGES.index(stage)
    NT = SEQ // 128
    ST = 2
    NST = NT // ST
    NTOK = NB * SEQ
    nc = bass.Bass("TRN2", target_bir_lowering=False)
    S = Sched(nc)
    cblob, coffs = _consts()
    CW = cblob.shape[1]

    def din(name, shape, dt=F32):
        return nc.dram_tensor(name, list(shape), dt, kind="ExternalInput").ap()

    x_d = din("x", [NTOK, D])
    cT_d = din("cT", [D, NB])
    pos_d = din("posT", [128, NB * NT], I32)
    wada_d = din("w_ada", [D, 6 * D])
    badaT_d = din("b_adaT", [128, 48])
    bada_d = din("b_ada", [1, 6 * D])
    g1T_d = din("g1T", [128, 8])
    g2T_d = din("g2T", [128, 8])
    win_d = din("w_in", [D, 3088])
    wout_d = din("w_out", [D, D])
    wq_d = din("w_query", [D, 2048])
    bqT_d = din("b_queryT", [128, 16])
    k1T_d = din("keys1T", [128, 128])
    k2T_d = din("keys2T", [128, 128])
    uT_d = din("uT", [D, NEXP])
    v_d = din("ev", [NEXP, D])
    vec64_d = din("vec64", [1, 6 * 64])
    vec128_d = din("vec128", [1, 2 * 128])
    wg2_d = din("w_gate2", [16, 256])
    bg_d = din("b_gate", [1, 256])
    cst_d = din("consts", [128, CW])
    y_d = nc.dram_tensor("y", [NTOK, D], F32, kind="ExternalOutput").ap()
    dbg_d = {n: nc.dram_tensor("dbg_" + n, list(shp), F32, kind="ExternalOutput").ap() for n, shp in dbg}

    win_s = nc.dram_tensor("win_s", [D, 3088], BF16, kind="Internal").ap()
    wout_s = nc.dram_tensor("wout_s", [D, D], BF16, kind="Internal").ap()
    wq_s = nc.dram_tensor("wq_s", [D, 2048], BF16, kind="Internal").ap()
    uT_s = nc.dram_tensor("uT_s", [D, NEXP], BF16, kind="Internal").ap()
    v_s = nc.dram_tensor("v_s", [NEXP, D], BF16, kind="Internal").ap()
    k_win_s, k_wout_s, k_wq_s, k_uT_s, k_v_s = (Key(n, const=True) for n in ("kwin", "kwout", "kwq", "kuT", "kv"))

    def sb(name, shape, dt=F32, const=False):
        return Tl(nc, name, shape, dt, const=const)

    cst = sb("cst", [128, CW], F32, const=True)
    identb = sb("identb", [128, 128], BF16, const=True)
    causb = sb("causb", [128, 128], BF16, const=True)
    onesr = sb("onesr", [1, 128], F32, const=True)
    cT = sb("cT", [128, 8, NB], F32, const=True)
    posi = sb("posi", [128, NB * NT], I32, const=True)
    posf = sb("posf", [128, NB * NT], F32, const=True)
    badaT = sb("badaT", [128, 48], F32, const=True)
    g1T = sb("g1T", [128, 8], F32, const=True)
    g2T = sb("g2T", [128, 8], F32, const=True)
    bqT = sb("bqT", [128, 16], F32, const=True)
    k1T = sb("k1T", [128, 128], BF16, const=True)
    k2T = sb("k2T", [128, 128], BF16, const=True)
    v64 = sb("v64", [128, 6 * 64], F32, const=True)
    v128 = sb("v128", [128, 256], F32, const=True)
    qgB = sb("qgB", [128, 64], F32, const=True)
    subgB = sb("subgB", [128, 128], F32, const=True)
    neglam = sb("neglam", [128, 1], F32, const=True)
    lamt = sb("lamt", [128, 64], F32)
    lams = sb("lams", [128, 4], F32)
    wg2 = sb("wg2", [16, 256], F32, const=True)
    bgr = sb("bgr", [1, 256], F32, const=True)
    cvals = sb("cvals", [128, 4], F32, const=True)

    def C(name):
        o, w = coffs[name]
        return cst[:, o:o + w]

    sT = sb("sT", [128, 8], F32)
    modT = sb("modT", [128, 4, 8], F32)
    gm1 = sb("gm1", [128, 8], F32)
    gm2 = sb("gm2", [128, 8], F32)
    gt1B = sb("gt1B", [128, D], F32)
    gt2B = sb("gt2B", [128, D], F32)
    wst = [sb("wst%d" % i, [128, 8, 128], F32) for i in range(2)]

    NRING = 4
    ring = [sb("ring%d" % i, [128, 8, 512], BF16) for i in range(NRING)]
    rc = [0]

    def ring_next():
        r = ring[rc[0] % NRING]
        rc[0] += 1
        return r

    KT = [sb("KT%d" % i, [128, 4, 128], BF16) for i in range(NT)]
    VC = [sb("VC%d" % i, [128, 4, 130], BF16) for i in range(NT)]
    Sst = [sb("Sst%d" % i, [128, 128], F32) for i in range(2)]

    x1 = sb("x1", [128, ST, D], F32)
    xk = [Key("x1_%d" % i) for i in range(ST)]
    junk = sb("junk", [128, D], BF16)
    xn = sb("xn", [128, D], BF16)
    st1 = sb("st1", [128, 4], F32)
    hT = sb("hT", [128, 8, 128], BF16)
    h2T = sb("h2T", [128, 8, ST * 128], BF16)
    sq = sb("sq", [128, 512], F32)
    ss8 = sb("ss8", [128, 8], F32)
    qn = sb("qn", [128, 512], F32)
    rt = [sb("rt%d" % i, [128, 256], F32) for i in range(2)]
    qr = sb("qr", [128, 512], BF16)
    qT = sb("qT", [128, 4, 128], BF16)
    ang = sb("ang", [128, 32], F32)
    ang2 = sb("ang2", [128, 32], F32)
    ang3 = sb("ang3", [128, 32], F32)
    angi = sb("angi", [128, 32], I32)
    sinT = sb("sinT", [128, 32], F32)
    cosT = sb("cosT", [128, 32], F32)
    PT = [sb("PT%d" % i, [128, 512], BF16) for i in range(2)]
    osb = sb("osb", [128, 128], F32)
    rz = sb("rz", [128, 4], F32)
    ybf = sb("ybf", [128, D], BF16)
    ykey2 = Key("ybf_gla")
    junk2 = sb("junk2", [128, 128], BF16)
    rz2 = sb("rz2", [128, 4], F32)
    osb2 = sb("osb2", [128, 128], F32)
    yT = sb("yT", [128, 8, 128], BF16)
    gqk = sb("gqk", [128, 512], F32)
    gv = sb("gv", [128, 512], F32)
    sr = sb("sr", [128, 512], F32)
    ggT = sb("ggT", [16, 128], F32)
    la = sb("la", [128, 256], F32)
    gl = [sb("gl%d" % i, [128, 256], F32) for i in range(8)]
    dec = sb("dec", [128, 2, 2], F32)
    T3 = sb("T3", [128, 3, 128], F32)
    ATs = sb("ATs", [128, 128], F32)

    psT = Tl(nc, "psT", [128, 1024], BF16, psum=True)
    psP = [Tl(nc, "psP%d" % i, [128, 512], F32, psum=True) for i in range(2)]
    psS = [Tl(nc, "psS%d" % i, [128, 512], F32, psum=True) for i in range(2)]
    psO = Tl(nc, "psO", [128, 512], F32, psum=True)
    psG = Tl(nc, "psG", [128, 512], F32, psum=True)
    psG2 = Tl(nc, "psG2", [128, 512], F32, psum=True)

    def mm(out, lhsT, rhs, start, stop, reads, writes, inc=True, skip=False):
        if skip:
            S.op("pe", lambda e: e.matmul(out, lhsT=lhsT, rhs=rhs, start=start, stop=stop, skip_group_check=True),
                 reads, writes, inc)
        else:
            S.op("pe", lambda e: e.matmul(out, lhsT=lhsT, rhs=rhs, start=start, stop=stop), reads, writes, inc)

    def tr(out, in_, ident, reads, writes, inc=True):
        S.op("pe", lambda e: e.transpose(out, in_, ident), reads, writes, inc)

    def act(out, in_, func, reads, writes, bias=None, scale=None, accum_out=None, ww_ok=False):
        kw = {}
        if bias is not None:
            kw["bias"] = bias
        if scale is not None:
            kw["scale"] = scale
        if accum_out is not None:
            kw["accum_out"] = accum_out
        S.op("act", lambda e: e.activation(out=out, in_=in_, func=func, **kw), reads, writes, ww_ok=ww_ok)

    def tt(eng, out, in0, in1, op, reads, writes):
        S.op(eng, lambda e: e.tensor_tensor(out=out, in0=in0, in1=in1, op=op), reads, writes)

    def ts(eng, out, in0, s1, s2, op0, op1, reads, writes):
        if s2 is None:
            S.op(eng, lambda e: e.tensor_scalar(out=out, in0=in0, scalar1=s1, scalar2=None, op0=op0), reads, writes)
        else:
            S.op(eng, lambda e: e.tensor_scalar(out=out, in0=in0, scalar1=s1, scalar2=s2, op0=op0, op1=op1),
                 reads, writes)

    def stt(eng, out, in0, scalar, in1, op0, op1, reads, writes):
        S.op(eng, lambda e: e.scalar_tensor_tensor(out=out, in0=in0, scalar=scalar, in1=in1, op0=op0, op1=op1),
             reads, writes)

    def cp(eng, out, in_, reads, writes):
        if eng == "act":
            S.op(eng, lambda e: e.activation(out=out, in_=in_, func=AF.Identity), reads, writes)
        else:
            S.op(eng, lambda e: e.tensor_copy(out=out, in_=in_), reads, writes)

    def red(eng, out, in_, op, reads, writes):
        S.op(eng, lambda e: e.tensor_reduce(out=out, in_=in_, axis=AX.X, op=op), reads, writes)

    def rsqrt_mean(dst, src, n, keys):
        act(dst, src, AF.Ln, list(keys) + [cvals.k], keys, bias=cvals[:, 1:2], scale=1.0 / n)
        act(dst, dst, AF.Exp, keys, keys, scale=-0.5)

    def ld(out, in_, tile, reads=(), **kw):
        return S.dma("sp", out, in_, owner=tile.k, reads=reads, writes=[tile.k], **kw)

    def cast_copy(dst, src, key, rows, cols, cchunk):
        for r0 in range(0, rows, 128):
            for c0 in range(0, cols, cchunk):
                c1 = min(cols, c0 + cchunk)
                if chain_casts and key.lw is not None:
                    S.wait_all("pool", [key.lw])
                S.dma("pool", dst[r0:r0 + 128, c0:c1], src[r0:r0 + 128, c0:c1], owner=key, writes=[key])

    cast_copy(win_s, win_d, k_win_s, D, 3088, 3088)
    cast_copy(wout_s, wout_d, k_wout_s, D, D, D)
    cast_copy(wq_s, wq_d, k_wq_s, D, 2048, 2048)
    if do_peer:
        cast_copy(uT_s, uT_d, k_uT_s, D, NEXP, 4096)
        cast_copy(v_s, v_d, k_v_s, NEXP, D, D)

    ld(cst[:, :], cst_d, cst)
    ld(cT[:, :, :], cT_d.rearrange("(k p) b -> p k b", p=128), cT, allow_slow_non_contiguous=True)
    ld(posi[:, :], pos_d, posi)
    ld(badaT[:, :], badaT_d, badaT)
    ld(g1T[:, :], g1T_d, g1T)
    ld(g2T[:, :], g2T_d, g2T)
    ld(bqT[:, :], bqT_d, bqT)
    ld(v64[:, :], vec64_d[0:1, :].to_broadcast([128, 384]), v64)
    ld(v128[:, :], vec128_d[0:1, :].to_broadcast([128, 256]), v128)
    ld(wg2[:, :], wg2_d, wg2)
    ld(bgr[:, :], bg_d, bgr)
    S.op("dve", lambda e: e.memset(onesr[:, :], 1.0), (), [onesr.k])
    S.op("dve", lambda e: e.memset(cvals[:, 0:1], -math.pi), (), [cvals.k])
    S.op("dve", lambda e: e.memset(cvals[:, 1:2], EPS), (), [cvals.k])
    S.op("dve", lambda e: e.memset(cvals[:, 2:3], 1.0), (), [cvals.k])
    S.op("dve", lambda e: e.memset(cvals[:, 3:4], 0.0), (), [cvals.k])
    cp("dve", identb[:, :], C("ident"), [cst.k], [identb.k])
    cp("dve", causb[:, :], C("causal"), [cst.k], [causb.k])
    cp("dve", posf[:, :], posi[:, :], [posi.k], [posf.k])
    ld(ATs[:, :], k1T_d, ATs)
    cp("dve", k1T[:, :], ATs[:, :], [ATs.k], [k1T.k])
    ld(ATs[:, :], k2T_d, ATs)
    cp("dve", k2T[:, :], ATs[:, :], [ATs.k], [k2T.k])
    ts("dve", qgB[:, :], v64[:, 0:64], 0.125, None, ALU.mult, None, [v64.k], [qgB.k])
    ts("dve", subgB[:, :], v128[:, 0:128], 0.8, None, ALU.mult, None, [v128.k], [subgB.k])
    tt("dve", lamt[:, :], v64[:, 128:192], v64[:, 192:256], ALU.mult, [v64.k], [lamt.k])
    red("dve", lams[:, 0:1], lamt[:, :], ALU.add, [lamt.k], [lams.k])
    tt("dve", lamt[:, :], v64[:, 256:320], v64[:, 320:384], ALU.mult, [v64.k, lams.k], [lamt.k])
    red("dve", lams[:, 1:2], lamt[:, :], ALU.add, [lamt.k], [lams.k])
    act(lams[:, 2:4], lams[:, 0:2], AF.Exp, [lams.k], [lams.k])
    tt("dve", lams[:, 0:1], lams[:, 3:4], lams[:, 2:3], ALU.subtract, [lams.k], [lams.k])
    ts("dve", neglam[:, :], lams[:, 0:1], -0.2, None, ALU.add, None, [lams.k], [neglam.k])
    for i in range(NT):
        S.op("pool", lambda e, i=i: e.memset(VC[i][:, :, :], 1.0), (), [VC[i].k])

    def adaln(b):
        act(sT[:, :], cT[:, :, b], AF.Silu, [cT.k], [sT.k])
        ld(gt1B[:, :], bada_d[0:1, 2 * D:3 * D].to_broadcast([128, D]), gt1B)
        ld(gt2B[:, :], bada_d[0:1, 5 * D:6 * D].to_broadcast([128, D]), gt2B)
        wi = 0
        for sec in range(6):
            for q8 in range(8):
                w = wst[wi % 2]
                wi += 1
                c0 = sec * D + q8 * 128
                ld(w[:, :, :], wada_d[:, c0:c0 + 128].rearrange("(k p) n -> p k n", p=128), w)
                ps = psP[wi % 2]
                if sec in (2, 5):
                    for kc in range(8):
                        mm(ps[:, 0:128], sT[:, kc:kc + 1].to_broadcast([128, 128]), w[:, kc, :],
                           kc == 0, kc == 7, [sT.k, w.k], [ps.k], inc=(kc == 7))
                    g = gt1B if sec == 2 else gt2B
                    tt("dve", g[:, q8 * 128:(q8 + 1) * 128], g[:, q8 * 128:(q8 + 1) * 128], ps[:, 0:128], ALU.add,
                       [ps.k, g.k], [g.k])
                else:
                    mi = {0: 0, 1: 1, 3: 2, 4: 3}[sec]
                    for kc in range(8):
                        mm(ps[:, 0:1], w[:, kc, :], sT[:, kc:kc + 1],
                           kc == 0, kc == 7, [sT.k, w.k], [ps.k], inc=(kc == 7))
                    tt("dve", modT[:, mi, q8:q8 + 1], ps[:, 0:1],
                       badaT[:, sec * 8 + q8:sec * 8 + q8 + 1], ALU.add, [ps.k, badaT.k], [modT.k])
        stt("dve", gm1[:, :], modT[:, 1, :], 1.0, g1T[:, :], ALU.add, ALU.mult, [modT.k, g1T.k], [gm1.k])
        stt("dve", gm2[:, :], modT[:, 3, :], 1.0, g2T[:, :], ALU.add, ALU.mult, [modT.k, g2T.k], [gm2.k])

    def norm_T(xap, xkey, gm, shi, dst, dcol):
        act(junk[:, :], xap, AF.Square, [xkey], [junk.k, st1.k], accum_out=st1[:, 0:1])
        rsqrt_mean(st1[:, 2:3], st1[:, 0:1], D, [st1.k])
        ts("dve", xn[:, :], xap, st1[:, 2:3], None, ALU.mult, None, [xkey, st1.k], [xn.k])
        for j in range(8):
            tr(psT[:, j * 128:(j + 1) * 128], xn[:, j * 128:(j + 1) * 128], identb[:, :],
               [xn.k, identb.k], [psT.k], inc=(j == 7))
        for j in range(8):
            act(dst[:, j, dcol:dcol + 128], psT[:, j * 128:(j + 1) * 128], AF.Identity,
                [psT.k, gm.k, modT.k], [dst.k], bias=modT[:, shi, j:j + 1], scale=gm[:, j:j + 1], ww_ok=True)

    def rope_tables(col):
        C1 = 6.28125
        C2 = 2.0 * math.pi - C1
        ts("dve", ang[:, :], C("invf"), posf[:, col:col + 1], None, ALU.mult, None, [cst.k, posf.k], [ang.k])
        for (shift, dst) in ((0.0, sinT), (0.5 * math.pi, cosT)):
            ts("dve", ang2[:, :], ang[:, :], shift, 1.0 / (2.0 * math.pi), ALU.add, ALU.mult, [ang.k], [ang2.k])
            cp("dve", angi[:, :], ang2[:, :], [ang2.k], [angi.k])
            cp("dve", ang2[:, :], angi[:, :], [angi.k], [ang2.k])
            stt("dve", ang3[:, :], ang2[:, :], -C1, ang[:, :], ALU.mult, ALU.add, [ang2.k, ang.k], [ang3.k])
            stt("dve", ang3[:, :], ang2[:, :], -C2, ang3[:, :], ALU.mult, ALU.add, [ang2.k, ang3.k], [ang3.k])
            ts("dve", ang3[:, :], ang3[:, :], shift, math.pi, ALU.add, ALU.min, [ang3.k], [ang3.k])
            ts("dve", ang3[:, :], ang3[:, :], -math.pi, None, ALU.max, None, [ang3.k], [ang3.k])
            act(dst[:, :], ang3[:, :], AF.Sin, [ang3.k], [dst.k])

    def proj_block(c0, ncols, ps, lhs=None):
        r = ring_next()
        ld(r[:, :, 0:ncols], win_s[:, c0:c0 + ncols].rearrange("(k p) n -> p k n", p=128), r, reads=[k_win_s])
        for kc in range(8):
            mm(ps[:, 0:ncols], hT[:, kc, :], r[:, kc, 0:ncols], kc == 0, kc == 7,
               [hT.k, r.k], [ps.k], inc=(kc == 7))
        return r

    def qk_post(ps, gB, dstT, dkey):
        act(sq[:, :], ps[:, :], AF.Square, [ps.k], [sq.k])
        red("dve", ss8[:, :], sq[:, :].rearrange("p (g d) -> p g d", d=64), ALU.add, [sq.k], [ss8.k])
        rsqrt_mean(ss8[:, :], ss8[:, :], 64, [ss8.k])
        q3 = qn[:, :].rearrange("p (g d) -> p g d", d=64)
        tt("dve", q3, ps[:, :].rearrange("p (g d) -> p g d", d=64),
           ss8[:, :].unsqueeze(2).to_broadcast([128, 8, 64]), ALU.mult, [ps.k, ss8.k], [qn.k])
        tt("dve", q3, q3, gB.unsqueeze(1).to_broadcast([128, 8, 64]), ALU.mult, [qn.k, v64.k, qgB.k], [qn.k])
        x1v = q3[:, :, 0:32]
        x2v = q3[:, :, 32:64]
        cb = cosT[:, :].unsqueeze(1).to_broadcast([128, 8, 32])
        sbb = sinT[:, :].unsqueeze(1).to_broadcast([128, 8, 32])
        r0, r1 = (t[:, :].rearrange("p (g d) -> p g d", d=32) for t in rt)
        qr3 = qr[:, :].rearrange("p (g d) -> p g d", d=64)
        tt("dve", r0, x1v, cb, ALU.mult, [qn.k, cosT.k], [rt[0].k])
        tt("dve", r1, x2v, sbb, ALU.mult, [qn.k, sinT.k], [rt[1].k])
        tt("dve", qr3[:, :, 0:32], r0, r1, ALU.subtract, [rt[0].k, rt[1].k], [qr.k])
        tt("dve", r0, x2v, cb, ALU.mult, [qn.k, cosT.k], [rt[0].k])
        tt("dve", r1, x1v, sbb, ALU.mult, [qn.k, sinT.k], [rt[1].k])
        tt("dve", qr3[:, :, 32:64], r0, r1, ALU.add, [rt[0].k, rt[1].k], [qr.k])
        for h in range(4):
            tr(psT[:, h * 128:(h + 1) * 128], qr[:, h * 128:(h + 1) * 128], identb[:, :],
               [qr.k, identb.k], [psT.k], inc=(h == 3))
        cp("dve", dstT[:, :, :], psT[:, 0:512].rearrange("p (h t) -> p h t", t=128),
           [psT.k], [dkey])

    dbg_toks = []

    def dump(name, ap, key):
        if name in dbg_d:
            dbg_toks.append(S.dma("sp", dbg_d[name], ap, owner=key, reads=[key]))

    def mixer_tile(b, i, slot):
        col = b * NT + i
        tok0 = b * SEQ + i * 128
        xs = x1[:, slot, :]
        xkey = xk[slot]
        S.dma("sp", xs, x_d[tok0:tok0 + 128, :], owner=xkey, writes=[xkey])
        if slevel < 2:
            return
        norm_T(xs, xkey, gm1, 0, hT, 0)
        if i == 0:
            rope_tables(col)
        if slevel < 3:
            return
        proj_block(0, 512, psP[0])
        qk_post(psP[0], qgB[:, :], qT, qT.k)
        proj_block(512, 512, psP[1])
        qk_post(psP[1], v64[:, 64:128], KT[i], KT[i].k)
        proj_block(1024, 512, psP[0])
        cp("act", VC[i][:, :, 0:128], psP[0][:, :].rearrange("p (h e) -> p h e", e=128), [psP[0].k], [VC[i].k])
        if slevel < 4:
            return
        proj_block(1536, 512, psP[1])
        cp("act", gqk[:, :], psP[1][:, :], [psP[1].k], [gqk.k])
        proj_block(2048, 512, psP[0])
        cp("act", gv[:, :], psP[0][:, :], [psP[0].k], [gv.k])
        proj_block(2560, 512, psP[1])
        act(sr[:, :], psP[1][:, :], AF.Silu, [psP[1].k], [sr.k])
        r = ring_next()
        ld(r[:, :, 0:16], win_s[:, 3072:3088].rearrange("(k p) n -> p k n", p=128), r, reads=[k_win_s])
        for kc in range(8):
            mm(psP[0][0:16, 0:128], r[:, kc, 0:16], hT[:, kc, :], kc == 0, kc == 7,
               [hT.k, r.k], [psP[0].k], inc=(kc == 7))
        cp("act", ggT[:, :], psP[0][0:16, 0:128], [psP[0].k], [ggT.k])

        if slevel < 5:
            return

        def attn_part():
            nkb = i + 1
            ngrp = (nkb + 3) // 4
            sidx = 0
            for h in range(4):
                for m in range(2):
                    pr = slice(m * 64, (m + 1) * 64)
                    for g in range(ngrp):
                        j0 = g * 4
                        nj = min(4, nkb - j0)
                        pss = psS[sidx % 2]
                        pt = PT[sidx % 2]
                        sidx += 1
                        for jj in range(nj):
                            j = j0 + jj
                            mm(pss[:, jj * 128:(jj + 1) * 128], KT[j][pr, h, :], qT[pr, h, :], True, True,
                               [KT[j].k, qT.k], [pss.k], inc=(jj == nj - 1))
                        act(pt[:, 0:nj * 128], pss[:, 0:nj * 128], AF.Exp, [pss.k], [pt.k])
                        if j0 + nj - 1 == i:
                            dsl = slice((nj - 1) * 128, nj * 128)
                            tt("pool", pt[:, dsl], pt[:, dsl], causb[:, :], ALU.mult, [pt.k, causb.k], [pt.k])
                        for jj in range(nj):
                            j = j0 + jj
                            mm(psO[:, m * 129:(m + 1) * 129], pt[:, jj * 128:(jj + 1) * 128], VC[j][:, h, 0:129],
                               j == 0, j == i, [pt.k, VC[j].k], [psO.k], inc=(jj == nj - 1))
                S.op("dve", lambda e: e.reciprocal(out=rz[:, 0:1], in_=psO[:, 128:129]), [psO.k], [rz.k])
                S.op("dve", lambda e: e.reciprocal(out=rz[:, 1:2], in_=psO[:, 257:258]), [psO.k], [rz.k])
                tt("dve", rz[:, 2:3], rz[:, 1:2], neglam[:, :], ALU.mult, [rz.k, neglam.k], [rz.k])
                ts("dve", osb[:, :], psO[:, 0:128], rz[:, 0:1], None, ALU.mult, None, [psO.k, rz.k], [osb.k])
                stt("dve", osb[:, :], psO[:, 129:257], rz[:, 2:3], osb[:, :], ALU.mult, ALU.add,
                    [psO.k, rz.k, osb.k], [osb.k])
                act(junk[:, 0:128], osb[:, :], AF.Square, [osb.k], [junk.k, rz.k], accum_out=rz[:, 3:4])
                rsqrt_mean(rz[:, 3:4], rz[:, 3:4], 128, [rz.k])
                stt("dve", ybf[:, h * 128:(h + 1) * 128], osb[:, :], rz[:, 3:4], subgB[:, :], ALU.mult, ALU.mult,
                    [osb.k, rz.k, subgB.k], [ybf.k])


        def gla_part():
            mm(psG[:, 0:256], ggT[:, :], wg2[:, :], True, False, [ggT.k, wg2.k], [psG.k], inc=False)
            mm(psG[:, 0:256], onesr[:, :], bgr[:, :], False, True, [onesr.k, bgr.k], [psG.k])
            act(la[:, :], psG[:, 0:256], AF.Exp, [psG.k], [la.k], scale=-1.0)
            act(la[:, :], la[:, :], AF.Ln, [la.k, cvals.k], [la.k], bias=cvals[:, 2:3])
            ts("dve", la[:, :], la[:, :], -1.0 / 16, None, ALU.mult, None, [la.k], [la.k])
            if GSUB < 1:
                return
            mm(psG[:, 0:256], C("mcum"), la[:, :], True, True, [cst.k, la.k], [psG.k], inc=False)
            mm(psG[:, 256:512], C("mmid"), la[:, :], True, True, [cst.k, la.k], [psG.k])
            mm(psG2[:, 0:256], C("mblk"), la[:, :], True, True, [cst.k, la.k], [psG2.k], inc=False)
            for hp in range(2):
                for hh in range(2):
                    h = hp * 2 + hh
                    mm(psG2[hh * 64:(hh + 1) * 64, 256 + hp * 2:256 + hp * 2 + 2], la[:, h * 64:(h + 1) * 64],
                       C("chunkind"), True, True, [la.k, cst.k], [psG2.k], inc=(hp == 1 and hh == 1))
            if GSUB < 2:
                return
            bc, d1, eq, ek, eb, d2, ed, qg = gl
            cp("dve", bc[:, :], psG[:, 0:256], [psG.k], [bc.k])
            tt("dve", d1[:, :], bc[:, :], psG[:, 256:512], ALU.subtract, [bc.k, psG.k], [d1.k])
            tt("dve", d2[:, :], psG2[:, 0:256], bc[:, :], ALU.subtract, [bc.k, psG2.k], [d2.k])
            act(dec[:, :, :], psG2[:, 256:260].rearrange("p (a c) -> p a c", c=2), AF.Exp, [psG2.k], [dec.k])
            act(eq[:, :], d1[:, :], AF.Exp, [d1.k], [eq.k])
            act(ek[:, :], d1[:, :], AF.Exp, [d1.k], [ek.k], scale=-1.0)
            act(eb[:, :], bc[:, :], AF.Exp, [bc.k], [eb.k])
            act(ed[:, :], d2[:, :], AF.Exp, [d2.k], [ed.k])
            stt("dve", eq[:, :], gqk[:, 0:256], 0.125, eq[:, :], ALU.mult, ALU.mult, [gqk.k, eq.k], [eq.k])
            tt("dve", ek[:, :], gqk[:, 256:512], ek[:, :], ALU.mult, [gqk.k, ek.k], [ek.k])
            stt("dve", eb[:, :], gqk[:, 0:256], 0.125, eb[:, :], ALU.mult, ALU.mult, [gqk.k, eb.k], [eb.k])
            tt("dve", ed[:, :], gqk[:, 256:512], ed[:, :], ALU.mult, [gqk.k, ed.k], [ed.k])
            ci = C("chunkind")
            ts("dve", d1[:, :], ed[:, :], ci[:, 0:1], None, ALU.mult, None, [ed.k, cst.k, d1.k], [d1.k])
            ts("dve", d2[:, :], ed[:, :], ci[:, 1:2], None, ALU.mult, None, [ed.k, cst.k, d2.k], [d2.k])
            if GSUB < 3:
                return
            for hp in range(2):
                cs = slice(hp * 128, (hp + 1) * 128)
                mm(psG[:, 0:128], eq[:, cs], C("ident"), True, True, [eq.k, cst.k], [psG.k], inc=False)
                mm(psG[:, 128:256], ek[:, cs], C("ident"), True, True, [ek.k, cst.k], [psG.k], inc=False)
                mm(psG[:, 256:384], eb[:, cs], C("ident"), True, True, [eb.k, cst.k], [psG.k])
                cp("act", T3[:, :, :], psG[:, 0:384].rearrange("p (a t) -> p a t", t=128), [psG.k], [T3.k])
                if GSUB < 4:
                    continue
                st = Sst[hp]
                if i == 0:
                    S.op("dve", lambda e, st=st: e.memset(st[:, :], 0.0), (), [st.k])
                for hh in range(2):
                    h = hp * 2 + hh
                    pr = slice(hh * 64, (hh + 1) * 64)
                    vcols = slice(h * 128, (h + 1) * 128)
                    for c, kdm in enumerate((d1, d2)):
                        mm(psG2[pr, c * 128:(c + 1) * 128], kdm[:, h * 64:(h + 1) * 64], gv[:, vcols], True, True,
                           [kdm.k, gv.k], [psG2.k], inc=False)
                    mm(psG2[:, 256:384], T3[pr, 1, :], T3[pr, 0, :], True, True, [T3.k], [psG2.k])
                    tt("dve", ATs[:, :], psG2[:, 256:384], C("mcum"), ALU.mult, [psG2.k, cst.k], [ATs.k])
                    if GSUB < 5:
                        continue
                    mm(psP[0][:, 0:128], ATs[:, :], gv[:, vcols], True, False, [ATs.k, gv.k], [psP[0].k], inc=False, skip=True)
                    mm(psP[0][0:64, 0:128], T3[pr, 2, 0:64], st[pr, :], False, False, [T3.k, st.k], [psP[0].k], inc=True,
                       skip=True)
                    if GSUB < 6:
                        continue
                    stt("dve", st[pr, :], st[pr, :], dec[pr, hp, 0:1], psG2[pr, 0:128], ALU.mult, ALU.add,
                        [st.k, dec.k, psG2.k], [st.k])
                    mm(psP[0][64:128, 0:128], T3[pr, 2, 64:128], st[pr, :], False, True, [T3.k, st.k], [psP[0].k], skip=True)
                    if GSUB < 7:
                        continue
                    stt("dve", st[pr, :], st[pr, :], dec[pr, hp, 1:2], psG2[pr, 128:256], ALU.mult, ALU.add,
                        [st.k, dec.k, psG2.k], [st.k])
                    if GSUB < 8:
                        continue
                    act(junk2[:, 0:128], psP[0][:, 0:128], AF.Square, [psP[0].k], [junk2.k, rz2.k], accum_out=rz2[:, 3:4])
                    rsqrt_mean(rz2[:, 3:4], rz2[:, 3:4], 128, [rz2.k])
                    stt("dve", osb2[:, :], psP[0][:, 0:128], rz2[:, 3:4], v128[:, 128:256], ALU.mult, ALU.mult,
                        [psP[0].k, rz2.k, v128.k], [osb2.k])
                    tt("dve", ybf[:, 512 + h * 128:512 + (h + 1) * 128], osb2[:, :], sr[:, vcols], ALU.mult,
                       [osb2.k, sr.k], [ykey2])


        if slevel >= 6 and INTERLEAVE:
            co = Co(gla_part)
            S.co = co
            attn_part()
            S.co = None
            co.finish()
        else:
            attn_part()
            if slevel >= 6:
                gla_part()
        if slevel < 7:
            return
        for j in range(8):
            tr(psT[:, j * 128:(j + 1) * 128], ybf[:, j * 128:(j + 1) * 128], identb[:, :],
               [ybf.k, ykey2, identb.k], [psT.k], inc=(j == 7))
        cp("act", yT[:, :, :], psT[:, :].rearrange("p (j t) -> p j t", t=128), [psT.k], [yT.k])
        for half in range(2):
            r = ring_next()
            ld(r[:, :, :], wout_s[:, half * 512:(half + 1) * 512].rearrange("(k p) n -> p k n", p=128), r, reads=[k_wout_s])
            ps = psP[half]
            for kc in range(8):
                mm(ps[:, :], yT[:, kc, :], r[:, kc, :], kc == 0, kc == 7, [yT.k, r.k], [ps.k],
                   inc=(kc == 7))
            cs = slice(half * 512, (half + 1) * 512)
            tt("dve", sq[:, :], ps[:, :], gt1B[:, cs], ALU.mult, [ps.k, gt1B.k], [sq.k])
            tt("dve", x1[:, slot, cs], sq[:, :], x1[:, slot, cs], ALU.add, [sq.k, xkey], [xkey])
        if do_peer:
            norm_T(xs, xkey, gm2, 2, h2T, slot * 128)
        if i + 1 < NT:
            rope_tables(col + 1)

    if do_peer:
        qpc = [sb("qpc%d" % i, [128, ST * 128], BF16) for i in range(2)]
        s1m = sb("s1m", [128, ST, 8, 128], F32)
        s2m = sb("s2m", [128, ST, 8, 128], F32)
        wk = sb("wk", [128, 256], F32)
        v1 = sb("v1", [128, 8, 16], F32)
        v2 = sb("v2", [128, 8, 16], F32)
        c24 = sb("c24", [128, 8, 24], F32)
        thr = sb("thr", [128, 8], F32)
        nrm = sb("nrm", [128, 8], F32)
        ex16 = sb("ex16", [128, 8, 16], F32)
        DG = [sb("DG%d" % t, [128, 8, 128], BF16) for t in range(ST)]
        zbs = [sb("zb%d" % i, [128, 8, 2, 128], BF16) for i in range(NZBUF)]
        zk2s = [Key("zk2_%d" % i) for i in range(NZBUF)]
        zb = sb("candb", [128, 8, 256], F32)
        cand = zb[:, :, :]
        wbs = [sb("wb%d" % i, [128, 8, 2, 128], BF16) for i in range(2)]
        mbs = [sb("mb%d" % i, [128, 8, 2, 128], BF16) for i in range(2)]
        gas = [sb("ga%d" % i, [128, 2, ST * 128], BF16) for i in range(2)]
        GTs = [sb("GT%d" % i, [128, 2, 128], BF16) for i in range(2)]

    out_toks = []

    def top16(src_ap, src_keys, dst, h):
        n = src_ap.shape[-1]
        S.op("dve", lambda e: e.max(out=dst[:, h, 0:8], in_=src_ap), src_keys, [dst.k])
        S.op("dve", lambda e: e.match_replace(out=wk[:, 0:n], in_to_replace=dst[:, h, 0:8], in_values=src_ap,
                                              imm_value=-BIG), list(src_keys) + [dst.k], [wk.k])
        S.op("dve", lambda e: e.max(out=dst[:, h, 8:16], in_=wk[:, 0:n]), [wk.k], [dst.k])

    def peer_supertile(b, st_i):
        T2 = ST * 128
        for blk in range(4):
            r = ring_next()
            ld(r[:, :, :], wq_s[:, blk * 512:(blk + 1) * 512].rearrange("(k p) n -> p k n", p=128), r, reads=[k_wq_s])
            for c4 in range(4):
                cc = blk * 4 + c4
                h, half = cc // 2, cc % 2
                ps = psP[cc % 2]
                qc = qpc[cc % 2]
                for kc in range(8):
                    mm(ps[:, 0:T2], r[:, kc, c4 * 128:(c4 + 1) * 128], h2T[:, kc, :], kc == 0, kc == 7,
                       [r.k, h2T.k], [ps.k], inc=(kc == 7))
                act(qc[:, :], ps[:, 0:T2], AF.Identity, [ps.k, bqT.k], [qc.k], bias=bqT[:, cc:cc + 1])
                pss = psS[cc % 2]
                kT = k1T if half == 0 else k2T
                dstm = s1m if half == 0 else s2m
                for t in range(ST):
                    mm(pss[:, t * 128:(t + 1) * 128], qc[:, t * 128:(t + 1) * 128], kT[:, :], True, True,
                       [qc.k, kT.k], [pss.k], inc=(t == ST - 1))
                cp("dve", dstm[:, :, h, :], pss[:, 0:T2].rearrange("p (t k) -> p t k", k=128), [pss.k], [dstm.k])
        for t in range(ST):
            for h in range(8):
                top16(s1m[:, t, h, :], [s1m.k], v1, h)
                top16(s2m[:, t, h, :], [s2m.k], v2, h)
            tt("dve", cand.rearrange("p h (a b) -> p h a b", b=16),
               v1[:, :, :].unsqueeze(3).to_broadcast([128, 8, 16, 16]),
               v2[:, :, :].unsqueeze(2).to_broadcast([128, 8, 16, 16]), ALU.add, [v1.k, v2.k], [zb.k])
            for h in range(8):
                top16(cand[:, h, :], [zb.k], c24, h)
                S.op("dve", lambda e, h=h: e.match_replace(out=wk[:, :], in_to_replace=c24[:, h, 8:16],
                                                           in_values=wk[:, :], imm_value=-BIG),
                     [wk.k, c24.k], [wk.k])
                S.op("dve", lambda e, h=h: e.max(out=c24[:, h, 16:24], in_=wk[:, :]), [wk.k], [c24.k])
            tt("dve", thr[:, :], c24[:, :, 15], c24[:, :, 16], ALU.add, [c24.k], [thr.k])
            ts("dve", thr[:, :], thr[:, :], 0.5, None, ALU.mult, None, [thr.k], [thr.k])
            tt("dve", ex16[:, :, :], c24[:, :, 0:16], thr[:, :].unsqueeze(2).to_broadcast([128, 8, 16]),
               ALU.subtract, [c24.k, thr.k], [ex16.k])
            act(ex16[:, :, :], ex16[:, :, :], AF.Exp, [ex16.k], [ex16.k])
            red("dve", nrm[:, :], ex16[:, :, :], ALU.add, [ex16.k], [nrm.k])
            S.op("dve", lambda e: e.reciprocal(out=nrm[:, :], in_=nrm[:, :]), [nrm.k], [nrm.k])
            ts("dve", nrm[:, :], nrm[:, :], 1.0 / CSH, None, ALU.mult, None, [nrm.k], [nrm.k])
            for h in range(8):
                ts("dve", DG[t][:, h, :], C("ident"), nrm[:, h:h + 1], None, ALU.mult, None, [cst.k, nrm.k],
                   [DG[t].k])
            for (sm, vv, sub_thr) in ((s1m, v1, True), (s2m, v2, False)):
                mskt = zb[:, :, 0:128]
                tt("dve", mskt, sm[:, t, :, :], vv[:, :, 15:16].to_broadcast([128, 8, 128]), ALU.is_lt,
                   [sm.k, vv.k], [zb.k])
                stt("dve", sm[:, t, :, :], mskt, -BIG, sm[:, t, :, :], ALU.mult, ALU.add,
                    [zb.k, sm.k], [sm.k])
                if sub_thr:
                    tt("dve", sm[:, t, :, :], sm[:, t, :, :], thr[:, :].unsqueeze(2).to_broadcast([128, 8, 128]),
                       ALU.subtract, [sm.k, thr.k], [sm.k])
                act(sm[:, t, :, :], sm[:, t, :, :], AF.Exp, [sm.k], [sm.k])
                if sub_thr:
                    ts("dve", sm[:, t, :, :], sm[:, t, :, :], CSH, None, ALU.mult, None, [sm.k], [sm.k])
        psWs = (psS[0], psP[0])
        psA = psS[1]
        psV = ((psO, psG), (psG2, psP[1]))
        NG = NEXP // 512
        slots = {}

        def load_group(g):
            ru = ring_next()
            ld(ru[:, :, :], uT_s[:, g * 512:(g + 1) * 512].rearrange("(k p) n -> p k n", p=128), ru, reads=[k_uT_s])
            rv = ring_next()
            rv4 = rv[:, :, :].rearrange("p (c a) n -> p c (a n)", a=2)
            ld(rv4, v_s[g * 512:(g + 1) * 512, :].rearrange("(c p) d -> p c d", p=128), rv, reads=[k_v_s])
            slots[g] = (ru, rv, rv4)

        def stage_A_mm(g, sub):
            ru = slots[g][0]
            for ii in range(2):
                ec = sub * 2 + ii
                for kc in range(8):
                    mm(psA[:, ii * T2:(ii + 1) * T2], ru[:, kc, ec * 128:(ec + 1) * 128], h2T[:, kc, :],
                       kc == 0, kc == 7, [ru.k, h2T.k], [psA.k], inc=(kc == 7))

        def stage_A_gelu(g, sub):
            gt_ = gas[(g * 2 + sub) % 2]
            act(gt_[:, :, :], psA[:, 0:2 * T2].rearrange("p (a t) -> p a t", t=T2), AF.Gelu, [psA.k], [gt_.k])

        its = [(g, sub, t) for g in range(NG) for sub in range(2) for t in range(ST)]

        NZ = len(zbs)

        NHA = NHA_ACT

        def stage_P(k):
            g, sub, t = its[k]
            i0 = g * 4 + sub * 2
            zt = zbs[k % NZ]
            zk2 = zk2s[k % NZ]
            for h in range(NHA):
                for ii in range(2):
                    act(zt[:, h, ii, :], s2m[:, t, h, :], AF.Identity, [s1m.k, s2m.k], [zt.k],
                        scale=s1m[:, t, h, i0 + ii:i0 + ii + 1], ww_ok=True)
            nd = 8 - NHA
            tt("dve", zt[:, NHA:8, :, :],
               s1m[:, t, NHA:8, i0:i0 + 2].unsqueeze(3).to_broadcast([128, nd, 2, 128]),
               s2m[:, t, NHA:8, :].unsqueeze(2).to_broadcast([128, nd, 2, 128]), ALU.mult,
               [s1m.k, s2m.k], [zk2])

        def stage_M(k):
            zt = zbs[k % NZ]
            zk2 = zk2s[k % NZ]
            wt = wbs[k % 2]
            mt = mbs[k % 2]
            ts("dve", mt[:, :, :, :], zt[:, :, :, :], 1.0, None, ALU.is_ge, None, [zt.k, zk2], [mt.k])
            tt("dve", wt[:, :, :, :], zt[:, :, :, :], mt[:, :, :, :], ALU.mult, [zt.k, zk2, mt.k], [wt.k])

        def stage_W(k):
            g, sub, t = its[k]
            wt = wbs[k % 2]
            pw = psWs[k % 2]
            first = True
            for h in range(8):
                for ii in range(2):
                    mm(pw[:, ii * 128:(ii + 1) * 128], wt[:, h, ii, :], DG[t][:, h, :], first, (h == 7),
                       [wt.k, DG[t].k], [pw.k], inc=(h == 7 and ii == 1), skip=True)
                    first = False

        def stage_G(k):
            g, sub, t = its[k]
            pw = psWs[k % 2]
            gt_ = gas[(g * 2 + sub) % 2]
            tt("dve", GTs[k % 2][:, :, :], gt_[:, :, t * 128:(t + 1) * 128],
               pw[:, 0:256].rearrange("p (a t) -> p a t", t=128), ALU.mult, [gt_.k, pw.k], [GTs[k % 2].k])

        def stage_V(k):
            g, sub, t = its[k]
            rv, rv4 = slots[g][1], slots[g][2]
            for ii in range(2):
                ec = sub * 2 + ii
                for half in range(2):
                    pv = psV[t][half]
                    mm(pv[:, :], GTs[k % 2][:, ii, :], rv4[:, ec, half * 512:(half + 1) * 512],
                       (g == 0 and sub == 0 and ii == 0), (g == NG - 1 and sub == 1 and ii == 1),
                       [GTs[k % 2].k, rv.k], [pv.k], inc=(ii == 1 and half == 1))

        N = len(its)
        load_group(0)
        stage_A_mm(0, 0)
        stage_A_gelu(0, 0)
        stage_P(0)
        for k in range(N + 1):
            if k + 1 < N:
                stage_P(k + 1)
            if k < N:
                stage_M(k)
                stage_W(k)
            if k >= 1:
                stage_G(k - 1)
                stage_V(k - 1)
            if k < N:
                g, sub, t = its[k]
                ng, nsub = (g, 1) if sub == 0 else (g + 1, 0)
                if ng < NG:
                    if t == 0:
                        if nsub == 0:
                            load_group(ng)
                        stage_A_mm(ng, nsub)
                    else:
                        stage_A_gelu(ng, nsub)
        for t in range(ST):
            for half in range(2):
                cs = slice(half * 512, (half + 1) * 512)
                tt("dve", sq[:, :], psV[t][half][:, :], gt2B[:, cs], ALU.mult, [psV[t][half].k, gt2B.k], [sq.k])
                tt("dve", x1[:, t, cs], sq[:, :], x1[:, t, cs], ALU.add, [sq.k, xk[t]], [xk[t]])
            tok0 = b * SEQ + (st_i * ST + t) * 128
            out_toks.append(S.dma("sp", y_d[tok0:tok0 + 128, :], x1[:, t, :], owner=xk[t], reads=[xk[t]]))

    for b in range(NB):
        if slevel >= 1:
            adaln(b)
        for st_i in range(NST):
            for t in range(ST):
                mixer_tile(b, st_i * ST + t, t)
            if do_peer:
                peer_supertile(b, st_i)
            else:
                for t in range(ST):
                    tok0 = b * SEQ + (st_i * ST + t) * 128
                    out_toks.append(S.dma("sp", y_d[tok0:tok0 + 128, :], x1[:, t, :], owner=xk[t], reads=[xk[t]]))
    print("sbuf bytes remaining", nc.sbuf_bytes_remaining)
    S.wait_all("sp", out_toks + dbg_toks)
    S.emit()
    return nc, S


def make_in_maps(inputs, n_cores, NB, SEQ):
    f = lambda a: np.ascontiguousarray(np.asarray(a, dtype=np.float32))
    x = f(inputs["x"])
    c = f(inputs["c"])
    pos = np.asarray(inputs["positions"]).astype(np.int32)
    NT = SEQ // 128
    cblob, _ = _consts()
    shared = {
        "w_ada": f(inputs["w_ada"][0]),
        "b_adaT": f(np.asarray(inputs["b_ada"][0]).reshape(48, 128).T),
        "b_ada": f(np.asarray(inputs["b_ada"][0]).reshape(1, -1)),
        "g1T": f(np.asarray(inputs["norm1_g"][0]).reshape(8, 128).T),
        "g2T": f(np.asarray(inputs["norm2_g"][0]).reshape(8, 128).T),
        "w_in": f(inputs["w_in"][0]),
        "w_out": f(inputs["w_out"][0]),
        "w_query": f(inputs["w_query"][0]),
        "b_queryT": f(np.asarray(inputs["b_query"][0]).reshape(16, 128).T),
        "keys1T": f(np.asarray(inputs["peer_keys1"][0]).T),
        "keys2T": f(np.asarray(inputs["peer_keys2"][0]).T),
        "uT": f(np.asarray(inputs["expert_u"][0]).T),
        "ev": f(inputs["expert_v"][0]),
        "vec64": f(np.concatenate([np.asarray(inputs[k][0]).reshape(-1) for k in
                                   ("qn_g", "kn_g", "lam_q1", "lam_k1", "lam_q2", "lam_k2")]).reshape(1, -1)),
        "vec128": f(np.concatenate([np.asarray(inputs[k][0]).reshape(-1) for k in
                                    ("diff_norm_g", "gla_norm_g")]).reshape(1, -1)),
        "w_gate2": f(inputs["w_gate2"][0]),
        "b_gate": f(np.asarray(inputs["b_gate"][0]).reshape(1, -1)),
        "consts": cblob,
    }
    maps = []
    for i in range(n_cores):
        bs = slice(i * NB, (i + 1) * NB)
        m = dict(shared)
        m["x"] = np.ascontiguousarray(x[bs].reshape(NB * SEQ, D))
        m["cT"] = np.ascontiguousarray(c[bs].T)
        m["posT"] = np.ascontiguousarray(pos[bs].reshape(NB * NT, 128).T)
        maps.append(m)
    return maps


def kernel(**inputs):
    x = np.asarray(inputs["x"])
    B, SEQ, _ = x.shape
    NB = B // NCORES
    nc, _ = build_program(NB, SEQ, do_peer=True)
    maps = make_in_maps(inputs, NCORES, NB, SEQ)
    res = run_bass_kernel_spmd(nc, maps, core_ids=list(range(NCORES)))
    out = np.concatenate([np.asarray(r["y"]).reshape(NB, SEQ, D) for r in res.results], axis=0)
    return out.astype(np.float32)
```
